# Optimizing a Trainium2 kernel written in Bass

```python
import jax, jax.numpy as jnp
from jax import lax
import numpy as np

D_MODEL = 1024
BATCH = 8
SEQ = 2048
DEPTH = 4

D_MIX = D_MODEL
POOL_WIDTH = D_MIX // 4
POOL_WINDOWS = (2, 4, 8, 16)
POOL_GROUPS = len(POOL_WINDOWS)
POOL_GROUP_DIM = POOL_WIDTH // POOL_GROUPS
HGRN_WIDTH = D_MIX // 4
HGRN_HEAD_DIM = 64
HGRN_HEADS = HGRN_WIDTH // HGRN_HEAD_DIM
HGRN_CHUNK = 64
LOG_F_FLOOR = 1e-30
ATTN_WIDTH = D_MIX - POOL_WIDTH - HGRN_WIDTH
HEAD_DIM = 64
N_HEADS = ATTN_WIDTH // HEAD_DIM
N_KV_HEADS = 2
KV_GROUP = N_HEADS // N_KV_HEADS
KV_WIDTH = N_KV_HEADS * HEAD_DIM
IDX_HEADS = 4
IDX_DIM = 64
TOPK_MAX = 256
Q_BLOCK = 128
MASK_VALUE = -1e30
ROPE_THETA = 500000.0
ROT_DIM = HEAD_DIM // 4
D_FF = 4 * D_MODEL
RMS_EPS = 1e-5
IN_SIZES = (POOL_WIDTH,
            HGRN_WIDTH, HGRN_WIDTH, HGRN_WIDTH, HGRN_WIDTH,
            ATTN_WIDTH, KV_WIDTH, KV_WIDTH,
            IDX_HEADS * IDX_DIM, IDX_DIM, IDX_HEADS)
D_IN = sum(IN_SIZES)

kernel_name = "hybrid_pool_hgrn2_dsa_trunk"


def rms_norm(x, g):
    xf = x.astype(jnp.float32)
    y = xf * lax.rsqrt(jnp.mean(xf * xf, axis=-1, keepdims=True) + RMS_EPS)
    return (y * g.astype(jnp.float32)).astype(x.dtype)


def split_cols(a, sizes):
    outs, start = [], 0
    for s in sizes:
        outs.append(a[..., start:start + s])
        start += s
    return outs


def rope_tables(positions):
    inv_freq = ROPE_THETA ** (-jnp.arange(0, ROT_DIM, 2, dtype=jnp.float32) / ROT_DIM)
    ang = positions.astype(jnp.float32)[..., None] * inv_freq
    return jnp.cos(ang), jnp.sin(ang)


def apply_partial_rope(x, cos, sin):
    half = ROT_DIM // 2
    shape = cos.shape[:2] + (1,) * (x.ndim - 3) + (half,)
    c, s = cos.reshape(shape), sin.reshape(shape)
    xf = x.astype(jnp.float32)
    x1, x2 = xf[..., :half], xf[..., half:ROT_DIM]
    out = jnp.concatenate([x1 * c - x2 * s, x2 * c + x1 * s, xf[..., ROT_DIM:]], axis=-1)
    return out.astype(x.dtype)


def multiscale_pool(u, w, scale):
    B, T, _ = u.shape
    uf = u.astype(jnp.float32).reshape(B, T, POOL_GROUPS, POOL_GROUP_DIM)
    cs = jnp.cumsum(uf, axis=1)
    t_idx = jnp.arange(T)
    means = []
    for g, win in enumerate(POOL_WINDOWS):
        c = cs[:, :, g]
        lag = jnp.pad(c, ((0, 0), (win, 0), (0, 0)))[:, :T]
        count = jnp.minimum(t_idx + 1, win).astype(jnp.float32)[None, :, None]
        means.append((c - lag) / count)
    pooled = jnp.stack(means, axis=2) - uf
    y = jnp.einsum('btgc,gcd->btgd', pooled, w.astype(jnp.float32))
    y = y.reshape(B, T, POOL_WIDTH) * scale.astype(jnp.float32)
    return y.astype(u.dtype)


def hgrn2(q_raw, f_raw, i_raw, g_raw, lb, norm_g):
    B, T, _ = q_raw.shape
    H, dh, C = HGRN_HEADS, HGRN_HEAD_DIM, HGRN_CHUNK
    N = T // C
    lb = lb.astype(jnp.float32)
    z = f_raw.astype(jnp.float32)
    q = jax.nn.silu(q_raw.astype(jnp.float32))
    sig = jax.nn.sigmoid(z)
    f = lb + (1.0 - lb) * sig
    log_f = jnp.log(jnp.maximum(f, LOG_F_FLOOR))
    k = (1.0 - lb) * (1.0 - sig)
    v = i_raw.astype(jnp.float32)

    def to_chunks(a):
        return a.reshape(B, N, C, H, dh).transpose(1, 0, 3, 2, 4)

    causal = jnp.tril(jnp.ones((C, C), dtype=bool))[:, :, None]

    def step(S, xs):
        qc, kc, lfc, vc = xs
        b = jnp.cumsum(lfc, axis=2)
        diff = b[:, :, :, None, :] - b[:, :, None, :, :]
        decay = jnp.where(causal, jnp.exp(jnp.minimum(diff, 0.0)), 0.0)
        scores = jnp.einsum('bhtk,bhtsk,bhsk->bhts', qc, decay, kc)
        o = jnp.einsum('bhts,bhsv->bhtv', scores, vc) \
            + jnp.einsum('bhtk,bhkv->bhtv', qc * jnp.exp(b), S)
        b_last = b[:, :, -1:, :]
        S = jnp.exp(b_last[:, :, 0, :])[..., None] * S \
            + jnp.einsum('bhsk,bhsv->bhkv', kc * jnp.exp(b_last - b), vc)
        return S, o

    S0 = jnp.zeros((B, H, dh, dh), jnp.float32)
    _, o = lax.scan(step, S0, (to_chunks(q), to_chunks(k), to_chunks(log_f), to_chunks(v)))
    o = o.transpose(1, 0, 3, 2, 4).reshape(B, T, H, dh)
    o = o * lax.rsqrt(jnp.mean(o * o, axis=-1, keepdims=True) + RMS_EPS) \
        * norm_g.astype(jnp.float32).reshape(H, dh)
    o = o.reshape(B, T, HGRN_WIDTH) * jax.nn.silu(g_raw.astype(jnp.float32))
    return o.astype(q_raw.dtype)


def dsa_sparse_attention(q, k, v, q_idx, k_idx, w_idx):
    B, T = q.shape[:2]
    nb = T // Q_BLOCK
    top_k = min(TOPK_MAX, T // 4)
    key_pos = jnp.arange(T)
    k_idx32 = k_idx.astype(jnp.float32)
    scale = HEAD_DIM ** -0.5

    def blocks(a):
        return jnp.moveaxis(a.reshape((B, nb, Q_BLOCK) + a.shape[2:]), 1, 0)

    def one_block(args):
        qb, qib, wb, start = args
        qpos = start + jnp.arange(Q_BLOCK)
        s_idx = jnp.einsum('bqhd,bsd->bqhs', qib.astype(jnp.float32), k_idx32)
        I = jnp.einsum('bqhs,bqh->bqs', jax.nn.relu(s_idx), wb.astype(jnp.float32))
        causal = key_pos[None, :] <= qpos[:, None]
        I = jnp.where(causal[None], I, MASK_VALUE)
        _, sel = lax.top_k(I, top_k)
        valid = sel <= qpos[None, :, None]
        kg = jax.vmap(lambda kk, ii: kk[ii])(k, sel)
        vg = jax.vmap(lambda vv, ii: vv[ii])(v, sel)
        sc = jnp.einsum('bqhgd,bqkhd->bqhgk', qb, kg).astype(jnp.float32) * scale
        sc = jnp.where(valid[:, :, None, None, :], sc, MASK_VALUE)
        p = jax.nn.softmax(sc, axis=-1).astype(v.dtype)
        return jnp.einsum('bqhgk,bqkhd->bqhgd', p, vg)

    starts = jnp.arange(nb) * Q_BLOCK
    out = lax.map(one_block, (blocks(q), blocks(q_idx), blocks(w_idx), starts))
    return jnp.moveaxis(out, 0, 1).reshape(B, T, ATTN_WIDTH)


def setup_inputs(seed: int = 0) -> dict:
    key = jax.random.key(seed)
    ks = jax.random.split(key, 14)
    f32 = jnp.float32
    x = jax.random.normal(ks[0], (BATCH, SEQ, D_MODEL), f32)
    offsets = jax.random.randint(ks[1], (BATCH, 1), 0, 4096, dtype=jnp.int32)
    positions = (offsets + jnp.arange(SEQ, dtype=jnp.int32)[None, :]).astype(jnp.int32)
    norm1_g = 1.0 + 0.02 * jax.random.normal(ks[2], (DEPTH, D_MODEL), f32)
    w_in = jax.random.normal(ks[3], (DEPTH, D_MODEL, D_IN), f32) * D_MODEL ** -0.5
    pool_w = jax.random.normal(ks[4], (DEPTH, POOL_GROUPS, POOL_GROUP_DIM, POOL_GROUP_DIM), f32) * POOL_GROUP_DIM ** -0.5
    pool_scale = 1.0 + 0.02 * jax.random.normal(ks[5], (DEPTH, POOL_WIDTH), f32)
    lb_logits = 0.5 * jax.random.normal(ks[6], (DEPTH, HGRN_WIDTH), f32)
    hgrn_norm_g = 1.0 + 0.02 * jax.random.normal(ks[7], (DEPTH, HGRN_WIDTH), f32)
    w_out = jax.random.normal(ks[8], (DEPTH, D_MIX, D_MODEL), f32) * D_MIX ** -0.5
    norm2_g = 1.0 + 0.02 * jax.random.normal(ks[9], (DEPTH, D_MODEL), f32)
    w_ff_in = jax.random.normal(ks[10], (DEPTH, D_MODEL, D_FF), f32) * D_MODEL ** -0.5
    w_ff_out = jax.random.normal(ks[11], (DEPTH, D_FF, D_MODEL), f32) * D_FF ** -0.5
    final_norm_g = 1.0 + 0.02 * jax.random.normal(ks[12], (D_MODEL,), f32)
    return {"x": x, "positions": positions, "norm1_g": norm1_g, "w_in": w_in,
            "pool_w": pool_w, "pool_scale": pool_scale, "lb_logits": lb_logits,
            "hgrn_norm_g": hgrn_norm_g, "w_out": w_out, "norm2_g": norm2_g,
            "w_ff_in": w_ff_in, "w_ff_out": w_ff_out, "final_norm_g": final_norm_g}


def reference(x, positions, norm1_g, w_in, pool_w, pool_scale, lb_logits, hgrn_norm_g,
              w_out, norm2_g, w_ff_in, w_ff_out, final_norm_g):
    B, T, _ = x.shape
    cos, sin = rope_tables(positions)
    p_lb = jax.nn.softmax(lb_logits.astype(jnp.float32), axis=0)
    lower_bounds = jnp.cumsum(p_lb, axis=0) - p_lb[0]
    idx_w_scale = (IDX_HEADS ** -0.5) * (IDX_DIM ** -0.5)
    for layer in range(DEPTH):
        h = rms_norm(x, norm1_g[layer])
        proj = h @ w_in[layer]
        (pool_in, hq, hf, hi, hg, q, k, v, qi, ki, wi) = split_cols(proj, IN_SIZES)
        y_pool = multiscale_pool(pool_in, pool_w[layer], pool_scale[layer])
        y_hgrn = hgrn2(hq, hf, hi, hg, lower_bounds[layer], hgrn_norm_g[layer])
        q = apply_partial_rope(q.reshape(B, T, N_KV_HEADS, KV_GROUP, HEAD_DIM), cos, sin)
        k = apply_partial_rope(k.reshape(B, T, N_KV_HEADS, HEAD_DIM), cos, sin)
        v = v.reshape(B, T, N_KV_HEADS, HEAD_DIM)
        qi = apply_partial_rope(qi.reshape(B, T, IDX_HEADS, IDX_DIM), cos, sin)
        ki = apply_partial_rope(ki, cos, sin)
        y_attn = dsa_sparse_attention(q, k, v, qi, ki, wi * idx_w_scale)
        mixed = jnp.concatenate([y_pool.astype(x.dtype), y_hgrn.astype(x.dtype),
                                 y_attn.astype(x.dtype)], axis=-1)
        x = x + mixed @ w_out[layer]
        h = rms_norm(x, norm2_g[layer])
        x = x + jnp.square(jax.nn.relu(h @ w_ff_in[layer])) @ w_ff_out[layer]
    return rms_norm(x, final_norm_g)
```

```python
import contextlib
import numpy as np
import concourse.bass as bass
import concourse.mybir as mybir
from concourse.bass_utils import run_bass_kernel_spmd

F32 = mybir.dt.float32
BF16 = mybir.dt.bfloat16
I32 = mybir.dt.int32
ALU = mybir.AluOpType
AF = mybir.ActivationFunctionType
AX = mybir.AxisListType

PE, ACT, DVE, POOL, SP = "tensor", "scalar", "vector", "gpsimd", "sync"
ENGS = (PE, ACT, DVE, POOL, SP)

D = 1024
DIN = 2372
DFF = 4096
TOPK_MAX = 256
EPS = 1e-5
BIG = 30000.0
NBIS = 16


class Res:
    __slots__ = ("name", "last_w", "readers")

    def __init__(self, name):
        self.name = name
        self.last_w = None
        self.readers = []


class Op:
    __slots__ = ("eng", "fn", "deps", "is_dma", "sem_key", "sem_val", "seq", "needed")

    def __init__(self, eng, fn, is_dma=False, sem_key=None):
        self.eng = eng
        self.fn = fn
        self.deps = []
        self.is_dma = is_dma
        self.sem_key = sem_key
        self.sem_val = 0
        self.seq = 0
        self.needed = False


class _Rec:
    def __init__(self):
        self.call = None

    def __getattr__(self, name):
        def f(*a, **k):
            self.call = (name, a, k)
            return self
        return f


class Sched:
    def __init__(self, nc):
        self.nc = nc
        self.ops = []
        self.dma_counts = {}
        self.last_on = {}
        self.barrier_deps = []
        self.after_barrier = set()
        self.max_stage = 99
        self.cur_stage = 0

    def stage(self, k):
        self.cur_stage = k

    def _dep(self, op, d):
        if d is None or d is op:
            return
        if d.eng == PE and op.eng == PE and not d.is_dma and not op.is_dma:
            return
        op.deps.append(d)
        d.needed = True

    def add(self, eng, fn, reads=(), writes=(), is_dma=False, sem_key=None, group=False):
        if self.cur_stage > self.max_stage:
            return None
        rec = _Rec()
        fn(rec)
        op = Op(eng, rec.call, is_dma, sem_key)
        if is_dma:
            c = self.dma_counts.get(sem_key, 0) + 16
            self.dma_counts[sem_key] = c
            op.sem_val = c
        if self.barrier_deps and eng not in self.after_barrier:
            self.after_barrier.add(eng)
            for d in self.barrier_deps:
                self._dep(op, d)
        for r in reads:
            self._dep(op, r.last_w)
        for w in writes:
            lw = w.last_w
            if not (group and lw is not None and lw.is_dma and lw.sem_key == sem_key and lw.eng == eng):
                self._dep(op, lw)
            for rd in w.readers:
                self._dep(op, rd)
        for r in reads:
            r.readers.append(op)
            if len(r.readers) > 24:
                seen = {}
                for o in r.readers:
                    seen[(o.eng, o.is_dma, o.sem_key)] = o
                r.readers = list(seen.values())
        for w in writes:
            w.last_w = op
            w.readers = []
        self.ops.append(op)
        self.last_on[(eng, is_dma, sem_key if is_dma else None)] = op
        return op

    def barrier(self):
        self.barrier_deps = list(self.last_on.values())
        self.after_barrier = set()

    def emit(self, final_waits=()):
        nc = self.nc
        cnt = {e: 0 for e in ENGS}
        for op in self.ops:
            if not op.is_dma and op.needed:
                cnt[op.eng] += 1
                op.seq = cnt[op.eng]
        with contextlib.ExitStack() as st:
            esem = {e: st.enter_context(nc.semaphore("s_" + e)) for e in ENGS}
            dsem = {k: st.enter_context(nc.semaphore("d_%s" % (k,))) for k in self.dma_counts}
            block = st.enter_context(nc.Block())
            per_eng = {e: [o for o in self.ops if o.eng == e] for e in ENGS}

            def run(engname, eng):
                waited = {}
                for op in per_eng[engname]:
                    need = {}
                    for d in op.deps:
                        if d.is_dma:
                            key, val = ("d", d.sem_key), d.sem_val
                        else:
                            key, val = ("e", d.eng), d.seq
                        if need.get(key, 0) < val:
                            need[key] = val
                    todo = []
                    for key, val in need.items():
                        if waited.get(key, 0) >= val:
                            continue
                        waited[key] = val
                        todo.append((dsem[key[1]] if key[0] == "d" else esem[key[1]], val))
                    name_, a_, k_ = op.fn
                    single = (not op.is_dma) and k_.get("accum_out", None) is None
                    fused = todo.pop() if (single and todo) else None
                    for sem, val in todo:
                        eng.wait_ge(sem, val)
                    ins = getattr(eng, name_)(*a_, **k_)
                    if fused is not None:
                        ins._wait_ge(fused[0], fused[1])
                    if op.is_dma:
                        ins.then_inc(dsem[op.sem_key], 16)
                    elif op.needed:
                        ins.then_inc(esem[op.eng], 1)
                if engname == SP:
                    for k in final_waits:
                        if k in dsem:
                            eng.wait_ge(dsem[k], self.dma_counts[k])

            @block.tensor
            def _(e):
                run(PE, e)

            @block.scalar
            def _(e):
                run(ACT, e)

            @block.vector
            def _(e):
                run(DVE, e)

            @block.gpsimd
            def _(e):
                run(POOL, e)

            @block.sync
            def _(e):
                run(SP, e)


class Arena:
    def __init__(self, ap, nwords):
        self.ap = ap
        self.n = nwords
        self.off = 0

    def reset(self, off=0):
        self.off = off

    def take(self, shape, dt):
        n = 1
        for s in shape:
            n *= s
        words = n if dt == F32 or dt == I32 else (n + 1) // 2
        words = (words + 1) // 2 * 2
        assert self.off + words <= self.n, ("arena overflow", self.off, words, self.n)
        v = self.ap[:, self.off:self.off + words]
        self.off += words
        if dt != F32:
            v = v.bitcast(dt)
        v = v[:, 0:n]
        if len(shape) == 2:
            v = v.rearrange("p (a b) -> p a b", b=shape[1])
        elif len(shape) == 3:
            v = v.rearrange("p (a b c) -> p a b c", b=shape[1], c=shape[2])
        elif len(shape) == 4:
            v = v.rearrange("p (a b c d) -> p a b c d", b=shape[1], c=shape[2], d=shape[3])
        return v


def build_program(NT=16, L=4, taps=(), max_stage=99, l0=0, l1=None, final=True):
    T = NT * 128
    TOPK = min(TOPK_MAX, T // 4)
    if l1 is None:
        l1 = L
    nc = bass.Bass("TRN2", target_bir_lowering=False)
    dr = {}

    def din(name, shape, dt=F32):
        dr[name] = nc.dram_tensor(name, shape, dt, kind="ExternalInput").ap()
        return dr[name]

    x_d = din("x", [T, D])
    pos_d = din("positions", [NT, 128], I32)
    n1_d = din("norm1_g", [L, D])
    win_d = din("w_in", [L, D, DIN])
    pw_d = din("pool_w", [L, 4, 64, 64])
    psc_d = din("pool_scale", [L, 256])
    lbl_d = din("lb_logits", [L, 256])
    hng_d = din("hgrn_norm_g", [L, 256])
    wout_d = din("w_out", [L, D, D])
    n2_d = din("norm2_g", [L, D])
    w1_d = din("w_ff_in", [L, D, DFF])
    w2_d = din("w_ff_out", [L, DFF, D])
    fg_d = din("final_norm_g", [D])
    out_d = nc.dram_tensor("out", [T, D], F32, kind="ExternalOutput").ap()
    tap_d = {}
    for (tname, tshape) in taps:
        tap_d[tname] = nc.dram_tensor("tap_" + tname, tshape, F32, kind="ExternalOutput").ap()

    S = Sched(nc)
    S.max_stage = max_stage
    with contextlib.ExitStack() as st:
        def sb(name, shape, dt=F32):
            return st.enter_context(nc.sbuf_tensor(name, shape, dt))

        x_sb = sb("x_sb", [128, NT, D])
        xR = [Res("x%d" % i) for i in range(NT)]
        gB = sb("gB", [128, D])
        gBR = Res("gB")
        ident_f = sb("ident_f", [128, 128])
        ident_b = sb("ident_b", [128, 128], BF16)
        identBIG4 = sb("identBIG4", [128, 4, 128], BF16)
        ltri = sb("ltri", [128, 128])
        lrem = sb("lrem", [128, 128])
        cmask = sb("cmask", [128, 128])
        chunkind = sb("chunkind", [128, 2])
        pow2tab = sb("pow2tab", [128, NBIS])
        corr = sb("corr", [128, 2, 16])
        cs_sb = sb("cs_sb", [128, NT, 8])
        sn_sb = sb("sn_sb", [128, NT, 8])
        poolW = sb("poolW", [128, L, 2, 128], BF16)
        pscol = sb("pscol", [128, L, 2])
        hngcol = sb("hngcol", [128, L, 2])
        pB = sb("pB", [128, L, 256])
        lbB = sb("lbB", [128, 256])
        omlbB = sb("omlbB", [128, 256])
        cR = Res("consts")
        pwR = Res("poolW"); colR = Res("cols"); pBR = Res("pB"); ropeR = Res("rope")
        lbR = Res("lb")

        arenaA_t = sb("arenaA", [128, 9728])
        AA = Arena(arenaA_t, 9728)

        def psb(name, shape):
            return st.enter_context(nc.psum_tensor(name, shape, F32))
        B0 = psb("B0", [128, 512]); B1 = psb("B1", [128, 512])
        B23 = psb("B23", [128, 1024])
        B4 = psb("B4", [128, 512]); B5 = psb("B5", [128, 512])
        B6 = psb("B6", [128, 512]); B7 = psb("B7", [128, 512])
        B0, B1, B23, B4, B5, B6, B7 = [t_[:, :] for t_ in (B0, B1, B23, B4, B5, B6, B7)]
        B2 = B23[:, 0:512]; B3 = B23[:, 512:1024]
        R0, R1, R23, R4, R5, R6, R7 = [Res("B%d" % i) for i in (0, 1, 23, 4, 5, 6, 7)]

        def bf(ap_f32):
            return ap_f32.bitcast(BF16)

        nB = int(nc.sbuf_bytes_remaining) // 4 - 16
        arenaB_t = sb("arenaB", [128, nB])
        AB = Arena(arenaB_t, nB)

        def dve(fn, r=(), w=()): return S.add(DVE, fn, r, w)
        def act(fn, r=(), w=()): return S.add(ACT, fn, r, w)
        def pool(fn, r=(), w=()): return S.add(POOL, fn, r, w)
        def pe(fn, r=(), w=()): return S.add(PE, fn, r, w)
        def dma(q, fn, r=(), w=(), key=None, group=True): return S.add(q, fn, r, w, is_dma=True, sem_key=key, group=group)

        def tap(name, ap, res):
            if name in tap_d:
                dma(POOL, lambda e: e.dma_start(out=tap_d[name], in_=ap), r=res, w=(), key="tap")

        pool(lambda e: e.memset(ident_f[:], 0.0), w=[cR])
        pool(lambda e: e.affine_select(out=ident_f[:], in_=ident_f[:], pattern=[[-1, 128]], compare_op=ALU.not_equal,
                                       fill=1.0, base=0, channel_multiplier=1), r=[cR], w=[cR])
        pool(lambda e: e.tensor_copy(out=ident_b[:], in_=ident_f[:]), r=[cR], w=[cR])
        for h4 in range(4):
            pool(lambda e, h4=h4: e.tensor_scalar(out=identBIG4[:, h4, :], in0=ident_f[:], scalar1=BIG, scalar2=None,
                                                   op0=ALU.mult), r=[cR], w=[cR])
        pool(lambda e: e.memset(ltri[:], 1.0), w=[cR])
        pool(lambda e: e.affine_select(out=ltri[:], in_=ltri[:], pattern=[[1, 128]], compare_op=ALU.is_ge, fill=0.0,
                                       base=0, channel_multiplier=-1), r=[cR], w=[cR])
        pool(lambda e: e.memset(ltri[0:64, 64:128], 0.0), r=[cR], w=[cR])
        pool(lambda e: e.memset(lrem[:], 1.0), w=[cR])
        pool(lambda e: e.affine_select(out=lrem[:], in_=lrem[:], pattern=[[-1, 128]], compare_op=ALU.is_gt, fill=0.0,
                                       base=0, channel_multiplier=1), r=[cR], w=[cR])
        pool(lambda e: e.memset(lrem[64:128, 0:64], 0.0), r=[cR], w=[cR])
        pool(lambda e: e.memset(cmask[:], 0.0), w=[cR])
        pool(lambda e: e.affine_select(out=cmask[:], in_=cmask[:], pattern=[[-1, 128]], compare_op=ALU.is_ge, fill=-1e30,
                                       base=0, channel_multiplier=1), r=[cR], w=[cR])
        pool(lambda e: e.memset(chunkind[:], 0.0), w=[cR])
        pool(lambda e: e.memset(chunkind[0:64, 0:1], 1.0), r=[cR], w=[cR])
        pool(lambda e: e.memset(chunkind[64:128, 1:2], 1.0), r=[cR], w=[cR])
        for k in range(NBIS):
            pool(lambda e, k=k: e.memset(pow2tab[:, k:k + 1], 2.0 ** (-k)), w=[cR])
        pool(lambda e: e.memset(corr[:], 1.0), w=[cR])
        for g, win in enumerate((2, 4, 8, 16)):
            ct, ph = g // 2, (g % 2) * 64
            for t in range(win - 1):
                pool(lambda e, ct=ct, ph=ph, t=t, win=win: e.memset(corr[ph:ph + 64, ct, t:t + 1], float(win) / (t + 1)),
                     r=[cR], w=[cR])

        pool(lambda e: e.memset(poolW[:], 0.0), w=[pwR])
        for l in range(L):
            for g in range(4):
                ct, ph = g // 2, (g % 2) * 64
                dma(POOL, lambda e, l=l, g=g, ct=ct, ph=ph: e.dma_start(out=poolW[ph:ph + 64, l, ct, ph:ph + 64], in_=pw_d[l, g]),
                    w=[pwR], key="c_pw")
        dma(SP, lambda e: e.dma_start(out=pscol[:], in_=psc_d.rearrange("l (c p) -> p l c", p=128), allow_slow_non_contiguous=True), w=[colR], key="c_col")
        dma(SP, lambda e: e.dma_start(out=hngcol[:], in_=hng_d.rearrange("l (c p) -> p l c", p=128), allow_slow_non_contiguous=True), w=[colR], key="c_col")
        dma(SP, lambda e: e.dma_start(out=pB[:], in_=lbl_d.rearrange("(o l) c -> o l c", o=1).to_broadcast([128, L, 256])), w=[pBR], key="c_pB")
        pos_i = AB.take([NT], I32)
        pos_f = AB.take([NT], F32)
        ang = AB.take([NT, 8], F32)
        angr = AB.take([NT, 8], F32)
        dma(SP, lambda e: e.dma_start(out=pos_i[:], in_=pos_d.rearrange("i p -> p i"), allow_slow_non_contiguous=True), w=[ropeR], key="c_pos")
        dve(lambda e: e.tensor_copy(out=pos_f[:], in_=pos_i[:]), r=[ropeR], w=[ropeR])
        for j in range(8):
            inv = float(np.float32(500000.0) ** np.float32(-(2.0 * j) / 16.0))
            dve(lambda e, j=j, inv=inv: e.tensor_scalar(out=ang[:, :, j], in0=pos_f[:], scalar1=inv, scalar2=None, op0=ALU.mult),
                r=[ropeR], w=[ropeR])
        TWO_PI = 2.0 * np.pi
        kf = AB.take([NT, 8], F32)
        ki_ = AB.take([NT, 8], I32)
        tt = AB.take([NT, 8], F32)
        for (dst, shift) in ((sn_sb, 0.0), (cs_sb, 0.5 * np.pi)):
            dve(lambda e, shift=shift: e.tensor_scalar(out=angr[:], in0=ang[:], scalar1=float(shift), scalar2=None, op0=ALU.add), r=[ropeR], w=[ropeR])
            dve(lambda e: e.tensor_scalar(out=kf[:], in0=angr[:], scalar1=float(1.0 / TWO_PI), scalar2=None, op0=ALU.mult), r=[ropeR], w=[ropeR])
            dve(lambda e: e.tensor_copy(out=ki_[:], in_=kf[:]), r=[ropeR], w=[ropeR])
            dve(lambda e: e.tensor_copy(out=kf[:], in_=ki_[:]), r=[ropeR], w=[ropeR])
            dve(lambda e: e.scalar_tensor_tensor(out=angr[:], in0=kf[:], scalar=float(-TWO_PI), in1=angr[:], op0=ALU.mult, op1=ALU.add), r=[ropeR], w=[ropeR])
            dve(lambda e: e.tensor_scalar(out=tt[:], in0=angr[:], scalar1=float(np.pi), scalar2=float(-TWO_PI), op0=ALU.is_gt, op1=ALU.mult), r=[ropeR], w=[ropeR])
            dve(lambda e: e.tensor_tensor(out=angr[:], in0=angr[:], in1=tt[:], op=ALU.add), r=[ropeR], w=[ropeR])
            dve(lambda e: e.tensor_scalar(out=tt[:], in0=angr[:], scalar1=float(-np.pi), scalar2=float(TWO_PI), op0=ALU.is_lt, op1=ALU.mult), r=[ropeR], w=[ropeR])
            dve(lambda e: e.tensor_tensor(out=angr[:], in0=angr[:], in1=tt[:], op=ALU.add), r=[ropeR], w=[ropeR])
            dve(lambda e: e.tensor_scalar(out=angr[:], in0=angr[:], scalar1=3.141592, scalar2=-3.141592, op0=ALU.min, op1=ALU.max), r=[ropeR], w=[ropeR])
            act(lambda e, dst=dst: e.activation(out=dst[:], in_=angr[:], func=AF.Sin), r=[ropeR], w=[ropeR])
        mx = AB.take([256], F32)
        zs = AB.take([256], F32)
        dve(lambda e: e.tensor_copy(out=mx[:], in_=pB[:, 0, :]), r=[pBR], w=[pBR])
        for l in range(1, L):
            dve(lambda e, l=l: e.tensor_tensor(out=mx[:], in0=mx[:], in1=pB[:, l, :], op=ALU.max), r=[pBR], w=[pBR])
        for l in range(L):
            dve(lambda e, l=l: e.tensor_tensor(out=pB[:, l, :], in0=pB[:, l, :], in1=mx[:], op=ALU.subtract), r=[pBR], w=[pBR])
        act(lambda e: e.activation(out=pB[:], in_=pB[:], func=AF.Exp), r=[pBR], w=[pBR])
        dve(lambda e: e.tensor_copy(out=zs[:], in_=pB[:, 0, :]), r=[pBR], w=[pBR])
        for l in range(1, L):
            dve(lambda e, l=l: e.tensor_tensor(out=zs[:], in0=zs[:], in1=pB[:, l, :], op=ALU.add), r=[pBR], w=[pBR])
        dve(lambda e: e.reciprocal(out=zs[:], in_=zs[:]), r=[pBR], w=[pBR])
        for l in range(L):
            dve(lambda e, l=l: e.tensor_tensor(out=pB[:, l, :], in0=pB[:, l, :], in1=zs[:], op=ALU.mult), r=[pBR], w=[pBR])

        for i in range(NT):
            q = SP if i % 2 == 0 else ACT
            dma(q, lambda e, i=i: e.dma_start(out=x_sb[:, i, :], in_=x_d[i * 128:(i + 1) * 128, :]), w=[xR[i]], key="x%d" % i)

        def rmsnorm_to_hT(i, hT_dst, hT_res, bank, bank_res, tmp):
            st_, hn, stR, hnR = tmp
            act(lambda e: e.activation(out=hn[:], in_=x_sb[:, i, :], func=AF.Square, accum_out=st_[:, 0:1]),
                r=[xR[i]], w=[stR, hnR])
            act(lambda e: e.activation(out=st_[:, 1:2], in_=st_[:, 0:1], func=AF.Ln, scale=1.0 / D, bias=EPS), r=[stR], w=[stR])
            act(lambda e: e.activation(out=st_[:, 2:3], in_=st_[:, 1:2], func=AF.Exp, scale=-0.5), r=[stR], w=[stR])
            dve(lambda e: e.scalar_tensor_tensor(out=hn[:], in0=x_sb[:, i, :], scalar=st_[:, 2:3], in1=gB[:],
                                                 op0=ALU.mult, op1=ALU.mult), r=[xR[i], stR, gBR], w=[hnR])
            pT = bf(bank).rearrange("p (k t) -> p k t", t=128)
            for k in range(8):
                pe(lambda e, k=k: e.transpose(out=pT[:, k, :], in_=hn[:, k * 128:(k + 1) * 128], identity=ident_b[:]),
                   r=[hnR, cR], w=[bank_res])
            act(lambda e: e.activation(out=hT_dst, in_=pT[:, 0:8, :], func=AF.Copy), r=[bank_res], w=[hT_res])

        for l in range(l0):
            if l == 0:
                dve(lambda e: e.memset(lbB[:], 0.0), w=[lbR])
            else:
                dve(lambda e, l=l: e.tensor_tensor(out=lbB[:], in0=lbB[:], in1=pB[:, l, :], op=ALU.add), r=[pBR, lbR], w=[lbR])
        for l in range(l0, l1):
            S.stage(0)
            S.barrier()
            AA.reset(); AB.reset()
            w_in = AA.take([8, DIN], BF16)
            winR = Res("w_in")
            w_out = AB.take([8, D], BF16)
            woutR = Res("w_out")
            wsrc = win_d[l].rearrange("(c p) n -> p c n", p=128)
            for k in range(8):
                dma(POOL, lambda e, k=k: e.dma_start(out=w_in[:, k, 0:1920], in_=wsrc[:, k, 0:1920]), w=[winR], key="win")
            dma(POOL, lambda e: e.dma_start(out=w_in[:, :, 1920:2240], in_=wsrc[:, :, 2048:2368]), w=[winR], key="win")
            dma(POOL, lambda e: e.dma_start(out=w_in[:, :, 2240:2368], in_=wsrc[:, :, 1920:2048]), w=[winR], key="win")
            dma(POOL, lambda e: e.dma_start(out=w_in[:, :, 2368:2372], in_=wsrc[:, :, 2368:2372]), w=[winR], key="win")
            wosrc = wout_d[l].rearrange("(c p) n -> p c n", p=128)
            for k in range(8):
                dma(POOL, lambda e, k=k: e.dma_start(out=w_out[:, k, :], in_=wosrc[:, k, :]), w=[woutR], key="wout")
            dma(SP, lambda e: e.dma_start(out=gB[:], in_=n1_d[l:l + 1, :].to_broadcast([128, D])), w=[gBR], key="g")
            if l == 0:
                dve(lambda e: e.memset(lbB[:], 0.0), w=[lbR])
            else:
                dve(lambda e, l=l: e.tensor_tensor(out=lbB[:], in0=lbB[:], in1=pB[:, l, :], op=ALU.add), r=[pBR, lbR], w=[lbR])
            dve(lambda e: e.tensor_scalar(out=omlbB[:], in0=lbB[:], scalar1=-1.0, scalar2=1.0, op0=ALU.mult, op1=ALU.add),
                r=[lbR], w=[lbR])

            KT = AB.take([T], BF16); KTR = Res("KT")
            Vs = AB.take([NT, 2, 65], BF16); VR = Res("V")
            kiT = AB.take([T // 2], F32); kiTR = Res("kiT")
            I_sb = AB.take([T], F32); IR = Res("I")
            Mb = AB.take([T], BF16); MbR = Res("Mb")
            Rh = [AB.take([512], F32) for _ in range(2)]; RhR = [Res("Rh0"), Res("Rh1")]
            PT = [AB.take([512], BF16) for _ in range(3)]; PTR = [Res("PT%d" % j) for j in range(3)]
            st_ = AB.take([8], F32); stR = Res("st")
            hn = AB.take([D], BF16); hnR = Res("hn")
            hT = AB.take([8, 128], BF16); hTR = Res("hT")
            mixT = AB.take([8, 128], BF16); mixR = Res("mixT")
            uext = [AB.take([2, 144], F32) for _ in range(2)]; uR = [Res("u0"), Res("u1")]
            slv = [AB.take([2, 144], F32) for _ in range(4)]; sR = Res("slv")
            pooled = AB.take([2, 128], BF16); pooledR = Res("pooled")
            H = {n: AB.take([256], F32) for n in ("t0", "t1", "f", "kk", "eb", "enb", "ebl", "gate")}
            HR = {n: Res("h_" + n) for n in H}
            H["logf"] = H["f"]; HR["logf"] = HR["f"]
            H["q"] = H["t1"]; HR["q"] = HR["t1"]
            qt_b = AB.take([256], BF16); kt_b = AB.take([256], BF16)
            khA = AB.take([256], BF16); khB = AB.take([256], BF16); v_b = AB.take([256], BF16)
            hbR = Res("h_bf"); qtR = Res("h_qt"); ktR = Res("h_kt"); vbR = Res("h_v")
            qTf = AB.take([4, 128], BF16); qTA = AB.take([4, 128], BF16); qTB = AB.take([4, 128], BF16); kTs = AB.take([4, 128], BF16)
            hTR2 = Res("h_T")
            Am = AB.take([4, 128], BF16); AmR = Res("Am")
            dec = AB.take([8], F32); decR = Res("dec")
            S32 = AB.take([4, 64], F32); Sbf = [AB.take([4, 64], BF16) for _ in range(2)]; Stmp = AB.take([4, 64], F32)
            SR = Res("S")
            hst = AB.take([16], F32)
            osb = H["enb"]; osq = H["t0"]; og = H["eb"]
            oR = Res("o")
            qk_b = AB.take([10, 64], BF16); qiki = AB.take([5, 64], F32)
            qk_flat = qk_b.rearrange("p h d -> p (h d)")
            rt = [AB.take([15, 8], F32) for _ in range(4)]
            ws = AB.take([4], F32)
            aR = Res("attn_tm")
            qidup = AB.take([4, 128], F32); kidup = AB.take([128], F32)
            qT_sb = AB.take([4, 128], BF16); qiT_sb = AB.take([4, 128], F32)
            aTR = Res("attn_T")
            bis = AB.take([32], F32); bisR = Res("bis")
            stepcol = AB.take([NBIS], F32)
            rden = AB.take([8], F32); ya = hn[:, 0:512]; yaR = hnR
            pool(lambda e: e.memset(uext[0][:, :, 0:16], 0.0), w=[uR[0]])
            pool(lambda e: e.memset(S32[0:64], 0.0), w=[SR])
            pool(lambda e: e.memset(Sbf[1][0:64], 0.0), w=[SR])
            pool(lambda e: e.memset(qTA[0:64], 0.0), w=[hTR2])
            pool(lambda e: e.memset(qTB[0:64], 0.0), w=[hTR2])
            pool(lambda e: e.memset(khA[64:128, :], 0.0), w=[hbR])
            pool(lambda e: e.memset(khB[0:64, :], 0.0), w=[hbR])
            pool(lambda e: e.memset(Vs[:, :, :, 64:65], 1.0), w=[VR])

            for i in range(NT):
                T0 = i * 128
                S.stage(1)
                rmsnorm_to_hT(i, hT[:], hTR, B0, R0, (st_, hn, stR, hnR))

                S.stage(1.5)
                pu = B1[:, 0:256].rearrange("p (c t) -> p c t", t=128)
                for ct in range(2):
                    for k in range(8):
                        pe(lambda e, ct=ct, k=k: e.matmul(pu[:, ct, :], lhsT=w_in[:, k, ct * 128:(ct + 1) * 128], rhs=hT[:, k, :],
                                                           start=(k == 0), stop=(k == 7)), r=[winR, hTR], w=[R1])
                ue = uext[i % 2]; un = uext[(i + 1) % 2]
                act(lambda e, ue=ue: e.activation(out=ue[:, :, 16:144], in_=pu, func=AF.Copy), r=[R1], w=[uR[i % 2]])
                pool(lambda e, ue=ue, un=un: e.tensor_copy(out=un[:, :, 0:16], in_=ue[:, :, 128:144]), r=[uR[i % 2]], w=[uR[(i + 1) % 2]])
                prev = ue
                S.stage(1.6)
                for lv, sh in enumerate((1, 2, 4, 8)):
                    lo = 2 * sh - 1
                    pool(lambda e, lv=lv, sh=sh, lo=lo, prev=prev: e.tensor_tensor(out=slv[lv][:, :, lo:144], in0=prev[:, :, lo:144],
                                                                                   in1=prev[:, :, lo - sh:144 - sh], op=ALU.add),
                         r=[uR[i % 2], sR], w=[sR])
                    prev = slv[lv]
                S.stage(1.7)
                for g, win in enumerate((2, 4, 8, 16)):
                    ct, ph = g // 2, (g % 2) * 64
                    if i == 0:
                        pool(lambda e, g=g, ct=ct, ph=ph: e.tensor_tensor(out=slv[g][ph:ph + 64, ct, 16:32], in0=slv[g][ph:ph + 64, ct, 16:32],
                                                                          in1=corr[ph:ph + 64, ct, :], op=ALU.mult), r=[sR, cR], w=[sR])
                    dve(lambda e, g=g, ct=ct, ph=ph, win=win, ue=ue: e.scalar_tensor_tensor(
                        out=pooled[ph:ph + 64, ct, :], in0=slv[g][ph:ph + 64, ct, 16:144], scalar=1.0 / win, in1=ue[ph:ph + 64, ct, 16:144],
                        op0=ALU.mult, op1=ALU.subtract), r=[sR, uR[i % 2]], w=[pooledR])
                S.stage(1.8)
                py = B1[:, 256:512].rearrange("p (c t) -> p c t", t=128)
                for ct in range(2):
                    pe(lambda e, ct=ct: e.matmul(py[:, ct, :], lhsT=poolW[:, l, ct, :], rhs=pooled[:, ct, :], start=True, stop=True),
                       r=[pwR, pooledR], w=[R1])
                S.stage(1.9)
                for ct in range(2):
                    act(lambda e, ct=ct: e.activation(out=mixT[:, ct, :], in_=py[:, ct, :], func=AF.Identity, scale=pscol[:, l, ct:ct + 1]),
                        r=[R1, colR], w=[mixR])

                S.stage(2)
                for j, bank in enumerate((B2, B3)):
                    for k in range(8):
                        pe(lambda e, j=j, k=k, bank=bank: e.matmul(bank, lhsT=hT[:, k, :], rhs=w_in[:, k, 256 + j * 512:256 + (j + 1) * 512],
                                                                   start=(k == 0), stop=(k == 7)), r=[hTR, winR], w=[R23])
                hq_p, hf_p, hi_p, hg_p = B2[:, 0:256], B2[:, 256:512], B3[:, 0:256], B3[:, 256:512]
                act(lambda e: e.activation(out=H["t0"], in_=hf_p, func=AF.Exp, scale=-1.0), r=[R23], w=[HR["t0"]])
                act(lambda e: e.activation(out=H["t1"], in_=hq_p, func=AF.Exp, scale=-1.0), r=[R23], w=[HR["t1"]])
                act(lambda e: e.activation(out=H["gate"], in_=hg_p, func=AF.Exp, scale=-1.0), r=[R23], w=[HR["gate"]])
                act(lambda e: e.activation(out=v_b, in_=hi_p, func=AF.Copy), r=[R23], w=[vbR])
                pool(lambda e: e.tensor_scalar(out=H["t0"], in0=H["t0"], scalar1=1.0, scalar2=None, op0=ALU.add), r=[HR["t0"]], w=[HR["t0"]])
                dve(lambda e: e.reciprocal(out=H["t0"], in_=H["t0"]), r=[HR["t0"]], w=[HR["t0"]])
                pool(lambda e: e.tensor_tensor(out=H["f"], in0=H["t0"], in1=omlbB[:], op=ALU.mult), r=[HR["t0"], lbR], w=[HR["f"]])
                pool(lambda e: e.tensor_tensor(out=H["f"], in0=H["f"], in1=lbB[:], op=ALU.add), r=[HR["f"], lbR], w=[HR["f"]])
                pool(lambda e: e.tensor_scalar(out=H["kk"], in0=H["f"], scalar1=-1.0, scalar2=1.0, op0=ALU.mult, op1=ALU.add),
                     r=[HR["f"]], w=[HR["kk"]])
                pool(lambda e: e.tensor_scalar(out=H["f"], in0=H["f"], scalar1=1e-30, scalar2=None, op0=ALU.max), r=[HR["f"], HR["kk"]], w=[HR["f"]])
                act(lambda e: e.activation(out=H["logf"], in_=H["f"], func=AF.Ln), r=[HR["f"]], w=[HR["logf"]])
                pe(lambda e: e.matmul(B4[:, 0:256], lhsT=ltri[:], rhs=H["logf"], start=True, stop=True), r=[cR, HR["logf"]], w=[R4])
                pe(lambda e: e.matmul(B4[:, 256:512], lhsT=lrem[:], rhs=H["logf"], start=True, stop=True), r=[cR, HR["logf"]], w=[R4])
                dl = B0[0:64, 0:8].rearrange("p (h c) -> p h c", c=2)
                for h in range(4):
                    pe(lambda e, h=h: e.matmul(dl[:, h, :], lhsT=H["logf"][:, h * 64:(h + 1) * 64], rhs=chunkind[:], start=True, stop=True),
                       r=[cR, HR["logf"]], w=[R0])
                act(lambda e: e.activation(out=H["eb"], in_=B4[:, 0:256], func=AF.Exp), r=[R4], w=[HR["eb"]])
                act(lambda e: e.activation(out=H["enb"], in_=B4[:, 0:256], func=AF.Exp, scale=-1.0), r=[R4], w=[HR["enb"]])
                act(lambda e: e.activation(out=H["ebl"], in_=B4[:, 256:512], func=AF.Exp), r=[R4], w=[HR["ebl"]])
                act(lambda e: e.activation(out=dec[0:64].rearrange("p (h c) -> p h c", c=2), in_=dl, func=AF.Exp), r=[R0], w=[decR])
                pool(lambda e: e.tensor_scalar(out=H["t1"], in0=H["t1"], scalar1=1.0, scalar2=None, op0=ALU.add), r=[HR["t1"]], w=[HR["t1"]])
                dve(lambda e: e.reciprocal(out=H["t1"], in_=H["t1"]), r=[HR["t1"]], w=[HR["t1"]])
                dve(lambda e: e.tensor_tensor(out=H["q"], in0=hq_p, in1=H["t1"], op=ALU.mult), r=[R23, HR["t1"]], w=[HR["q"]])
                pool(lambda e: e.tensor_scalar(out=H["gate"], in0=H["gate"], scalar1=1.0, scalar2=None, op0=ALU.add), r=[HR["gate"]], w=[HR["gate"]])
                dve(lambda e: e.reciprocal(out=H["gate"], in_=H["gate"]), r=[HR["gate"]], w=[HR["gate"]])
                dve(lambda e: e.tensor_tensor(out=H["gate"], in0=hg_p, in1=H["gate"], op=ALU.mult), r=[R23, HR["gate"]], w=[HR["gate"]])
                pool(lambda e: e.tensor_tensor(out=qt_b, in0=H["q"], in1=H["eb"], op=ALU.mult), r=[HR["q"], HR["eb"]], w=[qtR])
                pool(lambda e: e.tensor_tensor(out=kt_b, in0=H["kk"], in1=H["enb"], op=ALU.mult), r=[HR["kk"], HR["enb"]], w=[ktR])
                pool(lambda e: e.tensor_tensor(out=khA[0:64, :], in0=H["kk"][0:64, :], in1=H["ebl"][0:64, :], op=ALU.mult),
                     r=[HR["kk"], HR["ebl"]], w=[hbR])
                pool(lambda e: e.tensor_tensor(out=khB[64:128, :], in0=H["kk"][64:128, :], in1=H["ebl"][64:128, :], op=ALU.mult),
                     r=[HR["kk"], HR["ebl"]], w=[hbR])
                tq = bf(B5)[0:64, 0:512].rearrange("p (h t) -> p h t", t=128)
                tk = bf(B5)[0:64, 512:1024].rearrange("p (h t) -> p h t", t=128)
                for h in range(4):
                    pe(lambda e, h=h: e.transpose(out=tq[:, h, :], in_=qt_b[:, h * 64:(h + 1) * 64], identity=ident_b[:]), r=[qtR, cR], w=[R5])
                for h in range(4):
                    pe(lambda e, h=h: e.transpose(out=tk[:, h, :], in_=kt_b[:, h * 64:(h + 1) * 64], identity=ident_b[:]), r=[ktR, cR], w=[R5])
                act(lambda e: e.activation(out=qTf[0:64], in_=tq, func=AF.Copy), r=[R5], w=[hTR2])
                dve(lambda e: e.tensor_copy(out=qTA[0:64, :, 0:64], in_=tq[:, :, 0:64]), r=[R5], w=[hTR2])
                dve(lambda e: e.tensor_copy(out=qTB[0:64, :, 64:128], in_=tq[:, :, 64:128]), r=[R5], w=[hTR2])
                act(lambda e: e.activation(out=kTs[0:64], in_=tk, func=AF.Copy), r=[R5], w=[hTR2])
                pA = B6.rearrange("p (h t) -> p h t", t=128)
                for h in range(4):
                    pe(lambda e, h=h: e.matmul(pA[:, h, :], lhsT=kTs[0:64, h, :], rhs=qTf[0:64, h, :], start=True, stop=True), r=[hTR2], w=[R6])
                dve(lambda e: e.tensor_tensor(out=Am, in0=pA, in1=ltri[:].unsqueeze(1).to_broadcast([128, 4, 128]), op=ALU.mult),
                    r=[R6, cR], w=[AmR])
                pKV = B7[0:64, :].rearrange("p (c h v) -> p c h v", c=2, h=4)
                for c, kh in enumerate((khA, khB)):
                    for h in range(4):
                        pe(lambda e, c=c, h=h, kh=kh: e.matmul(pKV[:, c, h, :], lhsT=kh[:, h * 64:(h + 1) * 64], rhs=v_b[:, h * 64:(h + 1) * 64],
                                                               start=True, stop=True), r=[hbR, vbR], w=[R7])
                decv = dec[0:64].rearrange("p (h c) -> p h c", c=2)
                for c in range(2):
                    dve(lambda e, c=c: e.tensor_tensor(out=Stmp[0:64], in0=S32[0:64], in1=decv[:, :, c:c + 1].to_broadcast([64, 4, 64]), op=ALU.mult),
                        r=[SR, decR], w=[SR])
                    dve(lambda e, c=c: e.tensor_tensor(out=S32[0:64], in0=pKV[:, c], in1=Stmp[0:64], op=ALU.add), r=[SR, R7], w=[SR])
                    if c == 0:
                        dve(lambda e: e.tensor_copy(out=Sbf[0][0:64], in_=S32[0:64]), r=[SR], w=[SR])
                        po = B4[:, 0:256].rearrange("p (h v) -> p h v", v=64)
                        for h in range(4):
                            pe(lambda e, h=h: e.matmul(po[:, h, :], lhsT=Am[:, h, :], rhs=v_b[:, h * 64:(h + 1) * 64], start=True, stop=False),
                               r=[AmR, vbR], w=[R4])
                            pe(lambda e, h=h: e.matmul(po[:, h, :], lhsT=qTA[0:64, h, :], rhs=Sbf[1][0:64, h, :], start=False, stop=False),
                               r=[hTR2, SR], w=[R4])
                            pe(lambda e, h=h: e.matmul(po[:, h, :], lhsT=qTB[0:64, h, :], rhs=Sbf[0][0:64, h, :], start=False, stop=True),
                               r=[hTR2, SR], w=[R4])
                    else:
                        dve(lambda e: e.tensor_copy(out=Sbf[1][0:64], in_=S32[0:64]), r=[SR], w=[SR])
                act(lambda e: e.activation(out=osb, in_=B4[:, 0:256], func=AF.Copy), r=[R4], w=[oR, HR["enb"]])
                dve(lambda e: e.tensor_tensor(out=osq, in0=osb, in1=osb, op=ALU.mult), r=[oR], w=[oR, HR["t0"]])
                dve(lambda e: e.tensor_reduce(out=hst[:, 0:4], in_=osq.rearrange("p (h v) -> p h v", v=64), axis=AX.X, op=ALU.add), r=[oR], w=[oR])
                act(lambda e: e.activation(out=hst[:, 4:8], in_=hst[:, 0:4], func=AF.Ln, scale=1.0 / 64, bias=EPS), r=[oR], w=[oR])
                act(lambda e: e.activation(out=hst[:, 8:12], in_=hst[:, 4:8], func=AF.Exp, scale=-0.5), r=[oR], w=[oR])
                dve(lambda e: e.tensor_tensor(out=og.rearrange("p (h v) -> p h v", v=64), in0=osb.rearrange("p (h v) -> p h v", v=64),
                                              in1=hst[:, 8:12].unsqueeze(2).to_broadcast([128, 4, 64]), op=ALU.mult), r=[oR], w=[oR, HR["eb"]])
                dve(lambda e: e.tensor_tensor(out=og, in0=og, in1=H["gate"], op=ALU.mult), r=[oR, HR["gate"]], w=[oR])
                pg = B5[:, 0:256].rearrange("p (c t) -> p c t", t=128)
                for c in range(2):
                    pe(lambda e, c=c: e.transpose(out=pg[:, c, :], in_=og[:, c * 128:(c + 1) * 128], identity=ident_f[:]), r=[oR, cR], w=[R5])
                for c in range(2):
                    act(lambda e, c=c: e.activation(out=mixT[:, 2 + c, :], in_=pg[:, c, :], func=AF.Identity, scale=hngcol[:, l, c:c + 1]),
                        r=[R5, colR], w=[mixR])
                if i == NT - 1:
                    tap("S32_l%d" % l, S32[0:64], [SR])

                S.stage(3)
                for k in range(8):
                    pe(lambda e, k=k: e.matmul(B23[:, 0:512], lhsT=hT[:, k, :], rhs=w_in[:, k, 1280:1792], start=(k == 0), stop=(k == 7)),
                       r=[hTR, winR], w=[R23])
                for k in range(8):
                    pe(lambda e, k=k: e.matmul(B23[:, 512:960], lhsT=hT[:, k, :], rhs=w_in[:, k, 1792:2240], start=(k == 0), stop=(k == 7)),
                       r=[hTR, winR], w=[R23])
                for k in range(8):
                    pe(lambda e, k=k: e.matmul(B4[:, 0:132], lhsT=hT[:, k, :], rhs=w_in[:, k, 2240:2372], start=(k == 0), stop=(k == 7)),
                       r=[hTR, winR], w=[R4])
                pv = B23[:, 0:960].rearrange("p (h d) -> p h d", d=64)
                x1, x2 = pv[:, :, 0:8], pv[:, :, 8:16]
                cB = cs_sb[:, i, :].unsqueeze(1).to_broadcast([128, 15, 8])
                sB = sn_sb[:, i, :].unsqueeze(1).to_broadcast([128, 15, 8])
                dve(lambda e: e.tensor_tensor(out=rt[0], in0=x1, in1=cB, op=ALU.mult), r=[R23, ropeR], w=[aR])
                dve(lambda e: e.tensor_tensor(out=rt[1], in0=x2, in1=sB, op=ALU.mult), r=[R23, ropeR], w=[aR])
                dve(lambda e: e.tensor_tensor(out=rt[2], in0=x2, in1=cB, op=ALU.mult), r=[R23, ropeR], w=[aR])
                dve(lambda e: e.tensor_tensor(out=rt[3], in0=x1, in1=sB, op=ALU.mult), r=[R23, ropeR], w=[aR])
                qdst = qk_b[:, 0:8, :].rearrange("p (j k) d -> p k j d", k=2)
                pool(lambda e: e.tensor_tensor(out=qdst[:, :, :, 0:8], in0=rt[0][:, 0:8, :].rearrange("p (k j) d -> p k j d", k=2),
                                               in1=rt[1][:, 0:8, :].rearrange("p (k j) d -> p k j d", k=2), op=ALU.subtract), r=[aR], w=[aR])
                pool(lambda e: e.tensor_tensor(out=qdst[:, :, :, 8:16], in0=rt[2][:, 0:8, :].rearrange("p (k j) d -> p k j d", k=2),
                                               in1=rt[3][:, 0:8, :].rearrange("p (k j) d -> p k j d", k=2), op=ALU.add), r=[aR], w=[aR])
                pool(lambda e: e.tensor_tensor(out=qk_b[:, 8:10, 0:8], in0=rt[0][:, 8:10, :], in1=rt[1][:, 8:10, :], op=ALU.subtract), r=[aR], w=[aR])
                pool(lambda e: e.tensor_tensor(out=qk_b[:, 8:10, 8:16], in0=rt[2][:, 8:10, :], in1=rt[3][:, 8:10, :], op=ALU.add), r=[aR], w=[aR])
                pool(lambda e: e.tensor_tensor(out=qiki[:, :, 0:8], in0=rt[0][:, 10:15, :], in1=rt[1][:, 10:15, :], op=ALU.subtract), r=[aR], w=[aR])
                pool(lambda e: e.tensor_tensor(out=qiki[:, :, 8:16], in0=rt[2][:, 10:15, :], in1=rt[3][:, 10:15, :], op=ALU.add), r=[aR], w=[aR])
                act(lambda e: e.activation(out=qdst[:, :, :, 16:64], in_=pv[:, 0:8, 16:64].rearrange("p (k j) d -> p k j d", k=2), func=AF.Copy),
                    r=[R23], w=[aR])
                act(lambda e: e.activation(out=qk_b[:, 8:10, 16:64], in_=pv[:, 8:10, 16:64], func=AF.Copy), r=[R23], w=[aR])
                act(lambda e: e.activation(out=qiki[:, :, 16:64], in_=pv[:, 10:15, 16:64], func=AF.Copy), r=[R23], w=[aR])
                act(lambda e: e.activation(out=Vs[:, i, :, 0:64], in_=B4[:, 0:128].rearrange("p (k d) -> p k d", d=64), func=AF.Copy), r=[R4], w=[VR])
                act(lambda e: e.activation(out=ws, in_=B4[:, 128:132], func=AF.Copy, scale=1.0 / 16.0), r=[R4], w=[aR])
                for dd in range(2):
                    pool(lambda e, dd=dd: e.tensor_copy(out=qidup[:, :, dd * 64:(dd + 1) * 64], in_=qiki[:, 0:4, :]), r=[aR], w=[aR])
                    pool(lambda e, dd=dd: e.tensor_copy(out=kidup[:, dd * 64:(dd + 1) * 64], in_=qiki[:, 4, :]), r=[aR], w=[aR])
                tqa = bf(B5)[:, 0:512].rearrange("p (j t) -> p j t", t=128)
                tka = bf(B5)[:, 512:640]
                for j in range(4):
                    pe(lambda e, j=j: e.transpose(out=tqa[:, j, :], in_=qk_flat[:, j * 128:(j + 1) * 128], identity=ident_b[:]), r=[aR, cR], w=[R5])
                pe(lambda e: e.transpose(out=tka, in_=qk_flat[:, 512:640], identity=ident_b[:]), r=[aR, cR], w=[R5])
                tqi = B6.rearrange("p (h t) -> p h t", t=128)
                for h in range(4):
                    pe(lambda e, h=h: e.transpose(out=tqi[:, h, :], in_=qidup[:, h, :], identity=ident_f[:]), r=[aR, cR], w=[R6])
                tki = B7[:, 0:128]
                pe(lambda e: e.transpose(out=tki, in_=kidup, identity=ident_f[:]), r=[aR, cR], w=[R7])
                act(lambda e: e.activation(out=qT_sb, in_=tqa, func=AF.Copy), r=[R5], w=[aTR])
                act(lambda e: e.activation(out=KT[:, T0:T0 + 128], in_=tka, func=AF.Copy), r=[R5], w=[KTR])
                dve(lambda e: e.tensor_copy(out=qiT_sb, in_=tqi), r=[R6], w=[aTR])
                half = 0 if T0 < T // 2 else 64
                kcol = T0 - (0 if half == 0 else T // 2)
                dve(lambda e, half=half, kcol=kcol: e.tensor_copy(out=kiT[half:half + 64, kcol:kcol + 128], in_=tki[half:half + 64, :]),
                    r=[R7], w=[kiTR])
                S.stage(4)
                n = T0 + 128
                nblk = (n + 511) // 512
                sbanks = ((B0, R0), (B1, R1))
                cntj = 0
                segs = []
                for hh in range(2):
                    a0, a1 = hh * (T // 2), min(n, (hh + 1) * (T // 2))
                    k0 = a0
                    while k0 < a1:
                        segs.append((k0, min(512, a1 - k0), hh * 64, k0 - a0))
                        k0 += 512
                for (k0, wdt, kh_, kc0) in segs:
                    for h in range(4):
                        bank, bres = sbanks[cntj % 2]
                        rh, rhr = Rh[cntj % 2], RhR[cntj % 2]
                        cntj += 1
                        pe(lambda e, bank=bank, h=h, kh_=kh_, kc0=kc0, wdt=wdt: e.matmul(
                            bank[:, 0:wdt], lhsT=qiT_sb[kh_:kh_ + 64, h, :], rhs=kiT[kh_:kh_ + 64, kc0:kc0 + wdt], start=True, stop=True),
                           r=[aTR, kiTR], w=[bres])
                        if h == 0:
                            dve(lambda e, bank=bank, k0=k0, wdt=wdt: e.tensor_scalar(out=I_sb[:, k0:k0 + wdt], in0=bank[:, 0:wdt], scalar1=0.0,
                                                                                     scalar2=ws[:, 0:1], op0=ALU.max, op1=ALU.mult),
                                r=[bres, aR], w=[IR])
                        else:
                            act(lambda e, bank=bank, rh=rh, wdt=wdt: e.activation(out=rh[:, 0:wdt], in_=bank[:, 0:wdt], func=AF.Relu), r=[bres], w=[rhr])
                            pool(lambda e, rh=rh, h=h, wdt=wdt: e.tensor_scalar(out=rh[:, 0:wdt], in0=rh[:, 0:wdt], scalar1=ws[:, h:h + 1], scalar2=None,
                                                                                op0=ALU.mult), r=[rhr, aR], w=[rhr])
                            pool(lambda e, rh=rh, k0=k0, wdt=wdt: e.tensor_tensor(out=I_sb[:, k0:k0 + wdt], in0=rh[:, 0:wdt], in1=I_sb[:, k0:k0 + wdt], op=ALU.add),
                                 r=[rhr, IR], w=[IR])
                S.stage(5)
                if n > TOPK:
                    dve(lambda e, n=n: e.tensor_reduce(out=bis[:, 0:1], in_=I_sb[:, 0:n], axis=AX.X, op=ALU.max, apply_absolute_value=True),
                        r=[IR], w=[bisR])
                pool(lambda e: e.tensor_tensor(out=I_sb[:, T0:T0 + 128], in0=I_sb[:, T0:T0 + 128], in1=cmask[:], op=ALU.add), r=[IR, cR], w=[IR])
                thr = bis[:, 1:2]
                if n > TOPK:
                    dve(lambda e: e.tensor_scalar(out=stepcol, in0=pow2tab[:], scalar1=bis[:, 0:1], scalar2=None, op0=ALU.mult), r=[bisR, cR], w=[bisR])
                    dve(lambda e: e.memset(thr, 0.0), r=[bisR], w=[bisR])
                    for k in range(NBIS):
                        dve(lambda e, n=n: e.tensor_scalar(out=Mb[:, 0:n], in0=I_sb[:, 0:n], scalar1=thr, scalar2=None, op0=ALU.is_ge, op1=ALU.add,
                                                            accum_out=bis[:, 2:3]), r=[IR, bisR], w=[MbR, bisR])
                        dve(lambda e: e.tensor_scalar(out=bis[:, 3:4], in0=bis[:, 2:3], scalar1=float(TOPK), scalar2=0.5, op0=ALU.is_ge, op1=ALU.subtract),
                            r=[bisR], w=[bisR])
                        dve(lambda e, k=k: e.scalar_tensor_tensor(out=thr, in0=bis[:, 3:4], scalar=stepcol[:, k:k + 1], in1=thr, op0=ALU.mult, op1=ALU.add),
                            r=[bisR], w=[bisR])
                    dve(lambda e: e.scalar_tensor_tensor(out=thr, in0=stepcol[:, NBIS - 1:NBIS], scalar=-0.5, in1=thr, op0=ALU.mult, op1=ALU.add),
                        r=[bisR], w=[bisR])
                else:
                    dve(lambda e: e.memset(thr, -1e29), r=[bisR], w=[bisR])
                dve(lambda e, n=n: e.tensor_scalar(out=Mb[:, 0:n], in0=I_sb[:, 0:n], scalar1=thr, scalar2=1.0, op0=ALU.is_ge, op1=ALU.subtract),
                    r=[IR, bisR], w=[MbR])
                if i == NT - 1 and l == 0:
                    tap("I", I_sb, [IR]); tap("bis", bis, [bisR]); tap("Mb", Mb, [MbR]); tap("qT", qT_sb, [aTR]); tap("KT", KT, [KTR])
                    tap("V", Vs, [VR]); tap("ws", ws, [aR]); tap("qiT", qiT_sb, [aTR]); tap("kiT", kiT, [kiTR])
                S.stage(6)
                sT = ((B4, R4), (B5, R5))
                pO = (B6[:, 0:260].rearrange("p (h v) -> p h v", v=65), B7[:, 0:260].rearrange("p (h v) -> p h v", v=65))
                pOR = (R6, R7)
                cj = 0
                for kv in range(2):
                    for c in range(i + 1):
                        bank, bres = sT[cj % 2]
                        pt, ptr = PT[cj % 3], PTR[cj % 3]
                        cj += 1
                        pe(lambda e, bank=bank, kv=kv, c=c: e.matmul(bank, lhsT=KT[kv * 64:(kv + 1) * 64, c * 128:(c + 1) * 128],
                                                                     rhs=qT_sb[kv * 64:(kv + 1) * 64, :, :], start=True, stop=False),
                           r=[KTR, aTR], w=[bres])
                        pe(lambda e, bank=bank, c=c: e.matmul(bank, lhsT=Mb[:, c * 128:(c + 1) * 128], rhs=identBIG4[:], start=False, stop=True),
                           r=[MbR, cR], w=[bres])
                        act(lambda e, bank=bank, pt=pt: e.activation(out=pt, in_=bank, func=AF.Exp, scale=0.125), r=[bres], w=[ptr])
                        for h4 in range(4):
                            pe(lambda e, kv=kv, c=c, h4=h4, pt=pt: e.matmul(pO[kv][:, h4, :], lhsT=pt[:, h4 * 128:(h4 + 1) * 128], rhs=Vs[:, c, kv, :],
                                                                            start=(c == 0 and h4 == 0), stop=(c == i and h4 == 3), skip_group_check=True),
                               r=[ptr, VR], w=[pOR[kv]])
                    dve(lambda e, kv=kv: e.reciprocal(out=rden[:, kv * 4:(kv + 1) * 4], in_=pO[kv][:, :, 64]), r=[pOR[kv]], w=[yaR])
                    dve(lambda e, kv=kv: e.tensor_tensor(out=ya[:, kv * 256:(kv + 1) * 256].rearrange("p (h v) -> p h v", v=64), in0=pO[kv][:, :, 0:64],
                                                         in1=rden[:, kv * 4:(kv + 1) * 4].unsqueeze(2).to_broadcast([128, 4, 64]), op=ALU.mult),
                        r=[pOR[kv], yaR], w=[yaR])
                if i == NT - 1 and l == 0:
                    tap("ya", ya, [yaR]); tap("rden", rden, [yaR])
                ty = bf(B0)[:, 0:512].rearrange("p (c t) -> p c t", t=128)
                for c in range(4):
                    pe(lambda e, c=c: e.transpose(out=ty[:, c, :], in_=ya[:, c * 128:(c + 1) * 128], identity=ident_b[:]), r=[yaR, cR], w=[R0])
                act(lambda e: e.activation(out=mixT[:, 4:8, :], in_=ty, func=AF.Copy), r=[R0], w=[mixR])
                S.stage(1)
                if ("mixT_l%d" % l) in tap_d:
                    dma(POOL, lambda e, i=i, l=l: e.dma_start(out=tap_d["mixT_l%d" % l][:, :, i * 128:(i + 1) * 128], in_=mixT), r=[mixR], key="tap")

                S.stage(7)
                for hf in range(2):
                    for k in range(8):
                        pe(lambda e, hf=hf, k=k: e.matmul(B23[:, hf * 512:(hf + 1) * 512], lhsT=mixT[:, k, :], rhs=w_out[:, k, hf * 512:(hf + 1) * 512],
                                                          start=(k == 0), stop=(k == 7)), r=[mixR, woutR], w=[R23])
                for hf in range(2):
                    dve(lambda e, hf=hf, i=i: e.tensor_tensor(out=x_sb[:, i, hf * 512:(hf + 1) * 512], in0=B23[:, hf * 512:(hf + 1) * 512],
                                                               in1=x_sb[:, i, hf * 512:(hf + 1) * 512], op=ALU.add), r=[R23, xR[i]], w=[xR[i]])

            if ("xmid_l%d" % l) in tap_d:
                for i in range(NT):
                    dma(SP, lambda e, i=i, l=l: e.dma_start(out=tap_d["xmid_l%d" % l][i * 128:(i + 1) * 128, :], in_=x_sb[:, i, :]), r=[xR[i]], key="tap")

            S.stage(8)
            S.barrier()
            AA.reset(); AB.reset()
            h2T = AA.take([8, T], BF16); h2R = [Res("h2T%d" % i) for i in range(NT)]
            NCH = 8
            W1 = [AB.take([8, 512], BF16) for _ in range(2)]; W1R = [Res("W1_0"), Res("W1_1")]
            W2 = [AB.take([4, D], BF16) for _ in range(2)]; W2R = [Res("W2_0"), Res("W2_1")]
            aT = [AB.take([4, 512], BF16) for _ in range(2)]; aTRs = [[Res("aT%d_%d" % (q_, j_)) for j_ in range(4)] for q_ in range(2)]
            r32 = [AB.take([512], F32) for _ in range(4)]; r32R = [Res("r32_%d" % q_) for q_ in range(4)]
            st2 = AB.take([8], F32); st2R = Res("st2")
            hn2 = AB.take([D], BF16); hn2R = Res("hn2")
            dma(SP, lambda e: e.dma_start(out=gB[:], in_=n2_d[l:l + 1, :].to_broadcast([128, D])), w=[gBR], key="g")
            w1src = w1_d[l].rearrange("(c p) n -> p c n", p=128)
            w2src = w2_d[l].rearrange("(c p) n -> p c n", p=128)

            def load_chunk(c):
                s = c % 2
                for k in range(8):
                    dma(POOL, lambda e, k=k, s=s, c=c: e.dma_start(out=W1[s][:, k, :], in_=w1src[:, k, c * 512:(c + 1) * 512]), w=[W1R[s]], key="w1_%d" % s)
                for j in range(4):
                    dma(POOL, lambda e, j=j, s=s, c=c: e.dma_start(out=W2[s][:, j, :], in_=w2src[:, c * 4 + j, :]), w=[W2R[s]], key="w2_%d" % s)

            load_chunk(0)
            load_chunk(1)
            nbanks = ((B0, R0), (B1, R1))
            for i in range(NT):
                bank, bres = nbanks[i % 2]
                rmsnorm_to_hT(i, h2T[:, :, i * 128:(i + 1) * 128], h2R[i], bank, bres, (st2, hn2, st2R, hn2R))
            abanks = ((B0, R0), (B1, R1), (B23[:, 0:512], R23))
            ybanks = ((B4, R4), (B5, R5), (B6, R6), (B7, R7))
            aj = 0
            yj = 0
            NG = NT // 4 if NT >= 4 else 1
            GT = NT // NG
            for c in range(NCH):
                s = c % 2
                for tg in range(NG):
                    a_t, a_r = aT[(c * NG + tg) % 2], aTRs[(c * NG + tg) % 2]
                    ntok = GT * 128
                    for j in range(4):
                        bank, bres = abanks[aj % 3]
                        aj += 1
                        for k in range(8):
                            pe(lambda e, bank=bank, j=j, k=k, s=s, tg=tg, ntok=ntok: e.matmul(
                                bank[:, 0:ntok], lhsT=W1[s][:, k, j * 128:(j + 1) * 128], rhs=h2T[:, k, tg * ntok:(tg + 1) * ntok],
                                start=(k == 0), stop=(k == 7)), r=[W1R[s]] + h2R[tg * GT:(tg + 1) * GT], w=[bres])
                        rr, rrR = r32[aj % 4], r32R[aj % 4]
                        act(lambda e, bank=bank, rr=rr, ntok=ntok: e.activation(out=rr[:, 0:ntok], in_=bank[:, 0:ntok], func=AF.Relu),
                            r=[bres], w=[rrR])
                        sq_eng = DVE if j % 2 == 0 else POOL
                        S.add(sq_eng, lambda e, j=j, a_t=a_t, ntok=ntok, rr=rr: e.tensor_tensor(out=a_t[:, j, 0:ntok], in0=rr[:, 0:ntok], in1=rr[:, 0:ntok], op=ALU.mult),
                              [rrR], [a_r[j]])
                    for t4 in range(GT):
                        ti = tg * GT + t4
                        for hf in range(2):
                            bank, bres = ybanks[yj % 4]
                            yj += 1
                            for j in range(4):
                                pe(lambda e, bank=bank, j=j, t4=t4, hf=hf, s=s, a_t=a_t: e.matmul(
                                    bank, lhsT=a_t[:, j, t4 * 128:(t4 + 1) * 128], rhs=W2[s][:, j, hf * 512:(hf + 1) * 512], start=(j == 0), stop=(j == 3)),
                                   r=[a_r[j], W2R[s]], w=[bres])
                            dve(lambda e, bank=bank, ti=ti, hf=hf: e.tensor_tensor(out=x_sb[:, ti, hf * 512:(hf + 1) * 512], in0=bank,
                                                                                    in1=x_sb[:, ti, hf * 512:(hf + 1) * 512], op=ALU.add),
                                r=[bres, xR[ti]], w=[xR[ti]])
                if c + 2 < NCH:
                    load_chunk(c + 2)
            if ("xout_l%d" % l) in tap_d:
                for i in range(NT):
                    dma(SP, lambda e, i=i, l=l: e.dma_start(out=tap_d["xout_l%d" % l][i * 128:(i + 1) * 128, :], in_=x_sb[:, i, :]), r=[xR[i]], key="tap")

        S.stage(0)
        S.barrier()
        AB.reset()
        if not final:
            for i in range(NT):
                dma(SP, lambda e, i=i: e.dma_start(out=out_d[i * 128:(i + 1) * 128, :], in_=x_sb[:, i, :]), r=[xR[i]], key="out")
        else:
            dma(SP, lambda e: e.dma_start(out=gB[:], in_=fg_d.rearrange("(o d) -> o d", o=1).to_broadcast([128, D])), w=[gBR], key="g")
            fo = [AB.take([D], F32) for _ in range(2)]; foR = [Res("fo0"), Res("fo1")]
            fj = AB.take([D], BF16); fjR = Res("fj")
            fst = AB.take([8], F32); fstR = Res("fst")
            for i in range(NT):
                o_, oR_ = fo[i % 2], foR[i % 2]
                act(lambda e, i=i: e.activation(out=fj, in_=x_sb[:, i, :], func=AF.Square, accum_out=fst[:, 0:1]), r=[xR[i]], w=[fstR, fjR])
                act(lambda e: e.activation(out=fst[:, 1:2], in_=fst[:, 0:1], func=AF.Ln, scale=1.0 / D, bias=EPS), r=[fstR], w=[fstR])
                act(lambda e: e.activation(out=fst[:, 2:3], in_=fst[:, 1:2], func=AF.Exp, scale=-0.5), r=[fstR], w=[fstR])
                dve(lambda e, i=i, o_=o_: e.scalar_tensor_tensor(out=o_, in0=x_sb[:, i, :], scalar=fst[:, 2:3], in1=gB[:], op0=ALU.mult, op1=ALU.mult),
                    r=[xR[i], fstR, gBR], w=[oR_])
                dma(SP, lambda e, i=i, o_=o_: e.dma_start(out=out_d[i * 128:(i + 1) * 128, :], in_=o_), r=[oR_], key="out")

        fw = ["out"] + (["tap"] if tap_d else [])
        S.emit(final_waits=fw)
    return nc


_NC_CACHE = {}
SPLITS = [(0, 2), (2, 4)]


def kernel(x, positions, norm1_g, w_in, pool_w, pool_scale, lb_logits, hgrn_norm_g, w_out, norm2_g,
           w_ff_in, w_ff_out, final_norm_g):
    B, T, _ = x.shape
    NT = T // 128
    L = w_in.shape[0]
    splits = SPLITS if L == 4 else [(0, L)]
    progs = []
    for (a_, b_) in splits:
        key = (NT, L, a_, b_)
        if key not in _NC_CACHE:
            _NC_CACHE[key] = build_program(NT=NT, L=L, l0=a_, l1=b_, final=(b_ == L))
        progs.append(_NC_CACHE[key])
    f = lambda a: np.ascontiguousarray(np.asarray(a, dtype=np.float32))
    shared = {"norm1_g": f(norm1_g), "w_in": f(w_in), "pool_w": f(pool_w), "pool_scale": f(pool_scale),
              "lb_logits": f(lb_logits), "hgrn_norm_g": f(hgrn_norm_g), "w_out": f(w_out), "norm2_g": f(norm2_g),
              "w_ff_in": f(w_ff_in), "w_ff_out": f(w_ff_out), "final_norm_g": f(final_norm_g)}
    xs = np.asarray(x, dtype=np.float32)
    ps = np.asarray(positions, dtype=np.int32)
    in_maps = []
    for b in range(B):
        m = dict(shared)
        m["x"] = np.ascontiguousarray(xs[b])
        m["positions"] = np.ascontiguousarray(ps[b].reshape(NT, 128))
        in_maps.append(m)
    for nc in progs:
        res = run_bass_kernel_spmd(nc, in_maps, core_ids=list(range(B)))
        outs = [np.asarray(r["out"]) for r in res.results]
        for b in range(B):
            in_maps[b]["x"] = np.ascontiguousarray(outs[b])
    return np.stack(outs, axis=0).astype(np.float32)
```

```python
import contextlib
import numpy as np
import concourse.bass as bass
import concourse.mybir as mybir
from concourse.bass_utils import run_bass_kernel_spmd

F32 = mybir.dt.float32
BF16 = mybir.dt.bfloat16
I32 = mybir.dt.int32
ALU = mybir.AluOpType
AF = mybir.ActivationFunctionType
AX = mybir.AxisListType

PE, ACT, DVE, POOL, SP = "tensor", "scalar", "vector", "gpsimd", "sync"
ENGS = (PE, ACT, DVE, POOL, SP)

D = 1024
DIN = 2372
DFF = 4096
TOPK_MAX = 256
EPS = 1e-5
BIG = 30000.0
NBIS = 12


class Res:
    __slots__ = ("name", "last_w", "readers")

    def __init__(self, name):
        self.name = name
        self.last_w = None
        self.readers = []


class Op:
    __slots__ = ("eng", "fn", "deps", "is_dma", "sem_key", "sem_val", "seq", "needed")

    def __init__(self, eng, fn, is_dma=False, sem_key=None):
        self.eng = eng
        self.fn = fn
        self.deps = []
        self.is_dma = is_dma
        self.sem_key = sem_key
        self.sem_val = 0
        self.seq = 0
        self.needed = False


class _Rec:
    def __init__(self):
        self.call = None

    def __getattr__(self, name):
        def f(*a, **k):
            self.call = (name, a, k)
            return self
        return f


class Sched:
    def __init__(self, nc):
        self.nc = nc
        self.ops = []
        self.dma_counts = {}
        self.last_on = {}
        self.barrier_deps = []
        self.after_barrier = set()
        self.max_stage = 99
        self.cur_stage = 0

    def stage(self, k):
        self.cur_stage = k

    def _dep(self, op, d):
        if d is None or d is op:
            return
        if d.eng == PE and op.eng == PE and not d.is_dma and not op.is_dma:
            return
        op.deps.append(d)
        d.needed = True

    def add(self, eng, fn, reads=(), writes=(), is_dma=False, sem_key=None, group=False):
        if self.cur_stage > self.max_stage:
            return None
        rec = _Rec()
        fn(rec)
        op = Op(eng, rec.call, is_dma, sem_key)
        if is_dma:
            c = self.dma_counts.get(sem_key, 0) + 16
            self.dma_counts[sem_key] = c
            op.sem_val = c
        if self.barrier_deps and eng not in self.after_barrier:
            self.after_barrier.add(eng)
            for d in self.barrier_deps:
                self._dep(op, d)
        for r in reads:
            self._dep(op, r.last_w)
        for w in writes:
            lw = w.last_w
            if not (group and lw is not None and lw.is_dma and lw.sem_key == sem_key and lw.eng == eng):
                self._dep(op, lw)
            for rd in w.readers:
                self._dep(op, rd)
        for r in reads:
            r.readers.append(op)
            if len(r.readers) > 24:
                seen = {}
                for o in r.readers:
                    seen[(o.eng, o.is_dma, o.sem_key)] = o
                r.readers = list(seen.values())
        for w in writes:
            w.last_w = op
            w.readers = []
        self.ops.append(op)
        self.last_on[(eng, is_dma, sem_key if is_dma else None)] = op
        return op

    def barrier(self):
        self.barrier_deps = list(self.last_on.values())
        self.after_barrier = set()

    def emit(self, final_waits=()):
        nc = self.nc
        cnt = {e: 0 for e in ENGS}
        for op in self.ops:
            if not op.is_dma and op.needed:
                cnt[op.eng] += 1
                op.seq = cnt[op.eng]
        with contextlib.ExitStack() as st:
            esem = {e: st.enter_context(nc.semaphore("s_" + e)) for e in ENGS}
            dsem = {k: st.enter_context(nc.semaphore("d_%s" % (k,))) for k in self.dma_counts}
            block = st.enter_context(nc.Block())
            per_eng = {e: [o for o in self.ops if o.eng == e] for e in ENGS}

            def run(engname, eng):
                waited = {}
                for op in per_eng[engname]:
                    need = {}
                    for d in op.deps:
                        if d.is_dma:
                            key, val = ("d", d.sem_key), d.sem_val
                        else:
                            key, val = ("e", d.eng), d.seq
                        if need.get(key, 0) < val:
                            need[key] = val
                    todo = []
                    for key, val in need.items():
                        if waited.get(key, 0) >= val:
                            continue
                        waited[key] = val
                        todo.append((dsem[key[1]] if key[0] == "d" else esem[key[1]], val))
                    name_, a_, k_ = op.fn
                    single = (not op.is_dma) and k_.get("accum_out", None) is None
                    fused = todo.pop() if (single and todo) else None
                    for sem, val in todo:
                        eng.wait_ge(sem, val)
                    ins = getattr(eng, name_)(*a_, **k_)
                    if fused is not None:
                        ins._wait_ge(fused[0], fused[1])
                    if op.is_dma:
                        ins.then_inc(dsem[op.sem_key], 16)
                    elif op.needed:
                        ins.then_inc(esem[op.eng], 1)
                if engname == SP:
                    for k in final_waits:
                        if k in dsem:
                            eng.wait_ge(dsem[k], self.dma_counts[k])

            @block.tensor
            def _(e):
                run(PE, e)

            @block.scalar
            def _(e):
                run(ACT, e)

            @block.vector
            def _(e):
                run(DVE, e)

            @block.gpsimd
            def _(e):
                run(POOL, e)

            @block.sync
            def _(e):
                run(SP, e)


class Arena:
    def __init__(self, ap, nwords):
        self.ap = ap
        self.n = nwords
        self.off = 0

    def reset(self, off=0):
        self.off = off

    def take(self, shape, dt):
        n = 1
        for s in shape:
            n *= s
        words = n if dt == F32 or dt == I32 else (n + 1) // 2
        words = (words + 1) // 2 * 2
        assert self.off + words <= self.n, ("arena overflow", self.off, words, self.n)
        v = self.ap[:, self.off:self.off + words]
        self.off += words
        if dt != F32:
            v = v.bitcast(dt)
        v = v[:, 0:n]
        if len(shape) == 2:
            v = v.rearrange("p (a b) -> p a b", b=shape[1])
        elif len(shape) == 3:
            v = v.rearrange("p (a b c) -> p a b c", b=shape[1], c=shape[2])
        elif len(shape) == 4:
            v = v.rearrange("p (a b c d) -> p a b c d", b=shape[1], c=shape[2], d=shape[3])
        return v


def build_program(NT=16, L=4, taps=(), max_stage=99, l0=0, l1=None, final=True):
    T = NT * 128
    TOPK = min(TOPK_MAX, T // 4)
    if l1 is None:
        l1 = L
    nc = bass.Bass("TRN2", target_bir_lowering=False)
    dr = {}

    def din(name, shape, dt=F32):
        dr[name] = nc.dram_tensor(name, shape, dt, kind="ExternalInput").ap()
        return dr[name]

    x_d = din("x", [T, D])
    pos_d = din("positions", [NT, 128], I32)
    n1_d = din("norm1_g", [L, D])
    win_d = din("w_in", [L, D, DIN])
    pw_d = din("pool_w", [L, 4, 64, 64])
    psc_d = din("pool_scale", [L, 256])
    lbl_d = din("lb_logits", [L, 256])
    hng_d = din("hgrn_norm_g", [L, 256])
    wout_d = din("w_out", [L, D, D])
    n2_d = din("norm2_g", [L, D])
    w1_d = din("w_ff_in", [L, D, DFF])
    w2_d = din("w_ff_out", [L, DFF, D])
    fg_d = din("final_norm_g", [D])
    out_d = nc.dram_tensor("out", [T, D], F32, kind="ExternalOutput").ap()
    tap_d = {}
    for (tname, tshape) in taps:
        tap_d[tname] = nc.dram_tensor("tap_" + tname, tshape, F32, kind="ExternalOutput").ap()

    S = Sched(nc)
    S.max_stage = max_stage
    with contextlib.ExitStack() as st:
        def sb(name, shape, dt=F32):
            return st.enter_context(nc.sbuf_tensor(name, shape, dt))

        x_sb = sb("x_sb", [128, NT, D])
        xR = [Res("x%d" % i) for i in range(NT)]
        gB = sb("gB", [128, D])
        gBR = Res("gB")
        ident_f = sb("ident_f", [128, 128])
        ident_b = sb("ident_b", [128, 128], BF16)
        identBIG4 = sb("identBIG4", [128, 4, 128], BF16)
        ltri = sb("ltri", [128, 128])
        lrem = sb("lrem", [128, 128])
        cmask = sb("cmask", [128, 128])
        chunkind = sb("chunkind", [128, 2])
        pow2tab = sb("pow2tab", [128, NBIS])
        corr = sb("corr", [128, 2, 16])
        cs_sb = sb("cs_sb", [128, NT, 8])
        sn_sb = sb("sn_sb", [128, NT, 8])
        poolW = sb("poolW", [128, L, 2, 128], BF16)
        pscol = sb("pscol", [128, L, 2])
        hngcol = sb("hngcol", [128, L, 2])
        pB = sb("pB", [128, L, 256])
        lbB = sb("lbB", [128, 256])
        omlbB = sb("omlbB", [128, 256])
        cR = Res("consts")
        pwR = Res("poolW"); colR = Res("cols"); pBR = Res("pB"); ropeR = Res("rope")
        lbR = Res("lb")

        arenaA_t = sb("arenaA", [128, 9728])
        AA = Arena(arenaA_t, 9728)

        def psb(name, shape):
            return st.enter_context(nc.psum_tensor(name, shape, F32))
        B0 = psb("B0", [128, 512]); B1 = psb("B1", [128, 512])
        B23 = psb("B23", [128, 1024])
        B4 = psb("B4", [128, 512]); B5 = psb("B5", [128, 512])
        B6 = psb("B6", [128, 512]); B7 = psb("B7", [128, 512])
        B0, B1, B23, B4, B5, B6, B7 = [t_[:, :] for t_ in (B0, B1, B23, B4, B5, B6, B7)]
        B2 = B23[:, 0:512]; B3 = B23[:, 512:1024]
        R0, R1, R23, R4, R5, R6, R7 = [Res("B%d" % i) for i in (0, 1, 23, 4, 5, 6, 7)]

        def bf(ap_f32):
            return ap_f32.bitcast(BF16)

        nB = int(nc.sbuf_bytes_remaining) // 4 - 16
        arenaB_t = sb("arenaB", [128, nB])
        AB = Arena(arenaB_t, nB)

        def dve(fn, r=(), w=()): return S.add(DVE, fn, r, w)
        def act(fn, r=(), w=()): return S.add(ACT, fn, r, w)
        def pool(fn, r=(), w=()): return S.add(POOL, fn, r, w)
        def pe(fn, r=(), w=()): return S.add(PE, fn, r, w)
        def dma(q, fn, r=(), w=(), key=None, group=True): return S.add(q, fn, r, w, is_dma=True, sem_key=key, group=group)

        def tap(name, ap, res):
            if name in tap_d:
                dma(POOL, lambda e: e.dma_start(out=tap_d[name], in_=ap), r=res, w=(), key="tap")

        pool(lambda e: e.memset(ident_f[:], 0.0), w=[cR])
        pool(lambda e: e.affine_select(out=ident_f[:], in_=ident_f[:], pattern=[[-1, 128]], compare_op=ALU.not_equal,
                                       fill=1.0, base=0, channel_multiplier=1), r=[cR], w=[cR])
        pool(lambda e: e.tensor_copy(out=ident_b[:], in_=ident_f[:]), r=[cR], w=[cR])
        for h4 in range(4):
            pool(lambda e, h4=h4: e.tensor_scalar(out=identBIG4[:, h4, :], in0=ident_f[:], scalar1=BIG, scalar2=None,
                                                   op0=ALU.mult), r=[cR], w=[cR])
        pool(lambda e: e.memset(ltri[:], 1.0), w=[cR])
        pool(lambda e: e.affine_select(out=ltri[:], in_=ltri[:], pattern=[[1, 128]], compare_op=ALU.is_ge, fill=0.0,
                                       base=0, channel_multiplier=-1), r=[cR], w=[cR])
        pool(lambda e: e.memset(ltri[0:64, 64:128], 0.0), r=[cR], w=[cR])
        pool(lambda e: e.memset(lrem[:], 1.0), w=[cR])
        pool(lambda e: e.affine_select(out=lrem[:], in_=lrem[:], pattern=[[-1, 128]], compare_op=ALU.is_gt, fill=0.0,
                                       base=0, channel_multiplier=1), r=[cR], w=[cR])
        pool(lambda e: e.memset(lrem[64:128, 0:64], 0.0), r=[cR], w=[cR])
        pool(lambda e: e.memset(cmask[:], 0.0), w=[cR])
        pool(lambda e: e.affine_select(out=cmask[:], in_=cmask[:], pattern=[[-1, 128]], compare_op=ALU.is_ge, fill=-1e30,
                                       base=0, channel_multiplier=1), r=[cR], w=[cR])
        pool(lambda e: e.memset(chunkind[:], 0.0), w=[cR])
        pool(lambda e: e.memset(chunkind[0:64, 0:1], 1.0), r=[cR], w=[cR])
        pool(lambda e: e.memset(chunkind[64:128, 1:2], 1.0), r=[cR], w=[cR])
        for k in range(NBIS):
            pool(lambda e, k=k: e.memset(pow2tab[:, k:k + 1], 2.0 ** (-k)), w=[cR])
        pool(lambda e: e.memset(corr[:], 1.0), w=[cR])
        for g, win in enumerate((2, 4, 8, 16)):
            ct, ph = g // 2, (g % 2) * 64
            for t in range(win - 1):
                pool(lambda e, ct=ct, ph=ph, t=t, win=win: e.memset(corr[ph:ph + 64, ct, t:t + 1], float(win) / (t + 1)),
                     r=[cR], w=[cR])

        pool(lambda e: e.memset(poolW[:], 0.0), w=[pwR])
        for l in range(L):
            for g in range(4):
                ct, ph = g // 2, (g % 2) * 64
                dma(POOL, lambda e, l=l, g=g, ct=ct, ph=ph: e.dma_start(out=poolW[ph:ph + 64, l, ct, ph:ph + 64], in_=pw_d[l, g]),
                    w=[pwR], key="c_pw")
        dma(SP, lambda e: e.dma_start(out=pscol[:], in_=psc_d.rearrange("l (c p) -> p l c", p=128), allow_slow_non_contiguous=True), w=[colR], key="c_col")
        dma(SP, lambda e: e.dma_start(out=hngcol[:], in_=hng_d.rearrange("l (c p) -> p l c", p=128), allow_slow_non_contiguous=True), w=[colR], key="c_col")
        dma(SP, lambda e: e.dma_start(out=pB[:], in_=lbl_d.rearrange("(o l) c -> o l c", o=1).to_broadcast([128, L, 256])), w=[pBR], key="c_pB")
        pos_i = AB.take([NT], I32)
        pos_f = AB.take([NT], F32)
        ang = AB.take([NT, 8], F32)
        angr = AB.take([NT, 8], F32)
        dma(SP, lambda e: e.dma_start(out=pos_i[:], in_=pos_d.rearrange("i p -> p i"), allow_slow_non_contiguous=True), w=[ropeR], key="c_pos")
        dve(lambda e: e.tensor_copy(out=pos_f[:], in_=pos_i[:]), r=[ropeR], w=[ropeR])
        for j in range(8):
            inv = float(np.float32(500000.0) ** np.float32(-(2.0 * j) / 16.0))
            dve(lambda e, j=j, inv=inv: e.tensor_scalar(out=ang[:, :, j], in0=pos_f[:], scalar1=inv, scalar2=None, op0=ALU.mult),
                r=[ropeR], w=[ropeR])
        TWO_PI = 2.0 * np.pi
        kf = AB.take([NT, 8], F32)
        ki_ = AB.take([NT, 8], I32)
        tt = AB.take([NT, 8], F32)
        for (dst, shift) in ((sn_sb, 0.0), (cs_sb, 0.5 * np.pi)):
            dve(lambda e, shift=shift: e.tensor_scalar(out=angr[:], in0=ang[:], scalar1=float(shift), scalar2=None, op0=ALU.add), r=[ropeR], w=[ropeR])
            dve(lambda e: e.tensor_scalar(out=kf[:], in0=angr[:], scalar1=float(1.0 / TWO_PI), scalar2=None, op0=ALU.mult), r=[ropeR], w=[ropeR])
            dve(lambda e: e.tensor_copy(out=ki_[:], in_=kf[:]), r=[ropeR], w=[ropeR])
            dve(lambda e: e.tensor_copy(out=kf[:], in_=ki_[:]), r=[ropeR], w=[ropeR])
            dve(lambda e: e.scalar_tensor_tensor(out=angr[:], in0=kf[:], scalar=float(-TWO_PI), in1=angr[:], op0=ALU.mult, op1=ALU.add), r=[ropeR], w=[ropeR])
            dve(lambda e: e.tensor_scalar(out=tt[:], in0=angr[:], scalar1=float(np.pi), scalar2=float(-TWO_PI), op0=ALU.is_gt, op1=ALU.mult), r=[ropeR], w=[ropeR])
            dve(lambda e: e.tensor_tensor(out=angr[:], in0=angr[:], in1=tt[:], op=ALU.add), r=[ropeR], w=[ropeR])
            dve(lambda e: e.tensor_scalar(out=tt[:], in0=angr[:], scalar1=float(-np.pi), scalar2=float(TWO_PI), op0=ALU.is_lt, op1=ALU.mult), r=[ropeR], w=[ropeR])
            dve(lambda e: e.tensor_tensor(out=angr[:], in0=angr[:], in1=tt[:], op=ALU.add), r=[ropeR], w=[ropeR])
            dve(lambda e: e.tensor_scalar(out=angr[:], in0=angr[:], scalar1=3.141592, scalar2=-3.141592, op0=ALU.min, op1=ALU.max), r=[ropeR], w=[ropeR])
            act(lambda e, dst=dst: e.activation(out=dst[:], in_=angr[:], func=AF.Sin), r=[ropeR], w=[ropeR])
        mx = AB.take([256], F32)
        zs = AB.take([256], F32)
        dve(lambda e: e.tensor_copy(out=mx[:], in_=pB[:, 0, :]), r=[pBR], w=[pBR])
        for l in range(1, L):
            dve(lambda e, l=l: e.tensor_tensor(out=mx[:], in0=mx[:], in1=pB[:, l, :], op=ALU.max), r=[pBR], w=[pBR])
        for l in range(L):
            dve(lambda e, l=l: e.tensor_tensor(out=pB[:, l, :], in0=pB[:, l, :], in1=mx[:], op=ALU.subtract), r=[pBR], w=[pBR])
        act(lambda e: e.activation(out=pB[:], in_=pB[:], func=AF.Exp), r=[pBR], w=[pBR])
        dve(lambda e: e.tensor_copy(out=zs[:], in_=pB[:, 0, :]), r=[pBR], w=[pBR])
        for l in range(1, L):
            dve(lambda e, l=l: e.tensor_tensor(out=zs[:], in0=zs[:], in1=pB[:, l, :], op=ALU.add), r=[pBR], w=[pBR])
        dve(lambda e: e.reciprocal(out=zs[:], in_=zs[:]), r=[pBR], w=[pBR])
        for l in range(L):
            dve(lambda e, l=l: e.tensor_tensor(out=pB[:, l, :], in0=pB[:, l, :], in1=zs[:], op=ALU.mult), r=[pBR], w=[pBR])

        for i in range(NT):
            q = SP if i % 2 == 0 else ACT
            dma(q, lambda e, i=i: e.dma_start(out=x_sb[:, i, :], in_=x_d[i * 128:(i + 1) * 128, :]), w=[xR[i]], key="x%d" % i)

        def rmsnorm_to_hT(i, hT_dst, hT_res, bank, bank_res, tmp):
            st_, hn, stR, hnR = tmp
            act(lambda e: e.activation(out=hn[:], in_=x_sb[:, i, :], func=AF.Square, accum_out=st_[:, 0:1]),
                r=[xR[i]], w=[stR, hnR])
            act(lambda e: e.activation(out=st_[:, 1:2], in_=st_[:, 0:1], func=AF.Ln, scale=1.0 / D, bias=EPS), r=[stR], w=[stR])
            act(lambda e: e.activation(out=st_[:, 2:3], in_=st_[:, 1:2], func=AF.Exp, scale=-0.5), r=[stR], w=[stR])
            dve(lambda e: e.scalar_tensor_tensor(out=hn[:], in0=x_sb[:, i, :], scalar=st_[:, 2:3], in1=gB[:],
                                                 op0=ALU.mult, op1=ALU.mult), r=[xR[i], stR, gBR], w=[hnR])
            pT = bf(bank).rearrange("p (k t) -> p k t", t=128)
            for k in range(8):
                pe(lambda e, k=k: e.transpose(out=pT[:, k, :], in_=hn[:, k * 128:(k + 1) * 128], identity=ident_b[:]),
                   r=[hnR, cR], w=[bank_res])
            act(lambda e: e.activation(out=hT_dst, in_=pT[:, 0:8, :], func=AF.Copy), r=[bank_res], w=[hT_res])

        for l in range(l0):
            if l == 0:
                dve(lambda e: e.memset(lbB[:], 0.0), w=[lbR])
            else:
                dve(lambda e, l=l: e.tensor_tensor(out=lbB[:], in0=lbB[:], in1=pB[:, l, :], op=ALU.add), r=[pBR, lbR], w=[lbR])
        for l in range(l0, l1):
            S.stage(0)
            S.barrier()
            AA.reset(); AB.reset()
            w_in = AA.take([8, DIN], BF16)
            winR = Res("w_in")
            w_out = AB.take([8, D], BF16)
            woutR = Res("w_out")
            wsrc = win_d[l].rearrange("(c p) n -> p c n", p=128)
            for k in range(8):
                dma(POOL, lambda e, k=k: e.dma_start(out=w_in[:, k, 0:1920], in_=wsrc[:, k, 0:1920]), w=[winR], key="win")
            dma(POOL, lambda e: e.dma_start(out=w_in[:, :, 1920:2240], in_=wsrc[:, :, 2048:2368]), w=[winR], key="win")
            dma(POOL, lambda e: e.dma_start(out=w_in[:, :, 2240:2368], in_=wsrc[:, :, 1920:2048]), w=[winR], key="win")
            dma(POOL, lambda e: e.dma_start(out=w_in[:, :, 2368:2372], in_=wsrc[:, :, 2368:2372]), w=[winR], key="win")
            wosrc = wout_d[l].rearrange("(c p) n -> p c n", p=128)
            for k in range(8):
                dma(POOL, lambda e, k=k: e.dma_start(out=w_out[:, k, :], in_=wosrc[:, k, :]), w=[woutR], key="wout")
            dma(SP, lambda e: e.dma_start(out=gB[:], in_=n1_d[l:l + 1, :].to_broadcast([128, D])), w=[gBR], key="g")
            if l == 0:
                dve(lambda e: e.memset(lbB[:], 0.0), w=[lbR])
            else:
                dve(lambda e, l=l: e.tensor_tensor(out=lbB[:], in0=lbB[:], in1=pB[:, l, :], op=ALU.add), r=[pBR, lbR], w=[lbR])
            dve(lambda e: e.tensor_scalar(out=omlbB[:], in0=lbB[:], scalar1=-1.0, scalar2=1.0, op0=ALU.mult, op1=ALU.add),
                r=[lbR], w=[lbR])

            KT = AB.take([T], BF16); KTR = Res("KT")
            Vs = AB.take([NT, 2, 65], BF16); VR = Res("V")
            kiT = AB.take([T // 2], F32); kiTR = Res("kiT")
            I_sb = AB.take([T], F32); IR = Res("I")
            Mb = AB.take([T], BF16); MbR = Res("Mb")
            Rh = [AB.take([512], F32) for _ in range(2)]; RhR = [Res("Rh0"), Res("Rh1")]
            PT = [AB.take([512], BF16) for _ in range(3)]; PTR = [Res("PT%d" % j) for j in range(3)]
            st_ = AB.take([8], F32); stR = Res("st")
            hn = AB.take([D], BF16); hnR = Res("hn")
            hT = AB.take([8, 128], BF16); hTR = Res("hT")
            mixT = AB.take([8, 128], BF16); mixR = Res("mixT")
            uext = [AB.take([2, 144], F32) for _ in range(2)]; uR = [Res("u0"), Res("u1")]
            slv = [AB.take([2, 144], F32) for _ in range(4)]; sR = Res("slv")
            pooled = AB.take([2, 128], BF16); pooledR = Res("pooled")
            H = {n: AB.take([256], F32) for n in ("t0", "t1", "f", "kk", "eb", "enb", "ebl", "gate")}
            HR = {n: Res("h_" + n) for n in H}
            H["logf"] = H["f"]; HR["logf"] = HR["f"]
            H["q"] = H["t1"]; HR["q"] = HR["t1"]
            qt_b = AB.take([256], BF16); kt_b = AB.take([256], BF16)
            khA = AB.take([256], BF16); khB = AB.take([256], BF16); v_b = AB.take([256], BF16)
            hbR = Res("h_bf"); qtR = Res("h_qt"); ktR = Res("h_kt"); vbR = Res("h_v")
            qTf = AB.take([4, 128], BF16); qTA = AB.take([4, 128], BF16); qTB = AB.take([4, 128], BF16); kTs = AB.take([4, 128], BF16)
            hTR2 = Res("h_T")
            Am = AB.take([4, 128], BF16); AmR = Res("Am")
            dec = AB.take([8], F32); decR = Res("dec")
            S32 = AB.take([4, 64], F32); Sbf = [AB.take([4, 64], BF16) for _ in range(2)]; Stmp = AB.take([4, 64], F32)
            SR = Res("S")
            hst = AB.take([16], F32)
            osb = H["enb"]; osq = H["t0"]; og = H["eb"]
            oR = Res("o")
            qk_b = AB.take([10, 64], BF16); qiki = AB.take([5, 64], F32)
            qk_flat = qk_b.rearrange("p h d -> p (h d)")
            rt = [AB.take([15, 8], F32) for _ in range(4)]
            ws = AB.take([4], F32)
            aR = Res("attn_tm")
            qidup = AB.take([4, 128], F32); kidup = AB.take([128], F32)
            qT_sb = AB.take([4, 128], BF16); qiT_sb = AB.take([4, 128], F32)
            aTR = Res("attn_T")
            bis = AB.take([32], F32); bisR = Res("bis")
            stepcol = AB.take([NBIS], F32)
            rden = AB.take([8], F32); ya = hn[:, 0:512]; yaR = hnR
            pool(lambda e: e.memset(uext[0][:, :, 0:16], 0.0), w=[uR[0]])
            pool(lambda e: e.memset(S32[0:64], 0.0), w=[SR])
            pool(lambda e: e.memset(Sbf[1][0:64], 0.0), w=[SR])
            pool(lambda e: e.memset(qTA[0:64], 0.0), w=[hTR2])
            pool(lambda e: e.memset(qTB[0:64], 0.0), w=[hTR2])
            pool(lambda e: e.memset(khA[64:128, :], 0.0), w=[hbR])
            pool(lambda e: e.memset(khB[0:64, :], 0.0), w=[hbR])
            pool(lambda e: e.memset(Vs[:, :, :, 64:65], 1.0), w=[VR])

            for i in range(NT):
                T0 = i * 128
                S.stage(1)
                rmsnorm_to_hT(i, hT[:], hTR, B0, R0, (st_, hn, stR, hnR))

                S.stage(1.5)
                pu = B1[:, 0:256].rearrange("p (c t) -> p c t", t=128)
                for ct in range(2):
                    for k in range(8):
                        pe(lambda e, ct=ct, k=k: e.matmul(pu[:, ct, :], lhsT=w_in[:, k, ct * 128:(ct + 1) * 128], rhs=hT[:, k, :],
                                                           start=(k == 0), stop=(k == 7)), r=[winR, hTR], w=[R1])
                ue = uext[i % 2]; un = uext[(i + 1) % 2]
                act(lambda e, ue=ue: e.activation(out=ue[:, :, 16:144], in_=pu, func=AF.Copy), r=[R1], w=[uR[i % 2]])
                pool(lambda e, ue=ue, un=un: e.tensor_copy(out=un[:, :, 0:16], in_=ue[:, :, 128:144]), r=[uR[i % 2]], w=[uR[(i + 1) % 2]])
                prev = ue
                S.stage(1.6)
                for lv, sh in enumerate((1, 2, 4, 8)):
                    lo = 2 * sh - 1
                    pool(lambda e, lv=lv, sh=sh, lo=lo, prev=prev: e.tensor_tensor(out=slv[lv][:, :, lo:144], in0=prev[:, :, lo:144],
                                                                                   in1=prev[:, :, lo - sh:144 - sh], op=ALU.add),
                         r=[uR[i % 2], sR], w=[sR])
                    prev = slv[lv]
                S.stage(1.7)
                for g, win in enumerate((2, 4, 8, 16)):
                    ct, ph = g // 2, (g % 2) * 64
                    if i == 0:
                        pool(lambda e, g=g, ct=ct, ph=ph: e.tensor_tensor(out=slv[g][ph:ph + 64, ct, 16:32], in0=slv[g][ph:ph + 64, ct, 16:32],
                                                                          in1=corr[ph:ph + 64, ct, :], op=ALU.mult), r=[sR, cR], w=[sR])
                    dve(lambda e, g=g, ct=ct, ph=ph, win=win, ue=ue: e.scalar_tensor_tensor(
                        out=pooled[ph:ph + 64, ct, :], in0=slv[g][ph:ph + 64, ct, 16:144], scalar=1.0 / win, in1=ue[ph:ph + 64, ct, 16:144],
                        op0=ALU.mult, op1=ALU.subtract), r=[sR, uR[i % 2]], w=[pooledR])
                S.stage(1.8)
                py = B1[:, 256:512].rearrange("p (c t) -> p c t", t=128)
                for ct in range(2):
                    pe(lambda e, ct=ct: e.matmul(py[:, ct, :], lhsT=poolW[:, l, ct, :], rhs=pooled[:, ct, :], start=True, stop=True),
                       r=[pwR, pooledR], w=[R1])
                S.stage(1.9)
                for ct in range(2):
                    act(lambda e, ct=ct: e.activation(out=mixT[:, ct, :], in_=py[:, ct, :], func=AF.Identity, scale=pscol[:, l, ct:ct + 1]),
                        r=[R1, colR], w=[mixR])

                S.stage(2)
                for j, bank in enumerate((B2, B3)):
                    for k in range(8):
                        pe(lambda e, j=j, k=k, bank=bank: e.matmul(bank, lhsT=hT[:, k, :], rhs=w_in[:, k, 256 + j * 512:256 + (j + 1) * 512],
                                                                   start=(k == 0), stop=(k == 7)), r=[hTR, winR], w=[R23])
                hq_p, hf_p, hi_p, hg_p = B2[:, 0:256], B2[:, 256:512], B3[:, 0:256], B3[:, 256:512]
                act(lambda e: e.activation(out=H["t0"], in_=hf_p, func=AF.Exp, scale=-1.0), r=[R23], w=[HR["t0"]])
                act(lambda e: e.activation(out=H["t1"], in_=hq_p, func=AF.Exp, scale=-1.0), r=[R23], w=[HR["t1"]])
                act(lambda e: e.activation(out=H["gate"], in_=hg_p, func=AF.Exp, scale=-1.0), r=[R23], w=[HR["gate"]])
                act(lambda e: e.activation(out=v_b, in_=hi_p, func=AF.Copy), r=[R23], w=[vbR])
                pool(lambda e: e.tensor_scalar(out=H["t0"], in0=H["t0"], scalar1=1.0, scalar2=None, op0=ALU.add), r=[HR["t0"]], w=[HR["t0"]])
                dve(lambda e: e.reciprocal(out=H["t0"], in_=H["t0"]), r=[HR["t0"]], w=[HR["t0"]])
                pool(lambda e: e.tensor_tensor(out=H["f"], in0=H["t0"], in1=omlbB[:], op=ALU.mult), r=[HR["t0"], lbR], w=[HR["f"]])
                pool(lambda e: e.tensor_tensor(out=H["f"], in0=H["f"], in1=lbB[:], op=ALU.add), r=[HR["f"], lbR], w=[HR["f"]])
                pool(lambda e: e.tensor_scalar(out=H["kk"], in0=H["f"], scalar1=-1.0, scalar2=1.0, op0=ALU.mult, op1=ALU.add),
                     r=[HR["f"]], w=[HR["kk"]])
                pool(lambda e: e.tensor_scalar(out=H["f"], in0=H["f"], scalar1=1e-30, scalar2=None, op0=ALU.max), r=[HR["f"], HR["kk"]], w=[HR["f"]])
                act(lambda e: e.activation(out=H["logf"], in_=H["f"], func=AF.Ln), r=[HR["f"]], w=[HR["logf"]])
                pe(lambda e: e.matmul(B4[:, 0:256], lhsT=ltri[:], rhs=H["logf"], start=True, stop=True), r=[cR, HR["logf"]], w=[R4])
                pe(lambda e: e.matmul(B4[:, 256:512], lhsT=lrem[:], rhs=H["logf"], start=True, stop=True), r=[cR, HR["logf"]], w=[R4])
                dl = B0[0:64, 0:8].rearrange("p (h c) -> p h c", c=2)
                for h in range(4):
                    pe(lambda e, h=h: e.matmul(dl[:, h, :], lhsT=H["logf"][:, h * 64:(h + 1) * 64], rhs=chunkind[:], start=True, stop=True),
                       r=[cR, HR["logf"]], w=[R0])
                act(lambda e: e.activation(out=H["eb"], in_=B4[:, 0:256], func=AF.Exp), r=[R4], w=[HR["eb"]])
                act(lambda e: e.activation(out=H["enb"], in_=B4[:, 0:256], func=AF.Exp, scale=-1.0), r=[R4], w=[HR["enb"]])
                act(lambda e: e.activation(out=H["ebl"], in_=B4[:, 256:512], func=AF.Exp), r=[R4], w=[HR["ebl"]])
                act(lambda e: e.activation(out=dec[0:64].rearrange("p (h c) -> p h c", c=2), in_=dl, func=AF.Exp), r=[R0], w=[decR])
                pool(lambda e: e.tensor_scalar(out=H["t1"], in0=H["t1"], scalar1=1.0, scalar2=None, op0=ALU.add), r=[HR["t1"]], w=[HR["t1"]])
                dve(lambda e: e.reciprocal(out=H["t1"], in_=H["t1"]), r=[HR["t1"]], w=[HR["t1"]])
                dve(lambda e: e.tensor_tensor(out=H["q"], in0=hq_p, in1=H["t1"], op=ALU.mult), r=[R23, HR["t1"]], w=[HR["q"]])
                pool(lambda e: e.tensor_scalar(out=H["gate"], in0=H["gate"], scalar1=1.0, scalar2=None, op0=ALU.add), r=[HR["gate"]], w=[HR["gate"]])
                dve(lambda e: e.reciprocal(out=H["gate"], in_=H["gate"]), r=[HR["gate"]], w=[HR["gate"]])
                dve(lambda e: e.tensor_tensor(out=H["gate"], in0=hg_p, in1=H["gate"], op=ALU.mult), r=[R23, HR["gate"]], w=[HR["gate"]])
                pool(lambda e: e.tensor_tensor(out=qt_b, in0=H["q"], in1=H["eb"], op=ALU.mult), r=[HR["q"], HR["eb"]], w=[qtR])
                pool(lambda e: e.tensor_tensor(out=kt_b, in0=H["kk"], in1=H["enb"], op=ALU.mult), r=[HR["kk"], HR["enb"]], w=[ktR])
                pool(lambda e: e.tensor_tensor(out=khA[0:64, :], in0=H["kk"][0:64, :], in1=H["ebl"][0:64, :], op=ALU.mult),
                     r=[HR["kk"], HR["ebl"]], w=[hbR])
                pool(lambda e: e.tensor_tensor(out=khB[64:128, :], in0=H["kk"][64:128, :], in1=H["ebl"][64:128, :], op=ALU.mult),
                     r=[HR["kk"], HR["ebl"]], w=[hbR])
                tq = bf(B5)[0:64, 0:512].rearrange("p (h t) -> p h t", t=128)
                tk = bf(B5)[0:64, 512:1024].rearrange("p (h t) -> p h t", t=128)
                for h in range(4):
                    pe(lambda e, h=h: e.transpose(out=tq[:, h, :], in_=qt_b[:, h * 64:(h + 1) * 64], identity=ident_b[:]), r=[qtR, cR], w=[R5])
                for h in range(4):
                    pe(lambda e, h=h: e.transpose(out=tk[:, h, :], in_=kt_b[:, h * 64:(h + 1) * 64], identity=ident_b[:]), r=[ktR, cR], w=[R5])
                act(lambda e: e.activation(out=qTf[0:64], in_=tq, func=AF.Copy), r=[R5], w=[hTR2])
                dve(lambda e: e.tensor_copy(out=qTA[0:64, :, 0:64], in_=tq[:, :, 0:64]), r=[R5], w=[hTR2])
                dve(lambda e: e.tensor_copy(out=qTB[0:64, :, 64:128], in_=tq[:, :, 64:128]), r=[R5], w=[hTR2])
                act(lambda e: e.activation(out=kTs[0:64], in_=tk, func=AF.Copy), r=[R5], w=[hTR2])
                pA = B6.rearrange("p (h t) -> p h t", t=128)
                for h in range(4):
                    pe(lambda e, h=h: e.matmul(pA[:, h, :], lhsT=kTs[0:64, h, :], rhs=qTf[0:64, h, :], start=True, stop=True), r=[hTR2], w=[R6])
                dve(lambda e: e.tensor_tensor(out=Am, in0=pA, in1=ltri[:].unsqueeze(1).to_broadcast([128, 4, 128]), op=ALU.mult),
                    r=[R6, cR], w=[AmR])
                pKV = B7[0:64, :].rearrange("p (c h v) -> p c h v", c=2, h=4)
                for c, kh in enumerate((khA, khB)):
                    for h in range(4):
                        pe(lambda e, c=c, h=h, kh=kh: e.matmul(pKV[:, c, h, :], lhsT=kh[:, h * 64:(h + 1) * 64], rhs=v_b[:, h * 64:(h + 1) * 64],
                                                               start=True, stop=True), r=[hbR, vbR], w=[R7])
                decv = dec[0:64].rearrange("p (h c) -> p h c", c=2)
                for c in range(2):
                    dve(lambda e, c=c: e.tensor_tensor(out=Stmp[0:64], in0=S32[0:64], in1=decv[:, :, c:c + 1].to_broadcast([64, 4, 64]), op=ALU.mult),
                        r=[SR, decR], w=[SR])
                    dve(lambda e, c=c: e.tensor_tensor(out=S32[0:64], in0=pKV[:, c], in1=Stmp[0:64], op=ALU.add), r=[SR, R7], w=[SR])
                    if c == 0:
                        dve(lambda e: e.tensor_copy(out=Sbf[0][0:64], in_=S32[0:64]), r=[SR], w=[SR])
                        po = B4[:, 0:256].rearrange("p (h v) -> p h v", v=64)
                        for h in range(4):
                            pe(lambda e, h=h: e.matmul(po[:, h, :], lhsT=Am[:, h, :], rhs=v_b[:, h * 64:(h + 1) * 64], start=True, stop=False),
                               r=[AmR, vbR], w=[R4])
                            pe(lambda e, h=h: e.matmul(po[:, h, :], lhsT=qTA[0:64, h, :], rhs=Sbf[1][0:64, h, :], start=False, stop=False),
                               r=[hTR2, SR], w=[R4])
                            pe(lambda e, h=h: e.matmul(po[:, h, :], lhsT=qTB[0:64, h, :], rhs=Sbf[0][0:64, h, :], start=False, stop=True),
                               r=[hTR2, SR], w=[R4])
                    else:
                        dve(lambda e: e.tensor_copy(out=Sbf[1][0:64], in_=S32[0:64]), r=[SR], w=[SR])
                act(lambda e: e.activation(out=osb, in_=B4[:, 0:256], func=AF.Copy), r=[R4], w=[oR, HR["enb"]])
                dve(lambda e: e.tensor_tensor(out=osq, in0=osb, in1=osb, op=ALU.mult), r=[oR], w=[oR, HR["t0"]])
                dve(lambda e: e.tensor_reduce(out=hst[:, 0:4], in_=osq.rearrange("p (h v) -> p h v", v=64), axis=AX.X, op=ALU.add), r=[oR], w=[oR])
                act(lambda e: e.activation(out=hst[:, 4:8], in_=hst[:, 0:4], func=AF.Ln, scale=1.0 / 64, bias=EPS), r=[oR], w=[oR])
                act(lambda e: e.activation(out=hst[:, 8:12], in_=hst[:, 4:8], func=AF.Exp, scale=-0.5), r=[oR], w=[oR])
                dve(lambda e: e.tensor_tensor(out=og.rearrange("p (h v) -> p h v", v=64), in0=osb.rearrange("p (h v) -> p h v", v=64),
                                              in1=hst[:, 8:12].unsqueeze(2).to_broadcast([128, 4, 64]), op=ALU.mult), r=[oR], w=[oR, HR["eb"]])
                dve(lambda e: e.tensor_tensor(out=og, in0=og, in1=H["gate"], op=ALU.mult), r=[oR, HR["gate"]], w=[oR])
                pg = B5[:, 0:256].rearrange("p (c t) -> p c t", t=128)
                for c in range(2):
                    pe(lambda e, c=c: e.transpose(out=pg[:, c, :], in_=og[:, c * 128:(c + 1) * 128], identity=ident_f[:]), r=[oR, cR], w=[R5])
                for c in range(2):
                    act(lambda e, c=c: e.activation(out=mixT[:, 2 + c, :], in_=pg[:, c, :], func=AF.Identity, scale=hngcol[:, l, c:c + 1]),
                        r=[R5, colR], w=[mixR])
                if i == NT - 1:
                    tap("S32_l%d" % l, S32[0:64], [SR])

                S.stage(3)
                for k in range(8):
                    pe(lambda e, k=k: e.matmul(B23[:, 0:512], lhsT=hT[:, k, :], rhs=w_in[:, k, 1280:1792], start=(k == 0), stop=(k == 7)),
                       r=[hTR, winR], w=[R23])
                for k in range(8):
                    pe(lambda e, k=k: e.matmul(B23[:, 512:960], lhsT=hT[:, k, :], rhs=w_in[:, k, 1792:2240], start=(k == 0), stop=(k == 7)),
                       r=[hTR, winR], w=[R23])
                for k in range(8):
                    pe(lambda e, k=k: e.matmul(B4[:, 0:132], lhsT=hT[:, k, :], rhs=w_in[:, k, 2240:2372], start=(k == 0), stop=(k == 7)),
                       r=[hTR, winR], w=[R4])
                pv = B23[:, 0:960].rearrange("p (h d) -> p h d", d=64)
                x1, x2 = pv[:, :, 0:8], pv[:, :, 8:16]
                cB = cs_sb[:, i, :].unsqueeze(1).to_broadcast([128, 15, 8])
                sB = sn_sb[:, i, :].unsqueeze(1).to_broadcast([128, 15, 8])
                dve(lambda e: e.tensor_tensor(out=rt[0], in0=x1, in1=cB, op=ALU.mult), r=[R23, ropeR], w=[aR])
                dve(lambda e: e.tensor_tensor(out=rt[1], in0=x2, in1=sB, op=ALU.mult), r=[R23, ropeR], w=[aR])
                dve(lambda e: e.tensor_tensor(out=rt[2], in0=x2, in1=cB, op=ALU.mult), r=[R23, ropeR], w=[aR])
                dve(lambda e: e.tensor_tensor(out=rt[3], in0=x1, in1=sB, op=ALU.mult), r=[R23, ropeR], w=[aR])
                qdst = qk_b[:, 0:8, :].rearrange("p (j k) d -> p k j d", k=2)
                pool(lambda e: e.tensor_tensor(out=qdst[:, :, :, 0:8], in0=rt[0][:, 0:8, :].rearrange("p (k j) d -> p k j d", k=2),
                                               in1=rt[1][:, 0:8, :].rearrange("p (k j) d -> p k j d", k=2), op=ALU.subtract), r=[aR], w=[aR])
                pool(lambda e: e.tensor_tensor(out=qdst[:, :, :, 8:16], in0=rt[2][:, 0:8, :].rearrange("p (k j) d -> p k j d", k=2),
                                               in1=rt[3][:, 0:8, :].rearrange("p (k j) d -> p k j d", k=2), op=ALU.add), r=[aR], w=[aR])
                pool(lambda e: e.tensor_tensor(out=qk_b[:, 8:10, 0:8], in0=rt[0][:, 8:10, :], in1=rt[1][:, 8:10, :], op=ALU.subtract), r=[aR], w=[aR])
                pool(lambda e: e.tensor_tensor(out=qk_b[:, 8:10, 8:16], in0=rt[2][:, 8:10, :], in1=rt[3][:, 8:10, :], op=ALU.add), r=[aR], w=[aR])
                pool(lambda e: e.tensor_tensor(out=qiki[:, :, 0:8], in0=rt[0][:, 10:15, :], in1=rt[1][:, 10:15, :], op=ALU.subtract), r=[aR], w=[aR])
                pool(lambda e: e.tensor_tensor(out=qiki[:, :, 8:16], in0=rt[2][:, 10:15, :], in1=rt[3][:, 10:15, :], op=ALU.add), r=[aR], w=[aR])
                act(lambda e: e.activation(out=qdst[:, :, :, 16:64], in_=pv[:, 0:8, 16:64].rearrange("p (k j) d -> p k j d", k=2), func=AF.Copy),
                    r=[R23], w=[aR])
                act(lambda e: e.activation(out=qk_b[:, 8:10, 16:64], in_=pv[:, 8:10, 16:64], func=AF.Copy), r=[R23], w=[aR])
                act(lambda e: e.activation(out=qiki[:, :, 16:64], in_=pv[:, 10:15, 16:64], func=AF.Copy), r=[R23], w=[aR])
                act(lambda e: e.activation(out=Vs[:, i, :, 0:64], in_=B4[:, 0:128].rearrange("p (k d) -> p k d", d=64), func=AF.Copy), r=[R4], w=[VR])
                act(lambda e: e.activation(out=ws, in_=B4[:, 128:132], func=AF.Copy, scale=1.0 / 16.0), r=[R4], w=[aR])
                for dd in range(2):
                    pool(lambda e, dd=dd: e.tensor_copy(out=qidup[:, :, dd * 64:(dd + 1) * 64], in_=qiki[:, 0:4, :]), r=[aR], w=[aR])
                    pool(lambda e, dd=dd: e.tensor_copy(out=kidup[:, dd * 64:(dd + 1) * 64], in_=qiki[:, 4, :]), r=[aR], w=[aR])
                tqa = bf(B5)[:, 0:512].rearrange("p (j t) -> p j t", t=128)
                tka = bf(B5)[:, 512:640]
                for j in range(4):
                    pe(lambda e, j=j: e.transpose(out=tqa[:, j, :], in_=qk_flat[:, j * 128:(j + 1) * 128], identity=ident_b[:]), r=[aR, cR], w=[R5])
                pe(lambda e: e.transpose(out=tka, in_=qk_flat[:, 512:640], identity=ident_b[:]), r=[aR, cR], w=[R5])
                tqi = B6.rearrange("p (h t) -> p h t", t=128)
                for h in range(4):
                    pe(lambda e, h=h: e.transpose(out=tqi[:, h, :], in_=qidup[:, h, :], identity=ident_f[:]), r=[aR, cR], w=[R6])
                tki = B7[:, 0:128]
                pe(lambda e: e.transpose(out=tki, in_=kidup, identity=ident_f[:]), r=[aR, cR], w=[R7])
                act(lambda e: e.activation(out=qT_sb, in_=tqa, func=AF.Copy), r=[R5], w=[aTR])
                act(lambda e: e.activation(out=KT[:, T0:T0 + 128], in_=tka, func=AF.Copy), r=[R5], w=[KTR])
                dve(lambda e: e.tensor_copy(out=qiT_sb, in_=tqi), r=[R6], w=[aTR])
                half = 0 if T0 < T // 2 else 64
                kcol = T0 - (0 if half == 0 else T // 2)
                dve(lambda e, half=half, kcol=kcol: e.tensor_copy(out=kiT[half:half + 64, kcol:kcol + 128], in_=tki[half:half + 64, :]),
                    r=[R7], w=[kiTR])
                S.stage(4)
                n = T0 + 128
                nblk = (n + 511) // 512
                sbanks = ((B0, R0), (B1, R1))
                cntj = 0
                segs = []
                for hh in range(2):
                    a0, a1 = hh * (T // 2), min(n, (hh + 1) * (T // 2))
                    k0 = a0
                    while k0 < a1:
                        segs.append((k0, min(512, a1 - k0), hh * 64, k0 - a0))
                        k0 += 512
                for (k0, wdt, kh_, kc0) in segs:
                    for h in range(4):
                        bank, bres = sbanks[cntj % 2]
                        rh, rhr = Rh[cntj % 2], RhR[cntj % 2]
                        cntj += 1
                        pe(lambda e, bank=bank, h=h, kh_=kh_, kc0=kc0, wdt=wdt: e.matmul(
                            bank[:, 0:wdt], lhsT=qiT_sb[kh_:kh_ + 64, h, :], rhs=kiT[kh_:kh_ + 64, kc0:kc0 + wdt], start=True, stop=True),
                           r=[aTR, kiTR], w=[bres])
                        if h == 0:
                            dve(lambda e, bank=bank, k0=k0, wdt=wdt: e.tensor_scalar(out=I_sb[:, k0:k0 + wdt], in0=bank[:, 0:wdt], scalar1=0.0,
                                                                                     scalar2=ws[:, 0:1], op0=ALU.max, op1=ALU.mult),
                                r=[bres, aR], w=[IR])
                        else:
                            act(lambda e, bank=bank, rh=rh, wdt=wdt: e.activation(out=rh[:, 0:wdt], in_=bank[:, 0:wdt], func=AF.Relu), r=[bres], w=[rhr])
                            dve(lambda e, rh=rh, h=h, k0=k0, wdt=wdt: e.scalar_tensor_tensor(
                                out=I_sb[:, k0:k0 + wdt], in0=rh[:, 0:wdt], scalar=ws[:, h:h + 1], in1=I_sb[:, k0:k0 + wdt],
                                op0=ALU.mult, op1=ALU.add), r=[rhr, aR, IR], w=[IR])
                S.stage(5)
                if n > TOPK:
                    dve(lambda e, n=n: e.tensor_reduce(out=bis[:, 0:1], in_=I_sb[:, 0:n], axis=AX.X, op=ALU.max, apply_absolute_value=True),
                        r=[IR], w=[bisR])
                pool(lambda e: e.tensor_tensor(out=I_sb[:, T0:T0 + 128], in0=I_sb[:, T0:T0 + 128], in1=cmask[:], op=ALU.add), r=[IR, cR], w=[IR])
                thr = bis[:, 1:2]
                if n > TOPK:
                    dve(lambda e: e.tensor_scalar(out=stepcol, in0=pow2tab[:], scalar1=bis[:, 0:1], scalar2=None, op0=ALU.mult), r=[bisR, cR], w=[bisR])
                    dve(lambda e: e.memset(thr, 0.0), r=[bisR], w=[bisR])
                    for k in range(NBIS):
                        dve(lambda e, n=n: e.tensor_scalar(out=Mb[:, 0:n], in0=I_sb[:, 0:n], scalar1=thr, scalar2=None, op0=ALU.is_ge, op1=ALU.add,
                                                            accum_out=bis[:, 2:3]), r=[IR, bisR], w=[MbR, bisR])
                        dve(lambda e: e.tensor_scalar(out=bis[:, 3:4], in0=bis[:, 2:3], scalar1=float(TOPK), scalar2=0.5, op0=ALU.is_ge, op1=ALU.subtract),
                            r=[bisR], w=[bisR])
                        dve(lambda e, k=k: e.scalar_tensor_tensor(out=thr, in0=bis[:, 3:4], scalar=stepcol[:, k:k + 1], in1=thr, op0=ALU.mult, op1=ALU.add),
                            r=[bisR], w=[bisR])
                    dve(lambda e: e.scalar_tensor_tensor(out=thr, in0=stepcol[:, NBIS - 1:NBIS], scalar=-0.5, in1=thr, op0=ALU.mult, op1=ALU.add),
                        r=[bisR], w=[bisR])
                else:
                    dve(lambda e: e.memset(thr, -1e29), r=[bisR], w=[bisR])
                dve(lambda e, n=n: e.tensor_scalar(out=Mb[:, 0:n], in0=I_sb[:, 0:n], scalar1=thr, scalar2=1.0, op0=ALU.is_ge, op1=ALU.subtract),
                    r=[IR, bisR], w=[MbR])
                if i == NT - 1 and l == 0:
                    tap("I", I_sb, [IR]); tap("bis", bis, [bisR]); tap("Mb", Mb, [MbR]); tap("qT", qT_sb, [aTR]); tap("KT", KT, [KTR])
                    tap("V", Vs, [VR]); tap("ws", ws, [aR]); tap("qiT", qiT_sb, [aTR]); tap("kiT", kiT, [kiTR])
                S.stage(6)
                sT = ((B4, R4), (B5, R5))
                pOT = (B6, B7)
                pOR = (R6, R7)
                pOq = (B0[:, 0:260].rearrange("p (h v) -> p h v", v=65), B1[:, 0:260].rearrange("p (h v) -> p h v", v=65))
                pOqR = (R0, R1)
                OTs = Rh[0]; OTsR = RhR[0]
                cj = 0
                for kv in range(2):
                    for c in range(i + 1):
                        bank, bres = sT[cj % 2]
                        pt, ptr = PT[cj % 3], PTR[cj % 3]
                        cj += 1
                        pe(lambda e, bank=bank, kv=kv, c=c: e.matmul(bank, lhsT=KT[kv * 64:(kv + 1) * 64, c * 128:(c + 1) * 128],
                                                                     rhs=qT_sb[kv * 64:(kv + 1) * 64, :, :], start=True, stop=False),
                           r=[KTR, aTR], w=[bres])
                        pe(lambda e, bank=bank, c=c: e.matmul(bank, lhsT=Mb[:, c * 128:(c + 1) * 128], rhs=identBIG4[:], start=False, stop=True),
                           r=[MbR, cR], w=[bres])
                        act(lambda e, bank=bank, pt=pt: e.activation(out=pt, in_=bank, func=AF.Exp, scale=0.125), r=[bres], w=[ptr])
                        pe(lambda e, kv=kv, c=c, pt=pt: e.matmul(pOT[kv][0:65, :], lhsT=Vs[:, c, kv, :], rhs=pt, start=(c == 0), stop=(c == i)),
                           r=[ptr, VR], w=[pOR[kv]])
                    act(lambda e, kv=kv: e.activation(out=OTs[0:65, :], in_=pOT[kv][0:65, :], func=AF.Copy), r=[pOR[kv]], w=[OTsR])
                    for h4 in range(4):
                        pe(lambda e, kv=kv, h4=h4: e.transpose(out=pOq[kv][:, h4, :], in_=OTs[0:65, h4 * 128:(h4 + 1) * 128], identity=ident_f[0:65, 0:65]),
                           r=[OTsR, cR], w=[pOqR[kv]])
                    dve(lambda e, kv=kv: e.reciprocal(out=rden[:, kv * 4:(kv + 1) * 4], in_=pOq[kv][:, :, 64]), r=[pOqR[kv]], w=[yaR])
                    dve(lambda e, kv=kv: e.tensor_tensor(out=ya[:, kv * 256:(kv + 1) * 256].rearrange("p (h v) -> p h v", v=64), in0=pOq[kv][:, :, 0:64],
                                                         in1=rden[:, kv * 4:(kv + 1) * 4].unsqueeze(2).to_broadcast([128, 4, 64]), op=ALU.mult),
                        r=[pOqR[kv], yaR], w=[yaR])
                if i == NT - 1 and l == 0:
                    tap("ya", ya, [yaR]); tap("rden", rden, [yaR])
                ty = bf(B0)[:, 0:512].rearrange("p (c t) -> p c t", t=128)
                for c in range(4):
                    pe(lambda e, c=c: e.transpose(out=ty[:, c, :], in_=ya[:, c * 128:(c + 1) * 128], identity=ident_b[:]), r=[yaR, cR], w=[R0])
                act(lambda e: e.activation(out=mixT[:, 4:8, :], in_=ty, func=AF.Copy), r=[R0], w=[mixR])
                S.stage(1)
                if ("mixT_l%d" % l) in tap_d:
                    dma(POOL, lambda e, i=i, l=l: e.dma_start(out=tap_d["mixT_l%d" % l][:, :, i * 128:(i + 1) * 128], in_=mixT), r=[mixR], key="tap")

                S.stage(7)
                for hf in range(2):
                    for k in range(8):
                        pe(lambda e, hf=hf, k=k: e.matmul(B23[:, hf * 512:(hf + 1) * 512], lhsT=mixT[:, k, :], rhs=w_out[:, k, hf * 512:(hf + 1) * 512],
                                                          start=(k == 0), stop=(k == 7)), r=[mixR, woutR], w=[R23])
                for hf in range(2):
                    dve(lambda e, hf=hf, i=i: e.tensor_tensor(out=x_sb[:, i, hf * 512:(hf + 1) * 512], in0=B23[:, hf * 512:(hf + 1) * 512],
                                                               in1=x_sb[:, i, hf * 512:(hf + 1) * 512], op=ALU.add), r=[R23, xR[i]], w=[xR[i]])

            if ("xmid_l%d" % l) in tap_d:
                for i in range(NT):
                    dma(SP, lambda e, i=i, l=l: e.dma_start(out=tap_d["xmid_l%d" % l][i * 128:(i + 1) * 128, :], in_=x_sb[:, i, :]), r=[xR[i]], key="tap")

            S.stage(8)
            S.barrier()
            AA.reset(); AB.reset()
            h2T = AA.take([8, T], BF16); h2R = [Res("h2T%d" % i) for i in range(NT)]
            NCH = 8
            W1 = [AB.take([8, 512], BF16) for _ in range(2)]; W1R = [Res("W1_0"), Res("W1_1")]
            W2 = [AB.take([4, D], BF16) for _ in range(2)]; W2R = [Res("W2_0"), Res("W2_1")]
            aT = [AB.take([4, 512], BF16) for _ in range(2)]; aTRs = [[Res("aT%d_%d" % (q_, j_)) for j_ in range(4)] for q_ in range(2)]
            r32 = [AB.take([512], F32) for _ in range(4)]; r32R = [Res("r32_%d" % q_) for q_ in range(4)]
            st2 = AB.take([8], F32); st2R = Res("st2")
            hn2 = AB.take([D], BF16); hn2R = Res("hn2")
            dma(SP, lambda e: e.dma_start(out=gB[:], in_=n2_d[l:l + 1, :].to_broadcast([128, D])), w=[gBR], key="g")
            w1src = w1_d[l].rearrange("(c p) n -> p c n", p=128)
            w2src = w2_d[l].rearrange("(c p) n -> p c n", p=128)

            def load_chunk(c):
                s = c % 2
                for k in range(8):
                    dma(POOL, lambda e, k=k, s=s, c=c: e.dma_start(out=W1[s][:, k, :], in_=w1src[:, k, c * 512:(c + 1) * 512]), w=[W1R[s]], key="w1_%d" % s)
                for j in range(4):
                    dma(POOL, lambda e, j=j, s=s, c=c: e.dma_start(out=W2[s][:, j, :], in_=w2src[:, c * 4 + j, :]), w=[W2R[s]], key="w2_%d" % s)

            load_chunk(0)
            load_chunk(1)
            nbanks = ((B0, R0), (B1, R1))
            for i in range(NT):
                bank, bres = nbanks[i % 2]
                rmsnorm_to_hT(i, h2T[:, :, i * 128:(i + 1) * 128], h2R[i], bank, bres, (st2, hn2, st2R, hn2R))
            abanks = ((B0, R0), (B1, R1), (B23[:, 0:512], R23))
            ybanks = ((B4, R4), (B5, R5), (B6, R6), (B7, R7))
            aj = 0
            yj = 0
            NG = NT // 4 if NT >= 4 else 1
            GT = NT // NG
            for c in range(NCH):
                s = c % 2
                for tg in range(NG):
                    a_t, a_r = aT[(c * NG + tg) % 2], aTRs[(c * NG + tg) % 2]
                    ntok = GT * 128
                    for j in range(4):
                        bank, bres = abanks[aj % 3]
                        aj += 1
                        for k in range(8):
                            pe(lambda e, bank=bank, j=j, k=k, s=s, tg=tg, ntok=ntok: e.matmul(
                                bank[:, 0:ntok], lhsT=W1[s][:, k, j * 128:(j + 1) * 128], rhs=h2T[:, k, tg * ntok:(tg + 1) * ntok],
                                start=(k == 0), stop=(k == 7)), r=[W1R[s]] + h2R[tg * GT:(tg + 1) * GT], w=[bres])
                        rr, rrR = r32[aj % 4], r32R[aj % 4]
                        act(lambda e, bank=bank, rr=rr, ntok=ntok: e.activation(out=rr[:, 0:ntok], in_=bank[:, 0:ntok], func=AF.Relu),
                            r=[bres], w=[rrR])
                        sq_eng = DVE if j % 2 == 0 else POOL
                        S.add(sq_eng, lambda e, j=j, a_t=a_t, ntok=ntok, rr=rr: e.tensor_tensor(out=a_t[:, j, 0:ntok], in0=rr[:, 0:ntok], in1=rr[:, 0:ntok], op=ALU.mult),
                              [rrR], [a_r[j]])
                    for t4 in range(GT):
                        ti = tg * GT + t4
                        for hf in range(2):
                            bank, bres = ybanks[yj % 4]
                            yj += 1
                            for j in range(4):
                                pe(lambda e, bank=bank, j=j, t4=t4, hf=hf, s=s, a_t=a_t: e.matmul(
                                    bank, lhsT=a_t[:, j, t4 * 128:(t4 + 1) * 128], rhs=W2[s][:, j, hf * 512:(hf + 1) * 512], start=(j == 0), stop=(j == 3)),
                                   r=[a_r[j], W2R[s]], w=[bres])
                            dve(lambda e, bank=bank, ti=ti, hf=hf: e.tensor_tensor(out=x_sb[:, ti, hf * 512:(hf + 1) * 512], in0=bank,
                                                                                    in1=x_sb[:, ti, hf * 512:(hf + 1) * 512], op=ALU.add),
                                r=[bres, xR[ti]], w=[xR[ti]])
                if c + 2 < NCH:
                    load_chunk(c + 2)
            if ("xout_l%d" % l) in tap_d:
                for i in range(NT):
                    dma(SP, lambda e, i=i, l=l: e.dma_start(out=tap_d["xout_l%d" % l][i * 128:(i + 1) * 128, :], in_=x_sb[:, i, :]), r=[xR[i]], key="tap")

        S.stage(0)
        S.barrier()
        AB.reset()
        if not final:
            for i in range(NT):
                dma(SP, lambda e, i=i: e.dma_start(out=out_d[i * 128:(i + 1) * 128, :], in_=x_sb[:, i, :]), r=[xR[i]], key="out")
        else:
            dma(SP, lambda e: e.dma_start(out=gB[:], in_=fg_d.rearrange("(o d) -> o d", o=1).to_broadcast([128, D])), w=[gBR], key="g")
            fo = [AB.take([D], F32) for _ in range(2)]; foR = [Res("fo0"), Res("fo1")]
            fj = AB.take([D], BF16); fjR = Res("fj")
            fst = AB.take([8], F32); fstR = Res("fst")
            for i in range(NT):
                o_, oR_ = fo[i % 2], foR[i % 2]
                act(lambda e, i=i: e.activation(out=fj, in_=x_sb[:, i, :], func=AF.Square, accum_out=fst[:, 0:1]), r=[xR[i]], w=[fstR, fjR])
                act(lambda e: e.activation(out=fst[:, 1:2], in_=fst[:, 0:1], func=AF.Ln, scale=1.0 / D, bias=EPS), r=[fstR], w=[fstR])
                act(lambda e: e.activation(out=fst[:, 2:3], in_=fst[:, 1:2], func=AF.Exp, scale=-0.5), r=[fstR], w=[fstR])
                dve(lambda e, i=i, o_=o_: e.scalar_tensor_tensor(out=o_, in0=x_sb[:, i, :], scalar=fst[:, 2:3], in1=gB[:], op0=ALU.mult, op1=ALU.mult),
                    r=[xR[i], fstR, gBR], w=[oR_])
                dma(SP, lambda e, i=i, o_=o_: e.dma_start(out=out_d[i * 128:(i + 1) * 128, :], in_=o_), r=[oR_], key="out")

        fw = ["out"] + (["tap"] if tap_d else [])
        S.emit(final_waits=fw)
    return nc


_NC_CACHE = {}
SPLITS = [(0, 4)]


def kernel(x, positions, norm1_g, w_in, pool_w, pool_scale, lb_logits, hgrn_norm_g, w_out, norm2_g,
           w_ff_in, w_ff_out, final_norm_g):
    B, T, _ = x.shape
    NT = T // 128
    L = w_in.shape[0]
    splits = SPLITS if L == 4 else [(0, L)]
    progs = []
    for (a_, b_) in splits:
        key = (NT, L, a_, b_)
        if key not in _NC_CACHE:
            _NC_CACHE[key] = build_program(NT=NT, L=L, l0=a_, l1=b_, final=(b_ == L))
        progs.append(_NC_CACHE[key])
    f = lambda a: np.ascontiguousarray(np.asarray(a, dtype=np.float32))
    shared = {"norm1_g": f(norm1_g), "w_in": f(w_in), "pool_w": f(pool_w), "pool_scale": f(pool_scale),
              "lb_logits": f(lb_logits), "hgrn_norm_g": f(hgrn_norm_g), "w_out": f(w_out), "norm2_g": f(norm2_g),
              "w_ff_in": f(w_ff_in), "w_ff_out": f(w_ff_out), "final_norm_g": f(final_norm_g)}
    xs = np.asarray(x, dtype=np.float32)
    ps = np.asarray(positions, dtype=np.int32)
    in_maps = []
    for b in range(B):
        m = dict(shared)
        m["x"] = np.ascontiguousarray(xs[b])
        m["positions"] = np.ascontiguousarray(ps[b].reshape(NT, 128))
        in_maps.append(m)
    for nc in progs:
        res = run_bass_kernel_spmd(nc, in_maps, core_ids=list(range(B)))
        outs = [np.asarray(r["out"]) for r in res.results]
        for b in range(B):
            in_maps[b]["x"] = np.ascontiguousarray(outs[b])
    return np.stack(outs, axis=0).astype(np.float32)
```

```python
import contextlib
import numpy as np
import concourse.bass as bass
import concourse.mybir as mybir
from concourse.bass_utils import run_bass_kernel_spmd

F32 = mybir.dt.float32
BF16 = mybir.dt.bfloat16
I32 = mybir.dt.int32
ALU = mybir.AluOpType
AF = mybir.ActivationFunctionType
AX = mybir.AxisListType

PE, ACT, DVE, POOL, SP = "tensor", "scalar", "vector", "gpsimd", "sync"
ENGS = (PE, ACT, DVE, POOL, SP)

D = 1024
DIN = 2372
DFF = 4096
TOPK_MAX = 256
EPS = 1e-5
BIG = 30000.0
NBIS = 12


class Res:
    __slots__ = ("name", "last_w", "readers")

    def __init__(self, name):
        self.name = name
        self.last_w = None
        self.readers = []


class Op:
    __slots__ = ("eng", "fn", "deps", "is_dma", "sem_key", "sem_val", "seq", "needed")

    def __init__(self, eng, fn, is_dma=False, sem_key=None):
        self.eng = eng
        self.fn = fn
        self.deps = []
        self.is_dma = is_dma
        self.sem_key = sem_key
        self.sem_val = 0
        self.seq = 0
        self.needed = False


class _Rec:
    def __init__(self):
        self.call = None

    def __getattr__(self, name):
        def f(*a, **k):
            self.call = (name, a, k)
            return self
        return f


class Sched:
    def __init__(self, nc):
        self.nc = nc
        self.ops = []
        self.dma_counts = {}
        self.last_on = {}
        self.barrier_deps = []
        self.after_barrier = set()
        self.max_stage = 99
        self.cur_stage = 0

    def stage(self, k):
        self.cur_stage = k

    def _dep(self, op, d):
        if d is None or d is op:
            return
        if d.eng == PE and op.eng == PE and not d.is_dma and not op.is_dma:
            return
        op.deps.append(d)
        d.needed = True

    def add(self, eng, fn, reads=(), writes=(), is_dma=False, sem_key=None, group=False):
        if self.cur_stage > self.max_stage:
            return None
        rec = _Rec()
        fn(rec)
        op = Op(eng, rec.call, is_dma, sem_key)
        if is_dma:
            c = self.dma_counts.get(sem_key, 0) + 16
            self.dma_counts[sem_key] = c
            op.sem_val = c
        if self.barrier_deps and eng not in self.after_barrier:
            self.after_barrier.add(eng)
            for d in self.barrier_deps:
                self._dep(op, d)
        for r in reads:
            self._dep(op, r.last_w)
        for w in writes:
            lw = w.last_w
            if not (group and lw is not None and lw.is_dma and lw.sem_key == sem_key and lw.eng == eng):
                self._dep(op, lw)
            for rd in w.readers:
                self._dep(op, rd)
        for r in reads:
            r.readers.append(op)
            if len(r.readers) > 24:
                seen = {}
                for o in r.readers:
                    seen[(o.eng, o.is_dma, o.sem_key)] = o
                r.readers = list(seen.values())
        for w in writes:
            w.last_w = op
            w.readers = []
        self.ops.append(op)
        self.last_on[(eng, is_dma, sem_key if is_dma else None)] = op
        return op

    def barrier(self):
        self.barrier_deps = list(self.last_on.values())
        self.after_barrier = set()

    def emit(self, final_waits=()):
        nc = self.nc
        cnt = {e: 0 for e in ENGS}
        for op in self.ops:
            if not op.is_dma and op.needed:
                cnt[op.eng] += 1
                op.seq = cnt[op.eng]
        with contextlib.ExitStack() as st:
            esem = {e: st.enter_context(nc.semaphore("s_" + e)) for e in ENGS}
            dsem = {k: st.enter_context(nc.semaphore("d_%s" % (k,))) for k in self.dma_counts}
            block = st.enter_context(nc.Block())
            per_eng = {e: [o for o in self.ops if o.eng == e] for e in ENGS}

            def run(engname, eng):
                waited = {}
                for op in per_eng[engname]:
                    need = {}
                    for d in op.deps:
                        if d.is_dma:
                            key, val = ("d", d.sem_key), d.sem_val
                        else:
                            key, val = ("e", d.eng), d.seq
                        if need.get(key, 0) < val:
                            need[key] = val
                    todo = []
                    for key, val in need.items():
                        if waited.get(key, 0) >= val:
                            continue
                        waited[key] = val
                        todo.append((dsem[key[1]] if key[0] == "d" else esem[key[1]], val))
                    name_, a_, k_ = op.fn
                    single = (not op.is_dma) and k_.get("accum_out", None) is None
                    fused = todo.pop() if (single and todo) else None
                    for sem, val in todo:
                        eng.wait_ge(sem, val)
                    ins = getattr(eng, name_)(*a_, **k_)
                    if fused is not None:
                        ins._wait_ge(fused[0], fused[1])
                    if op.is_dma:
                        ins.then_inc(dsem[op.sem_key], 16)
                    elif op.needed:
                        ins.then_inc(esem[op.eng], 1)
                if engname == SP:
                    for k in final_waits:
                        if k in dsem:
                            eng.wait_ge(dsem[k], self.dma_counts[k])

            @block.tensor
            def _(e):
                run(PE, e)

            @block.scalar
            def _(e):
                run(ACT, e)

            @block.vector
            def _(e):
                run(DVE, e)

            @block.gpsimd
            def _(e):
                run(POOL, e)

            @block.sync
            def _(e):
                run(SP, e)


class Arena:
    def __init__(self, ap, nwords):
        self.ap = ap
        self.n = nwords
        self.off = 0

    def reset(self, off=0):
        self.off = off

    def take(self, shape, dt):
        n = 1
        for s in shape:
            n *= s
        words = n if dt == F32 or dt == I32 else (n + 1) // 2
        words = (words + 1) // 2 * 2
        assert self.off + words <= self.n, ("arena overflow", self.off, words, self.n)
        v = self.ap[:, self.off:self.off + words]
        self.off += words
        if dt != F32:
            v = v.bitcast(dt)
        v = v[:, 0:n]
        if len(shape) == 2:
            v = v.rearrange("p (a b) -> p a b", b=shape[1])
        elif len(shape) == 3:
            v = v.rearrange("p (a b c) -> p a b c", b=shape[1], c=shape[2])
        elif len(shape) == 4:
            v = v.rearrange("p (a b c d) -> p a b c d", b=shape[1], c=shape[2], d=shape[3])
        return v


def build_program(NT=16, L=4, taps=(), max_stage=99, l0=0, l1=None, final=True):
    T = NT * 128
    TOPK = min(TOPK_MAX, T // 4)
    if l1 is None:
        l1 = L
    nc = bass.Bass("TRN2", target_bir_lowering=False)
    dr = {}

    def din(name, shape, dt=F32):
        dr[name] = nc.dram_tensor(name, shape, dt, kind="ExternalInput").ap()
        return dr[name]

    x_d = din("x", [T, D])
    pos_d = din("positions", [NT, 128], I32)
    n1_d = din("norm1_g", [L, D])
    win_d = din("w_in", [L, D, DIN])
    pw_d = din("pool_w", [L, 4, 64, 64])
    psc_d = din("pool_scale", [L, 256])
    lbl_d = din("lb_logits", [L, 256])
    hng_d = din("hgrn_norm_g", [L, 256])
    wout_d = din("w_out", [L, D, D])
    n2_d = din("norm2_g", [L, D])
    w1_d = din("w_ff_in", [L, D, DFF])
    w2_d = din("w_ff_out", [L, DFF, D])
    fg_d = din("final_norm_g", [D])
    out_d = nc.dram_tensor("out", [T, D], F32, kind="ExternalOutput").ap()
    tap_d = {}
    for (tname, tshape) in taps:
        tap_d[tname] = nc.dram_tensor("tap_" + tname, tshape, F32, kind="ExternalOutput").ap()

    S = Sched(nc)
    S.max_stage = max_stage
    with contextlib.ExitStack() as st:
        def sb(name, shape, dt=F32):
            return st.enter_context(nc.sbuf_tensor(name, shape, dt))

        x_sb = sb("x_sb", [128, NT, D])
        xR = [Res("x%d" % i) for i in range(NT)]
        gB = sb("gB", [128, D])
        gBR = Res("gB")
        ident_f = sb("ident_f", [128, 128])
        ident_b = sb("ident_b", [128, 128], BF16)
        identBIG4 = sb("identBIG4", [128, 4, 128], BF16)
        ltri = sb("ltri", [128, 128])
        lrem = sb("lrem", [128, 128])
        cmask = sb("cmask", [128, 128])
        chunkind = sb("chunkind", [128, 2])
        pow2tab = sb("pow2tab", [128, NBIS])
        corr = sb("corr", [128, 2, 16])
        cs_sb = sb("cs_sb", [128, NT, 8])
        sn_sb = sb("sn_sb", [128, NT, 8])
        poolW = sb("poolW", [128, L, 2, 128], BF16)
        pscol = sb("pscol", [128, L, 2])
        hngcol = sb("hngcol", [128, L, 2])
        pB = sb("pB", [128, L, 256])
        lbB = sb("lbB", [128, 256])
        omlbB = sb("omlbB", [128, 256])
        cR = Res("consts")
        pwR = Res("poolW"); colR = Res("cols"); pBR = Res("pB"); ropeR = Res("rope")
        lbR = Res("lb")

        arenaA_t = sb("arenaA", [128, 9728])
        AA = Arena(arenaA_t, 9728)

        def psb(name, shape):
            return st.enter_context(nc.psum_tensor(name, shape, F32))
        B0 = psb("B0", [128, 512]); B1 = psb("B1", [128, 512])
        B23 = psb("B23", [128, 1024])
        B4 = psb("B4", [128, 512]); B5 = psb("B5", [128, 512])
        B6 = psb("B6", [128, 512]); B7 = psb("B7", [128, 512])
        B0, B1, B23, B4, B5, B6, B7 = [t_[:, :] for t_ in (B0, B1, B23, B4, B5, B6, B7)]
        B2 = B23[:, 0:512]; B3 = B23[:, 512:1024]
        R0, R1, R23, R4, R5, R6, R7 = [Res("B%d" % i) for i in (0, 1, 23, 4, 5, 6, 7)]

        def bf(ap_f32):
            return ap_f32.bitcast(BF16)

        nB = int(nc.sbuf_bytes_remaining) // 4 - 16
        arenaB_t = sb("arenaB", [128, nB])
        AB = Arena(arenaB_t, nB)

        def dve(fn, r=(), w=()): return S.add(DVE, fn, r, w)
        def act(fn, r=(), w=()): return S.add(ACT, fn, r, w)
        def pool(fn, r=(), w=()): return S.add(POOL, fn, r, w)
        def pe(fn, r=(), w=()): return S.add(PE, fn, r, w)
        def dma(q, fn, r=(), w=(), key=None, group=True): return S.add(q, fn, r, w, is_dma=True, sem_key=key, group=group)

        def tap(name, ap, res):
            if name in tap_d:
                dma(POOL, lambda e: e.dma_start(out=tap_d[name], in_=ap), r=res, w=(), key="tap")

        pool(lambda e: e.memset(ident_f[:], 0.0), w=[cR])
        pool(lambda e: e.affine_select(out=ident_f[:], in_=ident_f[:], pattern=[[-1, 128]], compare_op=ALU.not_equal,
                                       fill=1.0, base=0, channel_multiplier=1), r=[cR], w=[cR])
        pool(lambda e: e.tensor_copy(out=ident_b[:], in_=ident_f[:]), r=[cR], w=[cR])
        for h4 in range(4):
            pool(lambda e, h4=h4: e.tensor_scalar(out=identBIG4[:, h4, :], in0=ident_f[:], scalar1=BIG, scalar2=None,
                                                   op0=ALU.mult), r=[cR], w=[cR])
        pool(lambda e: e.memset(ltri[:], 1.0), w=[cR])
        pool(lambda e: e.affine_select(out=ltri[:], in_=ltri[:], pattern=[[1, 128]], compare_op=ALU.is_ge, fill=0.0,
                                       base=0, channel_multiplier=-1), r=[cR], w=[cR])
        pool(lambda e: e.memset(ltri[0:64, 64:128], 0.0), r=[cR], w=[cR])
        pool(lambda e: e.memset(lrem[:], 1.0), w=[cR])
        pool(lambda e: e.affine_select(out=lrem[:], in_=lrem[:], pattern=[[-1, 128]], compare_op=ALU.is_gt, fill=0.0,
                                       base=0, channel_multiplier=1), r=[cR], w=[cR])
        pool(lambda e: e.memset(lrem[64:128, 0:64], 0.0), r=[cR], w=[cR])
        pool(lambda e: e.memset(cmask[:], 0.0), w=[cR])
        pool(lambda e: e.affine_select(out=cmask[:], in_=cmask[:], pattern=[[-1, 128]], compare_op=ALU.is_ge, fill=-1e30,
                                       base=0, channel_multiplier=1), r=[cR], w=[cR])
        pool(lambda e: e.memset(chunkind[:], 0.0), w=[cR])
        pool(lambda e: e.memset(chunkind[0:64, 0:1], 1.0), r=[cR], w=[cR])
        pool(lambda e: e.memset(chunkind[64:128, 1:2], 1.0), r=[cR], w=[cR])
        for k in range(NBIS):
            pool(lambda e, k=k: e.memset(pow2tab[:, k:k + 1], 2.0 ** (-k)), w=[cR])
        pool(lambda e: e.memset(corr[:], 1.0), w=[cR])
        for g, win in enumerate((2, 4, 8, 16)):
            ct, ph = g // 2, (g % 2) * 64
            for t in range(win - 1):
                pool(lambda e, ct=ct, ph=ph, t=t, win=win: e.memset(corr[ph:ph + 64, ct, t:t + 1], float(win) / (t + 1)),
                     r=[cR], w=[cR])

        pool(lambda e: e.memset(poolW[:], 0.0), w=[pwR])
        for l in range(L):
            for g in range(4):
                ct, ph = g // 2, (g % 2) * 64
                dma(POOL, lambda e, l=l, g=g, ct=ct, ph=ph: e.dma_start(out=poolW[ph:ph + 64, l, ct, ph:ph + 64], in_=pw_d[l, g]),
                    w=[pwR], key="c_pw")
        dma(SP, lambda e: e.dma_start(out=pscol[:], in_=psc_d.rearrange("l (c p) -> p l c", p=128), allow_slow_non_contiguous=True), w=[colR], key="c_col")
        dma(SP, lambda e: e.dma_start(out=hngcol[:], in_=hng_d.rearrange("l (c p) -> p l c", p=128), allow_slow_non_contiguous=True), w=[colR], key="c_col")
        dma(SP, lambda e: e.dma_start(out=pB[:], in_=lbl_d.rearrange("(o l) c -> o l c", o=1).to_broadcast([128, L, 256])), w=[pBR], key="c_pB")
        pos_i = AB.take([NT], I32)
        pos_f = AB.take([NT], F32)
        ang = AB.take([NT, 8], F32)
        angr = AB.take([NT, 8], F32)
        dma(SP, lambda e: e.dma_start(out=pos_i[:], in_=pos_d.rearrange("i p -> p i"), allow_slow_non_contiguous=True), w=[ropeR], key="c_pos")
        dve(lambda e: e.tensor_copy(out=pos_f[:], in_=pos_i[:]), r=[ropeR], w=[ropeR])
        for j in range(8):
            inv = float(np.float32(500000.0) ** np.float32(-(2.0 * j) / 16.0))
            dve(lambda e, j=j, inv=inv: e.tensor_scalar(out=ang[:, :, j], in0=pos_f[:], scalar1=inv, scalar2=None, op0=ALU.mult),
                r=[ropeR], w=[ropeR])
        TWO_PI = 2.0 * np.pi
        kf = AB.take([NT, 8], F32)
        ki_ = AB.take([NT, 8], I32)
        tt = AB.take([NT, 8], F32)
        for (dst, shift) in ((sn_sb, 0.0), (cs_sb, 0.5 * np.pi)):
            dve(lambda e, shift=shift: e.tensor_scalar(out=angr[:], in0=ang[:], scalar1=float(shift), scalar2=None, op0=ALU.add), r=[ropeR], w=[ropeR])
            dve(lambda e: e.tensor_scalar(out=kf[:], in0=angr[:], scalar1=float(1.0 / TWO_PI), scalar2=None, op0=ALU.mult), r=[ropeR], w=[ropeR])
            dve(lambda e: e.tensor_copy(out=ki_[:], in_=kf[:]), r=[ropeR], w=[ropeR])
            dve(lambda e: e.tensor_copy(out=kf[:], in_=ki_[:]), r=[ropeR], w=[ropeR])
            dve(lambda e: e.scalar_tensor_tensor(out=angr[:], in0=kf[:], scalar=float(-TWO_PI), in1=angr[:], op0=ALU.mult, op1=ALU.add), r=[ropeR], w=[ropeR])
            dve(lambda e: e.tensor_scalar(out=tt[:], in0=angr[:], scalar1=float(np.pi), scalar2=float(-TWO_PI), op0=ALU.is_gt, op1=ALU.mult), r=[ropeR], w=[ropeR])
            dve(lambda e: e.tensor_tensor(out=angr[:], in0=angr[:], in1=tt[:], op=ALU.add), r=[ropeR], w=[ropeR])
            dve(lambda e: e.tensor_scalar(out=tt[:], in0=angr[:], scalar1=float(-np.pi), scalar2=float(TWO_PI), op0=ALU.is_lt, op1=ALU.mult), r=[ropeR], w=[ropeR])
            dve(lambda e: e.tensor_tensor(out=angr[:], in0=angr[:], in1=tt[:], op=ALU.add), r=[ropeR], w=[ropeR])
            dve(lambda e: e.tensor_scalar(out=angr[:], in0=angr[:], scalar1=3.141592, scalar2=-3.141592, op0=ALU.min, op1=ALU.max), r=[ropeR], w=[ropeR])
            act(lambda e, dst=dst: e.activation(out=dst[:], in_=angr[:], func=AF.Sin), r=[ropeR], w=[ropeR])
        mx = AB.take([256], F32)
        zs = AB.take([256], F32)
        dve(lambda e: e.tensor_copy(out=mx[:], in_=pB[:, 0, :]), r=[pBR], w=[pBR])
        for l in range(1, L):
            dve(lambda e, l=l: e.tensor_tensor(out=mx[:], in0=mx[:], in1=pB[:, l, :], op=ALU.max), r=[pBR], w=[pBR])
        for l in range(L):
            dve(lambda e, l=l: e.tensor_tensor(out=pB[:, l, :], in0=pB[:, l, :], in1=mx[:], op=ALU.subtract), r=[pBR], w=[pBR])
        act(lambda e: e.activation(out=pB[:], in_=pB[:], func=AF.Exp), r=[pBR], w=[pBR])
        dve(lambda e: e.tensor_copy(out=zs[:], in_=pB[:, 0, :]), r=[pBR], w=[pBR])
        for l in range(1, L):
            dve(lambda e, l=l: e.tensor_tensor(out=zs[:], in0=zs[:], in1=pB[:, l, :], op=ALU.add), r=[pBR], w=[pBR])
        dve(lambda e: e.reciprocal(out=zs[:], in_=zs[:]), r=[pBR], w=[pBR])
        for l in range(L):
            dve(lambda e, l=l: e.tensor_tensor(out=pB[:, l, :], in0=pB[:, l, :], in1=zs[:], op=ALU.mult), r=[pBR], w=[pBR])

        for i in range(NT):
            q = SP if i % 2 == 0 else ACT
            dma(q, lambda e, i=i: e.dma_start(out=x_sb[:, i, :], in_=x_d[i * 128:(i + 1) * 128, :]), w=[xR[i]], key="x%d" % i)

        def rmsnorm_to_hT(i, hT_dst, hT_res, bank, bank_res, tmp):
            st_, hn, stR, hnR = tmp
            act(lambda e: e.activation(out=hn[:], in_=x_sb[:, i, :], func=AF.Square, accum_out=st_[:, 0:1]),
                r=[xR[i]], w=[stR, hnR])
            act(lambda e: e.activation(out=st_[:, 1:2], in_=st_[:, 0:1], func=AF.Ln, scale=1.0 / D, bias=EPS), r=[stR], w=[stR])
            act(lambda e: e.activation(out=st_[:, 2:3], in_=st_[:, 1:2], func=AF.Exp, scale=-0.5), r=[stR], w=[stR])
            dve(lambda e: e.scalar_tensor_tensor(out=hn[:], in0=x_sb[:, i, :], scalar=st_[:, 2:3], in1=gB[:],
                                                 op0=ALU.mult, op1=ALU.mult), r=[xR[i], stR, gBR], w=[hnR])
            pT = bf(bank).rearrange("p (k t) -> p k t", t=128)
            for k in range(8):
                pe(lambda e, k=k: e.transpose(out=pT[:, k, :], in_=hn[:, k * 128:(k + 1) * 128], identity=ident_b[:]),
                   r=[hnR, cR], w=[bank_res])
            act(lambda e: e.activation(out=hT_dst, in_=pT[:, 0:8, :], func=AF.Copy), r=[bank_res], w=[hT_res])

        for l in range(l0):
            if l == 0:
                dve(lambda e: e.memset(lbB[:], 0.0), w=[lbR])
            else:
                dve(lambda e, l=l: e.tensor_tensor(out=lbB[:], in0=lbB[:], in1=pB[:, l, :], op=ALU.add), r=[pBR, lbR], w=[lbR])
        for l in range(l0, l1):
            S.stage(0)
            S.barrier()
            AA.reset(); AB.reset()
            w_in = AA.take([8, DIN], BF16)
            winR = Res("w_in")
            w_out = AB.take([8, D], BF16)
            woutR = Res("w_out")
            wsrc = win_d[l].rearrange("(c p) n -> p c n", p=128)
            for k in range(8):
                dma(POOL, lambda e, k=k: e.dma_start(out=w_in[:, k, 0:1920], in_=wsrc[:, k, 0:1920]), w=[winR], key="win")
            dma(POOL, lambda e: e.dma_start(out=w_in[:, :, 1920:2240], in_=wsrc[:, :, 2048:2368]), w=[winR], key="win")
            dma(POOL, lambda e: e.dma_start(out=w_in[:, :, 2240:2368], in_=wsrc[:, :, 1920:2048]), w=[winR], key="win")
            dma(POOL, lambda e: e.dma_start(out=w_in[:, :, 2368:2372], in_=wsrc[:, :, 2368:2372]), w=[winR], key="win")
            wosrc = wout_d[l].rearrange("(c p) n -> p c n", p=128)
            for k in range(8):
                dma(POOL, lambda e, k=k: e.dma_start(out=w_out[:, k, :], in_=wosrc[:, k, :]), w=[woutR], key="wout")
            dma(SP, lambda e: e.dma_start(out=gB[:], in_=n1_d[l:l + 1, :].to_broadcast([128, D])), w=[gBR], key="g")
            if l == 0:
                dve(lambda e: e.memset(lbB[:], 0.0), w=[lbR])
            else:
                dve(lambda e, l=l: e.tensor_tensor(out=lbB[:], in0=lbB[:], in1=pB[:, l, :], op=ALU.add), r=[pBR, lbR], w=[lbR])
            dve(lambda e: e.tensor_scalar(out=omlbB[:], in0=lbB[:], scalar1=-1.0, scalar2=1.0, op0=ALU.mult, op1=ALU.add),
                r=[lbR], w=[lbR])

            KT = AB.take([T], BF16); KTR = Res("KT")
            Vs = AB.take([NT, 2, 65], BF16); VR = Res("V")
            kiT = AB.take([T // 2], F32); kiTR = Res("kiT")
            I_sb = AB.take([T], F32); IR = Res("I")
            Mb = AB.take([T], BF16); MbR = Res("Mb")
            Rh = [AB.take([512], F32) for _ in range(2)]; RhR = [Res("Rh0"), Res("Rh1")]
            PT = [AB.take([512], BF16) for _ in range(3)]; PTR = [Res("PT%d" % j) for j in range(3)]
            st_ = AB.take([8], F32); stR = Res("st")
            hn = AB.take([D], BF16); hnR = Res("hn")
            hT = AB.take([8, 128], BF16); hTR = Res("hT")
            mixT = AB.take([8, 128], BF16); mixR = Res("mixT")
            uext = [AB.take([2, 144], F32) for _ in range(2)]; uR = [Res("u0"), Res("u1")]
            slv = [AB.take([2, 144], F32) for _ in range(4)]; sR = Res("slv")
            pooled = AB.take([2, 128], BF16); pooledR = Res("pooled")
            H = {n: AB.take([256], F32) for n in ("t0", "t1", "f", "kk", "eb", "enb", "ebl", "gate")}
            HR = {n: Res("h_" + n) for n in H}
            H["logf"] = H["f"]; HR["logf"] = HR["f"]
            H["q"] = H["t1"]; HR["q"] = HR["t1"]
            qt_b = AB.take([256], BF16); kt_b = AB.take([256], BF16)
            khA = AB.take([256], BF16); khB = AB.take([256], BF16); v_b = AB.take([256], BF16)
            hbR = Res("h_bf"); qtR = Res("h_qt"); ktR = Res("h_kt"); vbR = Res("h_v")
            qTf = AB.take([4, 128], BF16); qTA = AB.take([4, 128], BF16); qTB = AB.take([4, 128], BF16); kTs = AB.take([4, 128], BF16)
            hTR2 = Res("h_T")
            Am = AB.take([4, 128], BF16); AmR = Res("Am")
            dec = AB.take([8], F32); decR = Res("dec")
            S32 = AB.take([4, 64], F32); Sbf = [AB.take([4, 64], BF16) for _ in range(2)]; Stmp = AB.take([4, 64], F32)
            SR = Res("S")
            hst = AB.take([16], F32)
            osb = H["enb"]; osq = H["t0"]; og = H["eb"]
            oR = Res("o")
            qk_b = AB.take([10, 64], BF16); qiki = AB.take([5, 64], F32)
            qk_flat = qk_b.rearrange("p h d -> p (h d)")
            rt = [AB.take([15, 8], F32) for _ in range(4)]
            ws = AB.take([4], F32)
            aR = Res("attn_tm")
            qidup = AB.take([4, 128], F32); kidup = AB.take([128], F32)
            qT_sb = AB.take([4, 128], BF16); qiT_sb = AB.take([4, 128], F32)
            aTR = Res("attn_T")
            bis = AB.take([32], F32); bisR = Res("bis")
            stepcol = AB.take([NBIS], F32)
            rden = AB.take([8], F32); ya = hn[:, 0:512]; yaR = hnR
            pool(lambda e: e.memset(uext[0][:, :, 0:16], 0.0), w=[uR[0]])
            pool(lambda e: e.memset(S32[0:64], 0.0), w=[SR])
            pool(lambda e: e.memset(Sbf[1][0:64], 0.0), w=[SR])
            pool(lambda e: e.memset(qTA[0:64], 0.0), w=[hTR2])
            pool(lambda e: e.memset(qTB[0:64], 0.0), w=[hTR2])
            pool(lambda e: e.memset(khA[64:128, :], 0.0), w=[hbR])
            pool(lambda e: e.memset(khB[0:64, :], 0.0), w=[hbR])
            pool(lambda e: e.memset(Vs[:, :, :, 64:65], 1.0), w=[VR])

            for i in range(NT):
                T0 = i * 128
                S.stage(1)
                rmsnorm_to_hT(i, hT[:], hTR, B0, R0, (st_, hn, stR, hnR))

                S.stage(1.5)
                pu = B1[:, 0:256].rearrange("p (c t) -> p c t", t=128)
                for ct in range(2):
                    for k in range(8):
                        pe(lambda e, ct=ct, k=k: e.matmul(pu[:, ct, :], lhsT=w_in[:, k, ct * 128:(ct + 1) * 128], rhs=hT[:, k, :],
                                                           start=(k == 0), stop=(k == 7)), r=[winR, hTR], w=[R1])
                ue = uext[i % 2]; un = uext[(i + 1) % 2]
                act(lambda e, ue=ue: e.activation(out=ue[:, :, 16:144], in_=pu, func=AF.Copy), r=[R1], w=[uR[i % 2]])
                dve(lambda e, ue=ue, un=un: e.tensor_copy(out=un[:, :, 0:16], in_=ue[:, :, 128:144]), r=[uR[i % 2]], w=[uR[(i + 1) % 2]])
                prev = ue
                S.stage(1.6)
                for lv, sh in enumerate((1, 2, 4, 8)):
                    lo = 2 * sh - 1
                    dve(lambda e, lv=lv, sh=sh, lo=lo, prev=prev: e.tensor_tensor(out=slv[lv][:, :, lo:144], in0=prev[:, :, lo:144],
                                                                                   in1=prev[:, :, lo - sh:144 - sh], op=ALU.add),
                         r=[uR[i % 2], sR], w=[sR])
                    prev = slv[lv]
                S.stage(1.7)
                for g, win in enumerate((2, 4, 8, 16)):
                    ct, ph = g // 2, (g % 2) * 64
                    if i == 0:
                        dve(lambda e, g=g, ct=ct, ph=ph: e.tensor_tensor(out=slv[g][ph:ph + 64, ct, 16:32], in0=slv[g][ph:ph + 64, ct, 16:32],
                                                                          in1=corr[ph:ph + 64, ct, :], op=ALU.mult), r=[sR, cR], w=[sR])
                    dve(lambda e, g=g, ct=ct, ph=ph, win=win, ue=ue: e.scalar_tensor_tensor(
                        out=pooled[ph:ph + 64, ct, :], in0=slv[g][ph:ph + 64, ct, 16:144], scalar=1.0 / win, in1=ue[ph:ph + 64, ct, 16:144],
                        op0=ALU.mult, op1=ALU.subtract), r=[sR, uR[i % 2]], w=[pooledR])
                S.stage(1.8)
                py = B1[:, 256:512].rearrange("p (c t) -> p c t", t=128)
                for ct in range(2):
                    pe(lambda e, ct=ct: e.matmul(py[:, ct, :], lhsT=poolW[:, l, ct, :], rhs=pooled[:, ct, :], start=True, stop=True),
                       r=[pwR, pooledR], w=[R1])
                S.stage(1.9)
                for ct in range(2):
                    act(lambda e, ct=ct: e.activation(out=mixT[:, ct, :], in_=py[:, ct, :], func=AF.Identity, scale=pscol[:, l, ct:ct + 1]),
                        r=[R1, colR], w=[mixR])

                S.stage(2)
                for j, bank in enumerate((B2, B3)):
                    for k in range(8):
                        pe(lambda e, j=j, k=k, bank=bank: e.matmul(bank, lhsT=hT[:, k, :], rhs=w_in[:, k, 256 + j * 512:256 + (j + 1) * 512],
                                                                   start=(k == 0), stop=(k == 7)), r=[hTR, winR], w=[R23])
                hq_p, hf_p, hi_p, hg_p = B2[:, 0:256], B2[:, 256:512], B3[:, 0:256], B3[:, 256:512]
                act(lambda e: e.activation(out=H["t0"], in_=hf_p, func=AF.Exp, scale=-1.0), r=[R23], w=[HR["t0"]])
                act(lambda e: e.activation(out=H["t1"], in_=hq_p, func=AF.Exp, scale=-1.0), r=[R23], w=[HR["t1"]])
                act(lambda e: e.activation(out=H["gate"], in_=hg_p, func=AF.Exp, scale=-1.0), r=[R23], w=[HR["gate"]])
                act(lambda e: e.activation(out=v_b, in_=hi_p, func=AF.Copy), r=[R23], w=[vbR])
                dve(lambda e: e.tensor_scalar(out=H["t0"], in0=H["t0"], scalar1=1.0, scalar2=None, op0=ALU.add), r=[HR["t0"]], w=[HR["t0"]])
                dve(lambda e: e.reciprocal(out=H["t0"], in_=H["t0"]), r=[HR["t0"]], w=[HR["t0"]])
                dve(lambda e: e.tensor_tensor(out=H["f"], in0=H["t0"], in1=omlbB[:], op=ALU.mult), r=[HR["t0"], lbR], w=[HR["f"]])
                dve(lambda e: e.tensor_tensor(out=H["f"], in0=H["f"], in1=lbB[:], op=ALU.add), r=[HR["f"], lbR], w=[HR["f"]])
                dve(lambda e: e.tensor_scalar(out=H["kk"], in0=H["f"], scalar1=-1.0, scalar2=1.0, op0=ALU.mult, op1=ALU.add),
                     r=[HR["f"]], w=[HR["kk"]])
                dve(lambda e: e.tensor_scalar(out=H["f"], in0=H["f"], scalar1=1e-30, scalar2=None, op0=ALU.max), r=[HR["f"], HR["kk"]], w=[HR["f"]])
                act(lambda e: e.activation(out=H["logf"], in_=H["f"], func=AF.Ln), r=[HR["f"]], w=[HR["logf"]])
                pe(lambda e: e.matmul(B4[:, 0:256], lhsT=ltri[:], rhs=H["logf"], start=True, stop=True), r=[cR, HR["logf"]], w=[R4])
                pe(lambda e: e.matmul(B4[:, 256:512], lhsT=lrem[:], rhs=H["logf"], start=True, stop=True), r=[cR, HR["logf"]], w=[R4])
                dl = B0[0:64, 0:8].rearrange("p (h c) -> p h c", c=2)
                for h in range(4):
                    pe(lambda e, h=h: e.matmul(dl[:, h, :], lhsT=H["logf"][:, h * 64:(h + 1) * 64], rhs=chunkind[:], start=True, stop=True),
                       r=[cR, HR["logf"]], w=[R0])
                act(lambda e: e.activation(out=H["eb"], in_=B4[:, 0:256], func=AF.Exp), r=[R4], w=[HR["eb"]])
                act(lambda e: e.activation(out=H["enb"], in_=B4[:, 0:256], func=AF.Exp, scale=-1.0), r=[R4], w=[HR["enb"]])
                act(lambda e: e.activation(out=H["ebl"], in_=B4[:, 256:512], func=AF.Exp), r=[R4], w=[HR["ebl"]])
                act(lambda e: e.activation(out=dec[0:64].rearrange("p (h c) -> p h c", c=2), in_=dl, func=AF.Exp), r=[R0], w=[decR])
                dve(lambda e: e.tensor_scalar(out=H["t1"], in0=H["t1"], scalar1=1.0, scalar2=None, op0=ALU.add), r=[HR["t1"]], w=[HR["t1"]])
                dve(lambda e: e.reciprocal(out=H["t1"], in_=H["t1"]), r=[HR["t1"]], w=[HR["t1"]])
                dve(lambda e: e.tensor_tensor(out=H["q"], in0=hq_p, in1=H["t1"], op=ALU.mult), r=[R23, HR["t1"]], w=[HR["q"]])
                dve(lambda e: e.tensor_scalar(out=H["gate"], in0=H["gate"], scalar1=1.0, scalar2=None, op0=ALU.add), r=[HR["gate"]], w=[HR["gate"]])
                dve(lambda e: e.reciprocal(out=H["gate"], in_=H["gate"]), r=[HR["gate"]], w=[HR["gate"]])
                dve(lambda e: e.tensor_tensor(out=H["gate"], in0=hg_p, in1=H["gate"], op=ALU.mult), r=[R23, HR["gate"]], w=[HR["gate"]])
                dve(lambda e: e.tensor_tensor(out=qt_b, in0=H["q"], in1=H["eb"], op=ALU.mult), r=[HR["q"], HR["eb"]], w=[qtR])
                dve(lambda e: e.tensor_tensor(out=kt_b, in0=H["kk"], in1=H["enb"], op=ALU.mult), r=[HR["kk"], HR["enb"]], w=[ktR])
                dve(lambda e: e.tensor_tensor(out=khA[0:64, :], in0=H["kk"][0:64, :], in1=H["ebl"][0:64, :], op=ALU.mult),
                     r=[HR["kk"], HR["ebl"]], w=[hbR])
                dve(lambda e: e.tensor_tensor(out=khB[64:128, :], in0=H["kk"][64:128, :], in1=H["ebl"][64:128, :], op=ALU.mult),
                     r=[HR["kk"], HR["ebl"]], w=[hbR])
                tq = bf(B5)[0:64, 0:512].rearrange("p (h t) -> p h t", t=128)
                tk = bf(B5)[0:64, 512:1024].rearrange("p (h t) -> p h t", t=128)
                for h in range(4):
                    pe(lambda e, h=h: e.transpose(out=tq[:, h, :], in_=qt_b[:, h * 64:(h + 1) * 64], identity=ident_b[:]), r=[qtR, cR], w=[R5])
                for h in range(4):
                    pe(lambda e, h=h: e.transpose(out=tk[:, h, :], in_=kt_b[:, h * 64:(h + 1) * 64], identity=ident_b[:]), r=[ktR, cR], w=[R5])
                act(lambda e: e.activation(out=qTf[0:64], in_=tq, func=AF.Copy), r=[R5], w=[hTR2])
                dve(lambda e: e.tensor_copy(out=qTA[0:64, :, 0:64], in_=tq[:, :, 0:64]), r=[R5], w=[hTR2])
                dve(lambda e: e.tensor_copy(out=qTB[0:64, :, 64:128], in_=tq[:, :, 64:128]), r=[R5], w=[hTR2])
                act(lambda e: e.activation(out=kTs[0:64], in_=tk, func=AF.Copy), r=[R5], w=[hTR2])
                pA = B6.rearrange("p (h t) -> p h t", t=128)
                for h in range(4):
                    pe(lambda e, h=h: e.matmul(pA[:, h, :], lhsT=kTs[0:64, h, :], rhs=qTf[0:64, h, :], start=True, stop=True), r=[hTR2], w=[R6])
                dve(lambda e: e.tensor_tensor(out=Am, in0=pA, in1=ltri[:].unsqueeze(1).to_broadcast([128, 4, 128]), op=ALU.mult),
                    r=[R6, cR], w=[AmR])
                pKV = B7[0:64, :].rearrange("p (c h v) -> p c h v", c=2, h=4)
                for c, kh in enumerate((khA, khB)):
                    for h in range(4):
                        pe(lambda e, c=c, h=h, kh=kh: e.matmul(pKV[:, c, h, :], lhsT=kh[:, h * 64:(h + 1) * 64], rhs=v_b[:, h * 64:(h + 1) * 64],
                                                               start=True, stop=True), r=[hbR, vbR], w=[R7])
                decv = dec[0:64].rearrange("p (h c) -> p h c", c=2)
                for c in range(2):
                    dve(lambda e, c=c: e.tensor_tensor(out=Stmp[0:64], in0=S32[0:64], in1=decv[:, :, c:c + 1].to_broadcast([64, 4, 64]), op=ALU.mult),
                        r=[SR, decR], w=[SR])
                    dve(lambda e, c=c: e.tensor_tensor(out=S32[0:64], in0=pKV[:, c], in1=Stmp[0:64], op=ALU.add), r=[SR, R7], w=[SR])
                    if c == 0:
                        dve(lambda e: e.tensor_copy(out=Sbf[0][0:64], in_=S32[0:64]), r=[SR], w=[SR])
                        po = B4[:, 0:256].rearrange("p (h v) -> p h v", v=64)
                        for h in range(4):
                            pe(lambda e, h=h: e.matmul(po[:, h, :], lhsT=Am[:, h, :], rhs=v_b[:, h * 64:(h + 1) * 64], start=True, stop=False),
                               r=[AmR, vbR], w=[R4])
                            pe(lambda e, h=h: e.matmul(po[:, h, :], lhsT=qTA[0:64, h, :], rhs=Sbf[1][0:64, h, :], start=False, stop=False),
                               r=[hTR2, SR], w=[R4])
                            pe(lambda e, h=h: e.matmul(po[:, h, :], lhsT=qTB[0:64, h, :], rhs=Sbf[0][0:64, h, :], start=False, stop=True),
                               r=[hTR2, SR], w=[R4])
                    else:
                        dve(lambda e: e.tensor_copy(out=Sbf[1][0:64], in_=S32[0:64]), r=[SR], w=[SR])
                act(lambda e: e.activation(out=osb, in_=B4[:, 0:256], func=AF.Copy), r=[R4], w=[oR, HR["enb"]])
                dve(lambda e: e.tensor_tensor(out=osq, in0=osb, in1=osb, op=ALU.mult), r=[oR], w=[oR, HR["t0"]])
                dve(lambda e: e.tensor_reduce(out=hst[:, 0:4], in_=osq.rearrange("p (h v) -> p h v", v=64), axis=AX.X, op=ALU.add), r=[oR], w=[oR])
                act(lambda e: e.activation(out=hst[:, 4:8], in_=hst[:, 0:4], func=AF.Ln, scale=1.0 / 64, bias=EPS), r=[oR], w=[oR])
                act(lambda e: e.activation(out=hst[:, 8:12], in_=hst[:, 4:8], func=AF.Exp, scale=-0.5), r=[oR], w=[oR])
                dve(lambda e: e.tensor_tensor(out=og.rearrange("p (h v) -> p h v", v=64), in0=osb.rearrange("p (h v) -> p h v", v=64),
                                              in1=hst[:, 8:12].unsqueeze(2).to_broadcast([128, 4, 64]), op=ALU.mult), r=[oR], w=[oR, HR["eb"]])
                dve(lambda e: e.tensor_tensor(out=og, in0=og, in1=H["gate"], op=ALU.mult), r=[oR, HR["gate"]], w=[oR])
                pg = B5[:, 0:256].rearrange("p (c t) -> p c t", t=128)
                for c in range(2):
                    pe(lambda e, c=c: e.transpose(out=pg[:, c, :], in_=og[:, c * 128:(c + 1) * 128], identity=ident_f[:]), r=[oR, cR], w=[R5])
                for c in range(2):
                    act(lambda e, c=c: e.activation(out=mixT[:, 2 + c, :], in_=pg[:, c, :], func=AF.Identity, scale=hngcol[:, l, c:c + 1]),
                        r=[R5, colR], w=[mixR])
                if i == NT - 1:
                    tap("S32_l%d" % l, S32[0:64], [SR])

                S.stage(3)
                for k in range(8):
                    pe(lambda e, k=k: e.matmul(B23[:, 0:512], lhsT=hT[:, k, :], rhs=w_in[:, k, 1280:1792], start=(k == 0), stop=(k == 7)),
                       r=[hTR, winR], w=[R23])
                for k in range(8):
                    pe(lambda e, k=k: e.matmul(B23[:, 512:960], lhsT=hT[:, k, :], rhs=w_in[:, k, 1792:2240], start=(k == 0), stop=(k == 7)),
                       r=[hTR, winR], w=[R23])
                for k in range(8):
                    pe(lambda e, k=k: e.matmul(B4[:, 0:132], lhsT=hT[:, k, :], rhs=w_in[:, k, 2240:2372], start=(k == 0), stop=(k == 7)),
                       r=[hTR, winR], w=[R4])
                pv = B23[:, 0:960].rearrange("p (h d) -> p h d", d=64)
                x1, x2 = pv[:, :, 0:8], pv[:, :, 8:16]
                cB = cs_sb[:, i, :].unsqueeze(1).to_broadcast([128, 15, 8])
                sB = sn_sb[:, i, :].unsqueeze(1).to_broadcast([128, 15, 8])
                dve(lambda e: e.tensor_tensor(out=rt[0], in0=x1, in1=cB, op=ALU.mult), r=[R23, ropeR], w=[aR])
                dve(lambda e: e.tensor_tensor(out=rt[1], in0=x2, in1=sB, op=ALU.mult), r=[R23, ropeR], w=[aR])
                dve(lambda e: e.tensor_tensor(out=rt[2], in0=x2, in1=cB, op=ALU.mult), r=[R23, ropeR], w=[aR])
                dve(lambda e: e.tensor_tensor(out=rt[3], in0=x1, in1=sB, op=ALU.mult), r=[R23, ropeR], w=[aR])
                qdst = qk_b[:, 0:8, :].rearrange("p (j k) d -> p k j d", k=2)
                dve(lambda e: e.tensor_tensor(out=qdst[:, :, :, 0:8], in0=rt[0][:, 0:8, :].rearrange("p (k j) d -> p k j d", k=2),
                                               in1=rt[1][:, 0:8, :].rearrange("p (k j) d -> p k j d", k=2), op=ALU.subtract), r=[aR], w=[aR])
                dve(lambda e: e.tensor_tensor(out=qdst[:, :, :, 8:16], in0=rt[2][:, 0:8, :].rearrange("p (k j) d -> p k j d", k=2),
                                               in1=rt[3][:, 0:8, :].rearrange("p (k j) d -> p k j d", k=2), op=ALU.add), r=[aR], w=[aR])
                dve(lambda e: e.tensor_tensor(out=qk_b[:, 8:10, 0:8], in0=rt[0][:, 8:10, :], in1=rt[1][:, 8:10, :], op=ALU.subtract), r=[aR], w=[aR])
                dve(lambda e: e.tensor_tensor(out=qk_b[:, 8:10, 8:16], in0=rt[2][:, 8:10, :], in1=rt[3][:, 8:10, :], op=ALU.add), r=[aR], w=[aR])
                dve(lambda e: e.tensor_tensor(out=qiki[:, :, 0:8], in0=rt[0][:, 10:15, :], in1=rt[1][:, 10:15, :], op=ALU.subtract), r=[aR], w=[aR])
                dve(lambda e: e.tensor_tensor(out=qiki[:, :, 8:16], in0=rt[2][:, 10:15, :], in1=rt[3][:, 10:15, :], op=ALU.add), r=[aR], w=[aR])
                act(lambda e: e.activation(out=qdst[:, :, :, 16:64], in_=pv[:, 0:8, 16:64].rearrange("p (k j) d -> p k j d", k=2), func=AF.Copy),
                    r=[R23], w=[aR])
                act(lambda e: e.activation(out=qk_b[:, 8:10, 16:64], in_=pv[:, 8:10, 16:64], func=AF.Copy), r=[R23], w=[aR])
                act(lambda e: e.activation(out=qiki[:, :, 16:64], in_=pv[:, 10:15, 16:64], func=AF.Copy), r=[R23], w=[aR])
                act(lambda e: e.activation(out=Vs[:, i, :, 0:64], in_=B4[:, 0:128].rearrange("p (k d) -> p k d", d=64), func=AF.Copy), r=[R4], w=[VR])
                act(lambda e: e.activation(out=ws, in_=B4[:, 128:132], func=AF.Copy, scale=1.0 / 16.0), r=[R4], w=[aR])
                for dd in range(2):
                    dve(lambda e, dd=dd: e.tensor_copy(out=qidup[:, :, dd * 64:(dd + 1) * 64], in_=qiki[:, 0:4, :]), r=[aR], w=[aR])
                    dve(lambda e, dd=dd: e.tensor_copy(out=kidup[:, dd * 64:(dd + 1) * 64], in_=qiki[:, 4, :]), r=[aR], w=[aR])
                tqa = bf(B5)[:, 0:512].rearrange("p (j t) -> p j t", t=128)
                tka = bf(B5)[:, 512:640]
                for j in range(4):
                    pe(lambda e, j=j: e.transpose(out=tqa[:, j, :], in_=qk_flat[:, j * 128:(j + 1) * 128], identity=ident_b[:]), r=[aR, cR], w=[R5])
                pe(lambda e: e.transpose(out=tka, in_=qk_flat[:, 512:640], identity=ident_b[:]), r=[aR, cR], w=[R5])
                tqi = B6.rearrange("p (h t) -> p h t", t=128)
                for h in range(4):
                    pe(lambda e, h=h: e.transpose(out=tqi[:, h, :], in_=qidup[:, h, :], identity=ident_f[:]), r=[aR, cR], w=[R6])
                tki = B7[:, 0:128]
                pe(lambda e: e.transpose(out=tki, in_=kidup, identity=ident_f[:]), r=[aR, cR], w=[R7])
                act(lambda e: e.activation(out=qT_sb, in_=tqa, func=AF.Copy), r=[R5], w=[aTR])
                act(lambda e: e.activation(out=KT[:, T0:T0 + 128], in_=tka, func=AF.Copy), r=[R5], w=[KTR])
                dve(lambda e: e.tensor_copy(out=qiT_sb, in_=tqi), r=[R6], w=[aTR])
                half = 0 if T0 < T // 2 else 64
                kcol = T0 - (0 if half == 0 else T // 2)
                dve(lambda e, half=half, kcol=kcol: e.tensor_copy(out=kiT[half:half + 64, kcol:kcol + 128], in_=tki[half:half + 64, :]),
                    r=[R7], w=[kiTR])
                S.stage(4)
                n = T0 + 128
                nblk = (n + 511) // 512
                sbanks = ((B0, R0), (B1, R1))
                cntj = 0
                segs = []
                for hh in range(2):
                    a0, a1 = hh * (T // 2), min(n, (hh + 1) * (T // 2))
                    k0 = a0
                    while k0 < a1:
                        segs.append((k0, min(512, a1 - k0), hh * 64, k0 - a0))
                        k0 += 512
                for (k0, wdt, kh_, kc0) in segs:
                    for h in range(4):
                        bank, bres = sbanks[cntj % 2]
                        rh, rhr = Rh[cntj % 2], RhR[cntj % 2]
                        cntj += 1
                        pe(lambda e, bank=bank, h=h, kh_=kh_, kc0=kc0, wdt=wdt: e.matmul(
                            bank[:, 0:wdt], lhsT=qiT_sb[kh_:kh_ + 64, h, :], rhs=kiT[kh_:kh_ + 64, kc0:kc0 + wdt], start=True, stop=True),
                           r=[aTR, kiTR], w=[bres])
                        if h == 0:
                            dve(lambda e, bank=bank, k0=k0, wdt=wdt: e.tensor_scalar(out=I_sb[:, k0:k0 + wdt], in0=bank[:, 0:wdt], scalar1=0.0,
                                                                                     scalar2=ws[:, 0:1], op0=ALU.max, op1=ALU.mult),
                                r=[bres, aR], w=[IR])
                        else:
                            act(lambda e, bank=bank, rh=rh, wdt=wdt: e.activation(out=rh[:, 0:wdt], in_=bank[:, 0:wdt], func=AF.Relu), r=[bres], w=[rhr])
                            dve(lambda e, rh=rh, h=h, k0=k0, wdt=wdt: e.scalar_tensor_tensor(
                                out=I_sb[:, k0:k0 + wdt], in0=rh[:, 0:wdt], scalar=ws[:, h:h + 1], in1=I_sb[:, k0:k0 + wdt],
                                op0=ALU.mult, op1=ALU.add), r=[rhr, aR, IR], w=[IR])
                S.stage(5)
                if n > TOPK:
                    dve(lambda e, n=n: e.tensor_reduce(out=bis[:, 0:1], in_=I_sb[:, 0:n], axis=AX.X, op=ALU.max, apply_absolute_value=True),
                        r=[IR], w=[bisR])
                dve(lambda e: e.tensor_tensor(out=I_sb[:, T0:T0 + 128], in0=I_sb[:, T0:T0 + 128], in1=cmask[:], op=ALU.add), r=[IR, cR], w=[IR])
                thr = bis[:, 1:2]
                if n > TOPK:
                    dve(lambda e: e.tensor_scalar(out=stepcol, in0=pow2tab[:], scalar1=bis[:, 0:1], scalar2=None, op0=ALU.mult), r=[bisR, cR], w=[bisR])
                    dve(lambda e: e.memset(thr, 0.0), r=[bisR], w=[bisR])
                    for k in range(NBIS):
                        dve(lambda e, n=n: e.tensor_scalar(out=Mb[:, 0:n], in0=I_sb[:, 0:n], scalar1=thr, scalar2=None, op0=ALU.is_ge, op1=ALU.add,
                                                            accum_out=bis[:, 2:3]), r=[IR, bisR], w=[MbR, bisR])
                        dve(lambda e: e.tensor_scalar(out=bis[:, 3:4], in0=bis[:, 2:3], scalar1=float(TOPK), scalar2=0.5, op0=ALU.is_ge, op1=ALU.subtract),
                            r=[bisR], w=[bisR])
                        dve(lambda e, k=k: e.scalar_tensor_tensor(out=thr, in0=bis[:, 3:4], scalar=stepcol[:, k:k + 1], in1=thr, op0=ALU.mult, op1=ALU.add),
                            r=[bisR], w=[bisR])
                    dve(lambda e: e.scalar_tensor_tensor(out=thr, in0=stepcol[:, NBIS - 1:NBIS], scalar=-0.5, in1=thr, op0=ALU.mult, op1=ALU.add),
                        r=[bisR], w=[bisR])
                else:
                    dve(lambda e: e.memset(thr, -1e29), r=[bisR], w=[bisR])
                dve(lambda e, n=n: e.tensor_scalar(out=Mb[:, 0:n], in0=I_sb[:, 0:n], scalar1=thr, scalar2=1.0, op0=ALU.is_ge, op1=ALU.subtract),
                    r=[IR, bisR], w=[MbR])
                if i == NT - 1 and l == 0:
                    tap("I", I_sb, [IR]); tap("bis", bis, [bisR]); tap("Mb", Mb, [MbR]); tap("qT", qT_sb, [aTR]); tap("KT", KT, [KTR])
                    tap("V", Vs, [VR]); tap("ws", ws, [aR]); tap("qiT", qiT_sb, [aTR]); tap("kiT", kiT, [kiTR])
                S.stage(6)
                sT = ((B4, R4), (B5, R5))
                pOT = (B6, B7)
                pOR = (R6, R7)
                pOq = (B0[:, 0:260].rearrange("p (h v) -> p h v", v=65), B1[:, 0:260].rearrange("p (h v) -> p h v", v=65))
                pOqR = (R0, R1)
                OTs = Rh[0]; OTsR = RhR[0]
                iters = [(kv, c) for kv in range(2) for c in range(i + 1)]

                def emit_qk(idx):
                    kv, c = iters[idx]
                    bank, bres = sT[idx % 2]
                    pt, ptr = PT[idx % 3], PTR[idx % 3]
                    pe(lambda e: e.matmul(bank, lhsT=KT[kv * 64:(kv + 1) * 64, c * 128:(c + 1) * 128],
                                          rhs=qT_sb[kv * 64:(kv + 1) * 64, :, :], start=True, stop=False), r=[KTR, aTR], w=[bres])
                    pe(lambda e: e.matmul(bank, lhsT=Mb[:, c * 128:(c + 1) * 128], rhs=identBIG4[:], start=False, stop=True),
                       r=[MbR, cR], w=[bres])
                    act(lambda e: e.activation(out=pt, in_=bank, func=AF.Exp, scale=0.125), r=[bres], w=[ptr])

                def emit_pv(idx):
                    kv, c = iters[idx]
                    pt, ptr = PT[idx % 3], PTR[idx % 3]
                    pe(lambda e: e.matmul(pOT[kv][0:65, :], lhsT=Vs[:, c, kv, :], rhs=pt, start=(c == 0), stop=(c == i)),
                       r=[ptr, VR], w=[pOR[kv]])
                    if c == i:
                        act(lambda e: e.activation(out=OTs[0:65, :], in_=pOT[kv][0:65, :], func=AF.Copy), r=[pOR[kv]], w=[OTsR])
                        for h4 in range(4):
                            pe(lambda e, h4=h4: e.transpose(out=pOq[kv][:, h4, :], in_=OTs[0:65, h4 * 128:(h4 + 1) * 128], identity=ident_f[0:65, 0:65]),
                               r=[OTsR, cR], w=[pOqR[kv]])
                        dve(lambda e: e.reciprocal(out=rden[:, kv * 4:(kv + 1) * 4], in_=pOq[kv][:, :, 64]), r=[pOqR[kv]], w=[yaR])
                        dve(lambda e: e.tensor_tensor(out=ya[:, kv * 256:(kv + 1) * 256].rearrange("p (h v) -> p h v", v=64), in0=pOq[kv][:, :, 0:64],
                                                      in1=rden[:, kv * 4:(kv + 1) * 4].unsqueeze(2).to_broadcast([128, 4, 64]), op=ALU.mult),
                            r=[pOqR[kv], yaR], w=[yaR])

                for idx in range(len(iters)):
                    emit_qk(idx)
                    if idx >= 1:
                        emit_pv(idx - 1)
                emit_pv(len(iters) - 1)
                if i == NT - 1 and l == 0:
                    tap("ya", ya, [yaR]); tap("rden", rden, [yaR])
                ty = bf(B0)[:, 0:512].rearrange("p (c t) -> p c t", t=128)
                for c in range(4):
                    pe(lambda e, c=c: e.transpose(out=ty[:, c, :], in_=ya[:, c * 128:(c + 1) * 128], identity=ident_b[:]), r=[yaR, cR], w=[R0])
                act(lambda e: e.activation(out=mixT[:, 4:8, :], in_=ty, func=AF.Copy), r=[R0], w=[mixR])
                S.stage(1)
                if ("mixT_l%d" % l) in tap_d:
                    dma(POOL, lambda e, i=i, l=l: e.dma_start(out=tap_d["mixT_l%d" % l][:, :, i * 128:(i + 1) * 128], in_=mixT), r=[mixR], key="tap")

                S.stage(7)
                for hf in range(2):
                    for k in range(8):
                        pe(lambda e, hf=hf, k=k: e.matmul(B23[:, hf * 512:(hf + 1) * 512], lhsT=mixT[:, k, :], rhs=w_out[:, k, hf * 512:(hf + 1) * 512],
                                                          start=(k == 0), stop=(k == 7)), r=[mixR, woutR], w=[R23])
                for hf in range(2):
                    dve(lambda e, hf=hf, i=i: e.tensor_tensor(out=x_sb[:, i, hf * 512:(hf + 1) * 512], in0=B23[:, hf * 512:(hf + 1) * 512],
                                                               in1=x_sb[:, i, hf * 512:(hf + 1) * 512], op=ALU.add), r=[R23, xR[i]], w=[xR[i]])

            if ("xmid_l%d" % l) in tap_d:
                for i in range(NT):
                    dma(SP, lambda e, i=i, l=l: e.dma_start(out=tap_d["xmid_l%d" % l][i * 128:(i + 1) * 128, :], in_=x_sb[:, i, :]), r=[xR[i]], key="tap")

            S.stage(8)
            S.barrier()
            AA.reset(); AB.reset()
            h2T = AA.take([8, T], BF16); h2R = [Res("h2T%d" % i) for i in range(NT)]
            NCH = 8
            W1 = [AB.take([8, 512], BF16) for _ in range(2)]; W1R = [Res("W1_0"), Res("W1_1")]
            W2 = [AB.take([4, D], BF16) for _ in range(2)]; W2R = [Res("W2_0"), Res("W2_1")]
            aT = [AB.take([4, 512], BF16) for _ in range(2)]; aTRs = [[Res("aT%d_%d" % (q_, j_)) for j_ in range(4)] for q_ in range(2)]
            r32 = [AB.take([512], F32) for _ in range(4)]; r32R = [Res("r32_%d" % q_) for q_ in range(4)]
            st2 = AB.take([8], F32); st2R = Res("st2")
            hn2 = AB.take([D], BF16); hn2R = Res("hn2")
            dma(SP, lambda e: e.dma_start(out=gB[:], in_=n2_d[l:l + 1, :].to_broadcast([128, D])), w=[gBR], key="g")
            w1src = w1_d[l].rearrange("(c p) n -> p c n", p=128)
            w2src = w2_d[l].rearrange("(c p) n -> p c n", p=128)

            def load_chunk(c):
                s = c % 2
                for k in range(8):
                    dma(POOL, lambda e, k=k, s=s, c=c: e.dma_start(out=W1[s][:, k, :], in_=w1src[:, k, c * 512:(c + 1) * 512]), w=[W1R[s]], key="w1_%d" % s)
                for j in range(4):
                    dma(POOL, lambda e, j=j, s=s, c=c: e.dma_start(out=W2[s][:, j, :], in_=w2src[:, c * 4 + j, :]), w=[W2R[s]], key="w2_%d" % s)

            load_chunk(0)
            load_chunk(1)
            nbanks = ((B0, R0), (B1, R1))
            for i in range(NT):
                bank, bres = nbanks[i % 2]
                rmsnorm_to_hT(i, h2T[:, :, i * 128:(i + 1) * 128], h2R[i], bank, bres, (st2, hn2, st2R, hn2R))
            abanks = ((B0, R0), (B1, R1), (B23[:, 0:512], R23))
            ybanks = ((B4, R4), (B5, R5), (B6, R6), (B7, R7))
            aj = 0
            yj = 0
            NG = NT // 4 if NT >= 4 else 1
            GT = NT // NG
            for c in range(NCH):
                s = c % 2
                for tg in range(NG):
                    a_t, a_r = aT[(c * NG + tg) % 2], aTRs[(c * NG + tg) % 2]
                    ntok = GT * 128
                    for j in range(4):
                        bank, bres = abanks[aj % 3]
                        aj += 1
                        for k in range(8):
                            pe(lambda e, bank=bank, j=j, k=k, s=s, tg=tg, ntok=ntok: e.matmul(
                                bank[:, 0:ntok], lhsT=W1[s][:, k, j * 128:(j + 1) * 128], rhs=h2T[:, k, tg * ntok:(tg + 1) * ntok],
                                start=(k == 0), stop=(k == 7)), r=[W1R[s]] + h2R[tg * GT:(tg + 1) * GT], w=[bres])
                        rr, rrR = r32[aj % 4], r32R[aj % 4]
                        act(lambda e, bank=bank, rr=rr, ntok=ntok: e.activation(out=rr[:, 0:ntok], in_=bank[:, 0:ntok], func=AF.Relu),
                            r=[bres], w=[rrR])
                        sq_eng = DVE
                        S.add(sq_eng, lambda e, j=j, a_t=a_t, ntok=ntok, rr=rr: e.tensor_tensor(out=a_t[:, j, 0:ntok], in0=rr[:, 0:ntok], in1=rr[:, 0:ntok], op=ALU.mult),
                              [rrR], [a_r[j]])
                    for t4 in range(GT):
                        ti = tg * GT + t4
                        for hf in range(2):
                            bank, bres = ybanks[yj % 4]
                            yj += 1
                            for j in range(4):
                                pe(lambda e, bank=bank, j=j, t4=t4, hf=hf, s=s, a_t=a_t: e.matmul(
                                    bank, lhsT=a_t[:, j, t4 * 128:(t4 + 1) * 128], rhs=W2[s][:, j, hf * 512:(hf + 1) * 512], start=(j == 0), stop=(j == 3)),
                                   r=[a_r[j], W2R[s]], w=[bres])
                            dve(lambda e, bank=bank, ti=ti, hf=hf: e.tensor_tensor(out=x_sb[:, ti, hf * 512:(hf + 1) * 512], in0=bank,
                                                                                    in1=x_sb[:, ti, hf * 512:(hf + 1) * 512], op=ALU.add),
                                r=[bres, xR[ti]], w=[xR[ti]])
                if c + 2 < NCH:
                    load_chunk(c + 2)
            if ("xout_l%d" % l) in tap_d:
                for i in range(NT):
                    dma(SP, lambda e, i=i, l=l: e.dma_start(out=tap_d["xout_l%d" % l][i * 128:(i + 1) * 128, :], in_=x_sb[:, i, :]), r=[xR[i]], key="tap")

        S.stage(0)
        S.barrier()
        AB.reset()
        if not final:
            for i in range(NT):
                dma(SP, lambda e, i=i: e.dma_start(out=out_d[i * 128:(i + 1) * 128, :], in_=x_sb[:, i, :]), r=[xR[i]], key="out")
        else:
            dma(SP, lambda e: e.dma_start(out=gB[:], in_=fg_d.rearrange("(o d) -> o d", o=1).to_broadcast([128, D])), w=[gBR], key="g")
            fo = [AB.take([D], F32) for _ in range(2)]; foR = [Res("fo0"), Res("fo1")]
            fj = AB.take([D], BF16); fjR = Res("fj")
            fst = AB.take([8], F32); fstR = Res("fst")
            for i in range(NT):
                o_, oR_ = fo[i % 2], foR[i % 2]
                act(lambda e, i=i: e.activation(out=fj, in_=x_sb[:, i, :], func=AF.Square, accum_out=fst[:, 0:1]), r=[xR[i]], w=[fstR, fjR])
                act(lambda e: e.activation(out=fst[:, 1:2], in_=fst[:, 0:1], func=AF.Ln, scale=1.0 / D, bias=EPS), r=[fstR], w=[fstR])
                act(lambda e: e.activation(out=fst[:, 2:3], in_=fst[:, 1:2], func=AF.Exp, scale=-0.5), r=[fstR], w=[fstR])
                dve(lambda e, i=i, o_=o_: e.scalar_tensor_tensor(out=o_, in0=x_sb[:, i, :], scalar=fst[:, 2:3], in1=gB[:], op0=ALU.mult, op1=ALU.mult),
                    r=[xR[i], fstR, gBR], w=[oR_])
                dma(SP, lambda e, i=i, o_=o_: e.dma_start(out=out_d[i * 128:(i + 1) * 128, :], in_=o_), r=[oR_], key="out")

        fw = ["out"] + (["tap"] if tap_d else [])
        S.emit(final_waits=fw)
    return nc


_NC_CACHE = {}
SPLITS = [(0, 4)]


def kernel(x, positions, norm1_g, w_in, pool_w, pool_scale, lb_logits, hgrn_norm_g, w_out, norm2_g,
           w_ff_in, w_ff_out, final_norm_g):
    B, T, _ = x.shape
    NT = T // 128
    L = w_in.shape[0]
    splits = SPLITS if L == 4 else [(0, L)]
    progs = []
    for (a_, b_) in splits:
        key = (NT, L, a_, b_)
        if key not in _NC_CACHE:
            _NC_CACHE[key] = build_program(NT=NT, L=L, l0=a_, l1=b_, final=(b_ == L))
        progs.append(_NC_CACHE[key])
    f = lambda a: np.ascontiguousarray(np.asarray(a, dtype=np.float32))
    shared = {"norm1_g": f(norm1_g), "w_in": f(w_in), "pool_w": f(pool_w), "pool_scale": f(pool_scale),
              "lb_logits": f(lb_logits), "hgrn_norm_g": f(hgrn_norm_g), "w_out": f(w_out), "norm2_g": f(norm2_g),
              "w_ff_in": f(w_ff_in), "w_ff_out": f(w_ff_out), "final_norm_g": f(final_norm_g)}
    xs = np.asarray(x, dtype=np.float32)
    ps = np.asarray(positions, dtype=np.int32)
    in_maps = []
    for b in range(B):
        m = dict(shared)
        m["x"] = np.ascontiguousarray(xs[b])
        m["positions"] = np.ascontiguousarray(ps[b].reshape(NT, 128))
        in_maps.append(m)
    for nc in progs:
        res = run_bass_kernel_spmd(nc, in_maps, core_ids=list(range(B)))
        outs = [np.asarray(r["out"]) for r in res.results]
        for b in range(B):
            in_maps[b]["x"] = np.ascontiguousarray(outs[b])
    return np.stack(outs, axis=0).astype(np.float32)
```

```python
import contextlib
import numpy as np
import concourse.bass as bass
import concourse.mybir as mybir
from concourse.bass_utils import run_bass_kernel_spmd

F32 = mybir.dt.float32
BF16 = mybir.dt.bfloat16
I32 = mybir.dt.int32
ALU = mybir.AluOpType
AF = mybir.ActivationFunctionType
AX = mybir.AxisListType

PE, ACT, DVE, POOL, SP = "tensor", "scalar", "vector", "gpsimd", "sync"
ENGS = (PE, ACT, DVE, POOL, SP)

D = 1024
DIN = 2372
DFF = 4096
TOPK_MAX = 256
EPS = 1e-5
BIG = 30000.0
NBIS = 12


class Res:
    __slots__ = ("name", "last_w", "readers")

    def __init__(self, name):
        self.name = name
        self.last_w = None
        self.readers = []


class Op:
    __slots__ = ("eng", "fn", "deps", "is_dma", "sem_key", "sem_val", "seq", "needed")

    def __init__(self, eng, fn, is_dma=False, sem_key=None):
        self.eng = eng
        self.fn = fn
        self.deps = []
        self.is_dma = is_dma
        self.sem_key = sem_key
        self.sem_val = 0
        self.seq = 0
        self.needed = False


class _Rec:
    def __init__(self):
        self.call = None

    def __getattr__(self, name):
        def f(*a, **k):
            self.call = (name, a, k)
            return self
        return f


class Sched:
    def __init__(self, nc):
        self.nc = nc
        self.ops = []
        self.dma_counts = {}
        self.last_on = {}
        self.barrier_deps = []
        self.after_barrier = set()
        self.max_stage = 99
        self.cur_stage = 0

    def stage(self, k):
        self.cur_stage = k

    def _dep(self, op, d):
        if d is None or d is op:
            return
        if d.eng == PE and op.eng == PE and not d.is_dma and not op.is_dma:
            return
        op.deps.append(d)
        d.needed = True

    def add(self, eng, fn, reads=(), writes=(), is_dma=False, sem_key=None, group=False):
        if self.cur_stage > self.max_stage:
            return None
        rec = _Rec()
        fn(rec)
        op = Op(eng, rec.call, is_dma, sem_key)
        if is_dma:
            c = self.dma_counts.get(sem_key, 0) + 16
            self.dma_counts[sem_key] = c
            op.sem_val = c
        if self.barrier_deps and eng not in self.after_barrier:
            self.after_barrier.add(eng)
            for d in self.barrier_deps:
                self._dep(op, d)
        for r in reads:
            self._dep(op, r.last_w)
        for w in writes:
            lw = w.last_w
            if not (group and lw is not None and lw.is_dma and lw.sem_key == sem_key and lw.eng == eng):
                self._dep(op, lw)
            for rd in w.readers:
                self._dep(op, rd)
        for r in reads:
            r.readers.append(op)
            if len(r.readers) > 24:
                seen = {}
                for o in r.readers:
                    seen[(o.eng, o.is_dma, o.sem_key)] = o
                r.readers = list(seen.values())
        for w in writes:
            w.last_w = op
            w.readers = []
        self.ops.append(op)
        self.last_on[(eng, is_dma, sem_key if is_dma else None)] = op
        return op

    def barrier(self):
        self.barrier_deps = list(self.last_on.values())
        self.after_barrier = set()

    def emit(self, final_waits=()):
        nc = self.nc
        cnt = {e: 0 for e in ENGS}
        for op in self.ops:
            if not op.is_dma and op.needed:
                cnt[op.eng] += 1
                op.seq = cnt[op.eng]
        with contextlib.ExitStack() as st:
            esem = {e: st.enter_context(nc.semaphore("s_" + e)) for e in ENGS}
            dsem = {k: st.enter_context(nc.semaphore("d_%s" % (k,))) for k in self.dma_counts}
            block = st.enter_context(nc.Block())
            per_eng = {e: [o for o in self.ops if o.eng == e] for e in ENGS}

            def run(engname, eng):
                waited = {}
                for op in per_eng[engname]:
                    need = {}
                    for d in op.deps:
                        if d.is_dma:
                            key, val = ("d", d.sem_key), d.sem_val
                        else:
                            key, val = ("e", d.eng), d.seq
                        if need.get(key, 0) < val:
                            need[key] = val
                    todo = []
                    for key, val in need.items():
                        if waited.get(key, 0) >= val:
                            continue
                        waited[key] = val
                        todo.append((dsem[key[1]] if key[0] == "d" else esem[key[1]], val))
                    name_, a_, k_ = op.fn
                    single = (not op.is_dma) and k_.get("accum_out", None) is None
                    fused = todo.pop() if (single and todo) else None
                    for sem, val in todo:
                        eng.wait_ge(sem, val)
                    ins = getattr(eng, name_)(*a_, **k_)
                    if fused is not None:
                        ins._wait_ge(fused[0], fused[1])
                    if op.is_dma:
                        ins.then_inc(dsem[op.sem_key], 16)
                    elif op.needed:
                        ins.then_inc(esem[op.eng], 1)
                if engname == SP:
                    for k in final_waits:
                        if k in dsem:
                            eng.wait_ge(dsem[k], self.dma_counts[k])

            @block.tensor
            def _(e):
                run(PE, e)

            @block.scalar
            def _(e):
                run(ACT, e)

            @block.vector
            def _(e):
                run(DVE, e)

            @block.gpsimd
            def _(e):
                run(POOL, e)

            @block.sync
            def _(e):
                run(SP, e)


class Arena:
    def __init__(self, ap, nwords):
        self.ap = ap
        self.n = nwords
        self.off = 0

    def reset(self, off=0):
        self.off = off

    def take(self, shape, dt):
        n = 1
        for s in shape:
            n *= s
        words = n if dt == F32 or dt == I32 else (n + 1) // 2
        words = (words + 1) // 2 * 2
        assert self.off + words <= self.n, ("arena overflow", self.off, words, self.n)
        v = self.ap[:, self.off:self.off + words]
        self.off += words
        if dt != F32:
            v = v.bitcast(dt)
        v = v[:, 0:n]
        if len(shape) == 2:
            v = v.rearrange("p (a b) -> p a b", b=shape[1])
        elif len(shape) == 3:
            v = v.rearrange("p (a b c) -> p a b c", b=shape[1], c=shape[2])
        elif len(shape) == 4:
            v = v.rearrange("p (a b c d) -> p a b c d", b=shape[1], c=shape[2], d=shape[3])
        return v


def build_program(NT=16, L=4, taps=(), max_stage=99, l0=0, l1=None, final=True):
    T = NT * 128
    TOPK = min(TOPK_MAX, T // 4)
    if l1 is None:
        l1 = L
    nc = bass.Bass("TRN2", target_bir_lowering=False)
    dr = {}

    def din(name, shape, dt=F32):
        dr[name] = nc.dram_tensor(name, shape, dt, kind="ExternalInput").ap()
        return dr[name]

    x_d = din("x", [T, D])
    pos_d = din("positions", [NT, 128], I32)
    n1_d = din("norm1_g", [L, D])
    win_d = din("w_in", [L, D, DIN])
    pw_d = din("pool_w", [L, 4, 64, 64])
    psc_d = din("pool_scale", [L, 256])
    lbl_d = din("lb_logits", [L, 256])
    hng_d = din("hgrn_norm_g", [L, 256])
    wout_d = din("w_out", [L, D, D])
    n2_d = din("norm2_g", [L, D])
    w1_d = din("w_ff_in", [L, D, DFF])
    w2_d = din("w_ff_out", [L, DFF, D])
    fg_d = din("final_norm_g", [D])
    out_d = nc.dram_tensor("out", [T, D], F32, kind="ExternalOutput").ap()
    tap_d = {}
    for (tname, tshape) in taps:
        tap_d[tname] = nc.dram_tensor("tap_" + tname, tshape, F32, kind="ExternalOutput").ap()

    S = Sched(nc)
    S.max_stage = max_stage
    with contextlib.ExitStack() as st:
        def sb(name, shape, dt=F32):
            return st.enter_context(nc.sbuf_tensor(name, shape, dt))

        x_sb = sb("x_sb", [128, NT, D])
        xR = [Res("x%d" % i) for i in range(NT)]
        gB = sb("gB", [128, D])
        gBR = Res("gB")
        ident_f = sb("ident_f", [128, 128])
        ident_b = sb("ident_b", [128, 128], BF16)
        identBIG4 = sb("identBIG4", [128, 4, 128], BF16)
        ltri = sb("ltri", [128, 128])
        lrem = sb("lrem", [128, 128])
        cmask = sb("cmask", [128, 128])
        chunkind = sb("chunkind", [128, 2])
        pow2tab = sb("pow2tab", [128, NBIS])
        corr = sb("corr", [128, 2, 16])
        cs_sb = sb("cs_sb", [128, NT, 8])
        sn_sb = sb("sn_sb", [128, NT, 8])
        poolW = sb("poolW", [128, L, 2, 128], BF16)
        pscol = sb("pscol", [128, L, 2])
        hngcol = sb("hngcol", [128, L, 2])
        pB = sb("pB", [128, L, 256])
        lbB = sb("lbB", [128, 256])
        omlbB = sb("omlbB", [128, 256])
        cR = Res("consts")
        pwR = Res("poolW"); colR = Res("cols"); pBR = Res("pB"); ropeR = Res("rope")
        lbR = Res("lb")

        arenaA_t = sb("arenaA", [128, 9728])
        AA = Arena(arenaA_t, 9728)

        def psb(name, shape):
            return st.enter_context(nc.psum_tensor(name, shape, F32))
        B0 = psb("B0", [128, 512]); B1 = psb("B1", [128, 512])
        B23 = psb("B23", [128, 1024])
        B4 = psb("B4", [128, 512]); B5 = psb("B5", [128, 512])
        B6 = psb("B6", [128, 512]); B7 = psb("B7", [128, 512])
        B0, B1, B23, B4, B5, B6, B7 = [t_[:, :] for t_ in (B0, B1, B23, B4, B5, B6, B7)]
        B2 = B23[:, 0:512]; B3 = B23[:, 512:1024]
        R0, R1, R23, R4, R5, R6, R7 = [Res("B%d" % i) for i in (0, 1, 23, 4, 5, 6, 7)]

        def bf(ap_f32):
            return ap_f32.bitcast(BF16)

        nB = int(nc.sbuf_bytes_remaining) // 4 - 16
        arenaB_t = sb("arenaB", [128, nB])
        AB = Arena(arenaB_t, nB)

        def dve(fn, r=(), w=()): return S.add(DVE, fn, r, w)
        def act(fn, r=(), w=()): return S.add(ACT, fn, r, w)
        def pool(fn, r=(), w=()): return S.add(POOL, fn, r, w)
        def pe(fn, r=(), w=()): return S.add(PE, fn, r, w)
        def dma(q, fn, r=(), w=(), key=None, group=True): return S.add(q, fn, r, w, is_dma=True, sem_key=key, group=group)

        def tap(name, ap, res):
            if name in tap_d:
                dma(POOL, lambda e: e.dma_start(out=tap_d[name], in_=ap), r=res, w=(), key="tap")

        pool(lambda e: e.memset(ident_f[:], 0.0), w=[cR])
        pool(lambda e: e.affine_select(out=ident_f[:], in_=ident_f[:], pattern=[[-1, 128]], compare_op=ALU.not_equal,
                                       fill=1.0, base=0, channel_multiplier=1), r=[cR], w=[cR])
        pool(lambda e: e.tensor_copy(out=ident_b[:], in_=ident_f[:]), r=[cR], w=[cR])
        for h4 in range(4):
            pool(lambda e, h4=h4: e.tensor_scalar(out=identBIG4[:, h4, :], in0=ident_f[:], scalar1=BIG, scalar2=None,
                                                   op0=ALU.mult), r=[cR], w=[cR])
        pool(lambda e: e.memset(ltri[:], 1.0), w=[cR])
        pool(lambda e: e.affine_select(out=ltri[:], in_=ltri[:], pattern=[[1, 128]], compare_op=ALU.is_ge, fill=0.0,
                                       base=0, channel_multiplier=-1), r=[cR], w=[cR])
        pool(lambda e: e.memset(ltri[0:64, 64:128], 0.0), r=[cR], w=[cR])
        pool(lambda e: e.memset(lrem[:], 1.0), w=[cR])
        pool(lambda e: e.affine_select(out=lrem[:], in_=lrem[:], pattern=[[-1, 128]], compare_op=ALU.is_gt, fill=0.0,
                                       base=0, channel_multiplier=1), r=[cR], w=[cR])
        pool(lambda e: e.memset(lrem[64:128, 0:64], 0.0), r=[cR], w=[cR])
        pool(lambda e: e.memset(cmask[:], 0.0), w=[cR])
        pool(lambda e: e.affine_select(out=cmask[:], in_=cmask[:], pattern=[[-1, 128]], compare_op=ALU.is_ge, fill=-1e30,
                                       base=0, channel_multiplier=1), r=[cR], w=[cR])
        pool(lambda e: e.memset(chunkind[:], 0.0), w=[cR])
        pool(lambda e: e.memset(chunkind[0:64, 0:1], 1.0), r=[cR], w=[cR])
        pool(lambda e: e.memset(chunkind[64:128, 1:2], 1.0), r=[cR], w=[cR])
        for k in range(NBIS):
            pool(lambda e, k=k: e.memset(pow2tab[:, k:k + 1], 2.0 ** (-k)), w=[cR])
        pool(lambda e: e.memset(corr[:], 1.0), w=[cR])
        for g, win in enumerate((2, 4, 8, 16)):
            ct, ph = g // 2, (g % 2) * 64
            for t in range(win - 1):
                pool(lambda e, ct=ct, ph=ph, t=t, win=win: e.memset(corr[ph:ph + 64, ct, t:t + 1], float(win) / (t + 1)),
                     r=[cR], w=[cR])

        pool(lambda e: e.memset(poolW[:], 0.0), w=[pwR])
        for l in range(L):
            for g in range(4):
                ct, ph = g // 2, (g % 2) * 64
                dma(POOL, lambda e, l=l, g=g, ct=ct, ph=ph: e.dma_start(out=poolW[ph:ph + 64, l, ct, ph:ph + 64], in_=pw_d[l, g]),
                    w=[pwR], key="c_pw")
        dma(SP, lambda e: e.dma_start(out=pscol[:], in_=psc_d.rearrange("l (c p) -> p l c", p=128), allow_slow_non_contiguous=True), w=[colR], key="c_col")
        dma(SP, lambda e: e.dma_start(out=hngcol[:], in_=hng_d.rearrange("l (c p) -> p l c", p=128), allow_slow_non_contiguous=True), w=[colR], key="c_col")
        dma(SP, lambda e: e.dma_start(out=pB[:], in_=lbl_d.rearrange("(o l) c -> o l c", o=1).to_broadcast([128, L, 256])), w=[pBR], key="c_pB")
        pos_i = AB.take([NT], I32)
        pos_f = AB.take([NT], F32)
        ang = AB.take([NT, 8], F32)
        angr = AB.take([NT, 8], F32)
        dma(SP, lambda e: e.dma_start(out=pos_i[:], in_=pos_d.rearrange("i p -> p i"), allow_slow_non_contiguous=True), w=[ropeR], key="c_pos")
        dve(lambda e: e.tensor_copy(out=pos_f[:], in_=pos_i[:]), r=[ropeR], w=[ropeR])
        for j in range(8):
            inv = float(np.float32(500000.0) ** np.float32(-(2.0 * j) / 16.0))
            dve(lambda e, j=j, inv=inv: e.tensor_scalar(out=ang[:, :, j], in0=pos_f[:], scalar1=inv, scalar2=None, op0=ALU.mult),
                r=[ropeR], w=[ropeR])
        TWO_PI = 2.0 * np.pi
        kf = AB.take([NT, 8], F32)
        ki_ = AB.take([NT, 8], I32)
        tt = AB.take([NT, 8], F32)
        for (dst, shift) in ((sn_sb, 0.0), (cs_sb, 0.5 * np.pi)):
            dve(lambda e, shift=shift: e.tensor_scalar(out=angr[:], in0=ang[:], scalar1=float(shift), scalar2=None, op0=ALU.add), r=[ropeR], w=[ropeR])
            dve(lambda e: e.tensor_scalar(out=kf[:], in0=angr[:], scalar1=float(1.0 / TWO_PI), scalar2=None, op0=ALU.mult), r=[ropeR], w=[ropeR])
            dve(lambda e: e.tensor_copy(out=ki_[:], in_=kf[:]), r=[ropeR], w=[ropeR])
            dve(lambda e: e.tensor_copy(out=kf[:], in_=ki_[:]), r=[ropeR], w=[ropeR])
            dve(lambda e: e.scalar_tensor_tensor(out=angr[:], in0=kf[:], scalar=float(-TWO_PI), in1=angr[:], op0=ALU.mult, op1=ALU.add), r=[ropeR], w=[ropeR])
            dve(lambda e: e.tensor_scalar(out=tt[:], in0=angr[:], scalar1=float(np.pi), scalar2=float(-TWO_PI), op0=ALU.is_gt, op1=ALU.mult), r=[ropeR], w=[ropeR])
            dve(lambda e: e.tensor_tensor(out=angr[:], in0=angr[:], in1=tt[:], op=ALU.add), r=[ropeR], w=[ropeR])
            dve(lambda e: e.tensor_scalar(out=tt[:], in0=angr[:], scalar1=float(-np.pi), scalar2=float(TWO_PI), op0=ALU.is_lt, op1=ALU.mult), r=[ropeR], w=[ropeR])
            dve(lambda e: e.tensor_tensor(out=angr[:], in0=angr[:], in1=tt[:], op=ALU.add), r=[ropeR], w=[ropeR])
            dve(lambda e: e.tensor_scalar(out=angr[:], in0=angr[:], scalar1=3.141592, scalar2=-3.141592, op0=ALU.min, op1=ALU.max), r=[ropeR], w=[ropeR])
            act(lambda e, dst=dst: e.activation(out=dst[:], in_=angr[:], func=AF.Sin), r=[ropeR], w=[ropeR])
        mx = AB.take([256], F32)
        zs = AB.take([256], F32)
        dve(lambda e: e.tensor_copy(out=mx[:], in_=pB[:, 0, :]), r=[pBR], w=[pBR])
        for l in range(1, L):
            dve(lambda e, l=l: e.tensor_tensor(out=mx[:], in0=mx[:], in1=pB[:, l, :], op=ALU.max), r=[pBR], w=[pBR])
        for l in range(L):
            dve(lambda e, l=l: e.tensor_tensor(out=pB[:, l, :], in0=pB[:, l, :], in1=mx[:], op=ALU.subtract), r=[pBR], w=[pBR])
        act(lambda e: e.activation(out=pB[:], in_=pB[:], func=AF.Exp), r=[pBR], w=[pBR])
        dve(lambda e: e.tensor_copy(out=zs[:], in_=pB[:, 0, :]), r=[pBR], w=[pBR])
        for l in range(1, L):
            dve(lambda e, l=l: e.tensor_tensor(out=zs[:], in0=zs[:], in1=pB[:, l, :], op=ALU.add), r=[pBR], w=[pBR])
        dve(lambda e: e.reciprocal(out=zs[:], in_=zs[:]), r=[pBR], w=[pBR])
        for l in range(L):
            dve(lambda e, l=l: e.tensor_tensor(out=pB[:, l, :], in0=pB[:, l, :], in1=zs[:], op=ALU.mult), r=[pBR], w=[pBR])

        for i in range(NT):
            q = SP if i % 2 == 0 else ACT
            dma(q, lambda e, i=i: e.dma_start(out=x_sb[:, i, :], in_=x_d[i * 128:(i + 1) * 128, :]), w=[xR[i]], key="x%d" % i)

        def rmsnorm_to_hT(i, hT_dst, hT_res, bank, bank_res, tmp):
            st_, hn, stR, hnR = tmp
            act(lambda e: e.activation(out=hn[:], in_=x_sb[:, i, :], func=AF.Square, accum_out=st_[:, 0:1]),
                r=[xR[i]], w=[stR, hnR])
            act(lambda e: e.activation(out=st_[:, 1:2], in_=st_[:, 0:1], func=AF.Ln, scale=1.0 / D, bias=EPS), r=[stR], w=[stR])
            act(lambda e: e.activation(out=st_[:, 2:3], in_=st_[:, 1:2], func=AF.Exp, scale=-0.5), r=[stR], w=[stR])
            dve(lambda e: e.scalar_tensor_tensor(out=hn[:], in0=x_sb[:, i, :], scalar=st_[:, 2:3], in1=gB[:],
                                                 op0=ALU.mult, op1=ALU.mult), r=[xR[i], stR, gBR], w=[hnR])
            pT = bf(bank).rearrange("p (k t) -> p k t", t=128)
            for k in range(8):
                pe(lambda e, k=k: e.transpose(out=pT[:, k, :], in_=hn[:, k * 128:(k + 1) * 128], identity=ident_b[:]),
                   r=[hnR, cR], w=[bank_res])
            act(lambda e: e.activation(out=hT_dst, in_=pT[:, 0:8, :], func=AF.Copy), r=[bank_res], w=[hT_res])

        for l in range(l0):
            if l == 0:
                dve(lambda e: e.memset(lbB[:], 0.0), w=[lbR])
            else:
                dve(lambda e, l=l: e.tensor_tensor(out=lbB[:], in0=lbB[:], in1=pB[:, l, :], op=ALU.add), r=[pBR, lbR], w=[lbR])
        for l in range(l0, l1):
            S.stage(0)
            S.barrier()
            AA.reset(); AB.reset()
            w_in = AA.take([8, DIN], BF16)
            winR = Res("w_in")
            w_out = AB.take([8, D], BF16)
            woutR = Res("w_out")
            wsrc = win_d[l].rearrange("(c p) n -> p c n", p=128)
            for k in range(8):
                dma(POOL, lambda e, k=k: e.dma_start(out=w_in[:, k, 0:1920], in_=wsrc[:, k, 0:1920]), w=[winR], key="win")
            dma(POOL, lambda e: e.dma_start(out=w_in[:, :, 1920:2240], in_=wsrc[:, :, 2048:2368]), w=[winR], key="win")
            dma(POOL, lambda e: e.dma_start(out=w_in[:, :, 2240:2368], in_=wsrc[:, :, 1920:2048]), w=[winR], key="win")
            dma(POOL, lambda e: e.dma_start(out=w_in[:, :, 2368:2372], in_=wsrc[:, :, 2368:2372]), w=[winR], key="win")
            wosrc = wout_d[l].rearrange("(c p) n -> p c n", p=128)
            for k in range(8):
                dma(POOL, lambda e, k=k: e.dma_start(out=w_out[:, k, :], in_=wosrc[:, k, :]), w=[woutR], key="wout")
            dma(SP, lambda e: e.dma_start(out=gB[:], in_=n1_d[l:l + 1, :].to_broadcast([128, D])), w=[gBR], key="g")
            if l == 0:
                dve(lambda e: e.memset(lbB[:], 0.0), w=[lbR])
            else:
                dve(lambda e, l=l: e.tensor_tensor(out=lbB[:], in0=lbB[:], in1=pB[:, l, :], op=ALU.add), r=[pBR, lbR], w=[lbR])
            dve(lambda e: e.tensor_scalar(out=omlbB[:], in0=lbB[:], scalar1=-1.0, scalar2=1.0, op0=ALU.mult, op1=ALU.add),
                r=[lbR], w=[lbR])

            KT = AB.take([T], BF16); KTR = Res("KT")
            Vs = AB.take([NT, 2, 65], BF16); VR = Res("V")
            kiT = AB.take([T // 2], F32); kiTR = Res("kiT")
            I_sb = AB.take([T], F32); IR = Res("I")
            Mb = AB.take([T], BF16); MbR = Res("Mb")
            Rh = [AB.take([512], F32) for _ in range(2)]; RhR = [Res("Rh0"), Res("Rh1")]
            PT = [AB.take([512], BF16) for _ in range(3)]; PTR = [Res("PT%d" % j) for j in range(3)]
            st_ = AB.take([8], F32); stR = Res("st")
            hn = AB.take([D], BF16); hnR = Res("hn")
            hT = AB.take([8, 128], BF16); hTR = Res("hT")
            mixT = AB.take([8, 128], BF16); mixR = Res("mixT")
            uext = [AB.take([2, 144], F32) for _ in range(2)]; uR = [Res("u0"), Res("u1")]
            slv = [AB.take([2, 144], F32) for _ in range(4)]; sR = Res("slv")
            pooled = AB.take([2, 128], BF16); pooledR = Res("pooled")
            H = {n: AB.take([256], F32) for n in ("t0", "t1", "f", "kk", "eb", "enb", "ebl", "gate")}
            HR = {n: Res("h_" + n) for n in H}
            H["logf"] = H["f"]; HR["logf"] = HR["f"]
            H["q"] = H["t1"]; HR["q"] = HR["t1"]
            qt_b = AB.take([256], BF16); kt_b = AB.take([256], BF16)
            khA = AB.take([256], BF16); khB = AB.take([256], BF16); v_b = AB.take([256], BF16)
            hbR = Res("h_bf"); qtR = Res("h_qt"); ktR = Res("h_kt"); vbR = Res("h_v")
            qTf = AB.take([4, 128], BF16); qTA = AB.take([4, 128], BF16); qTB = AB.take([4, 128], BF16); kTs = AB.take([4, 128], BF16)
            hTR2 = Res("h_T")
            Am = AB.take([4, 128], BF16); AmR = Res("Am")
            dec = AB.take([8], F32); decR = Res("dec")
            S32 = AB.take([4, 64], F32); Sbf = [AB.take([4, 64], BF16) for _ in range(2)]; Stmp = AB.take([4, 64], F32)
            SR = Res("S")
            hst = AB.take([16], F32)
            osb = H["enb"]; osq = H["t0"]; og = H["eb"]
            oR = Res("o")
            qk_b = AB.take([10, 64], BF16); qiki = AB.take([5, 64], F32)
            qk_flat = qk_b.rearrange("p h d -> p (h d)")
            rt = [AB.take([15, 8], F32) for _ in range(4)]
            ws = AB.take([4], F32)
            aR = Res("attn_tm")
            qidup = AB.take([4, 128], F32); kidup = AB.take([128], F32)
            qT_sb = AB.take([4, 128], BF16); qiT_sb = AB.take([4, 128], F32)
            aTR = Res("attn_T")
            bis = AB.take([32], F32); bisR = Res("bis")
            stepcol = AB.take([NBIS], F32)
            rden = AB.take([8], F32); ya = hn[:, 0:512]; yaR = hnR
            pool(lambda e: e.memset(uext[0][:, :, 0:16], 0.0), w=[uR[0]])
            pool(lambda e: e.memset(S32[0:64], 0.0), w=[SR])
            pool(lambda e: e.memset(Sbf[1][0:64], 0.0), w=[SR])
            pool(lambda e: e.memset(qTA[0:64], 0.0), w=[hTR2])
            pool(lambda e: e.memset(qTB[0:64], 0.0), w=[hTR2])
            pool(lambda e: e.memset(khA[64:128, :], 0.0), w=[hbR])
            pool(lambda e: e.memset(khB[0:64, :], 0.0), w=[hbR])
            pool(lambda e: e.memset(Vs[:, :, :, 64:65], 1.0), w=[VR])

            for i in range(NT):
                T0 = i * 128
                S.stage(1)
                rmsnorm_to_hT(i, hT[:], hTR, B0, R0, (st_, hn, stR, hnR))

                S.stage(1.5)
                pu = B1[:, 0:256].rearrange("p (c t) -> p c t", t=128)
                for ct in range(2):
                    for k in range(8):
                        pe(lambda e, ct=ct, k=k: e.matmul(pu[:, ct, :], lhsT=w_in[:, k, ct * 128:(ct + 1) * 128], rhs=hT[:, k, :],
                                                           start=(k == 0), stop=(k == 7)), r=[winR, hTR], w=[R1])
                ue = uext[i % 2]; un = uext[(i + 1) % 2]
                act(lambda e, ue=ue: e.activation(out=ue[:, :, 16:144], in_=pu, func=AF.Copy), r=[R1], w=[uR[i % 2]])
                dve(lambda e, ue=ue, un=un: e.tensor_copy(out=un[:, :, 0:16], in_=ue[:, :, 128:144]), r=[uR[i % 2]], w=[uR[(i + 1) % 2]])
                prev = ue
                S.stage(1.6)
                for lv, sh in enumerate((1, 2, 4, 8)):
                    lo = 2 * sh - 1
                    dve(lambda e, lv=lv, sh=sh, lo=lo, prev=prev: e.tensor_tensor(out=slv[lv][:, :, lo:144], in0=prev[:, :, lo:144],
                                                                                   in1=prev[:, :, lo - sh:144 - sh], op=ALU.add),
                         r=[uR[i % 2], sR], w=[sR])
                    prev = slv[lv]
                S.stage(1.7)
                for g, win in enumerate((2, 4, 8, 16)):
                    ct, ph = g // 2, (g % 2) * 64
                    if i == 0:
                        dve(lambda e, g=g, ct=ct, ph=ph: e.tensor_tensor(out=slv[g][ph:ph + 64, ct, 16:32], in0=slv[g][ph:ph + 64, ct, 16:32],
                                                                          in1=corr[ph:ph + 64, ct, :], op=ALU.mult), r=[sR, cR], w=[sR])
                    dve(lambda e, g=g, ct=ct, ph=ph, win=win, ue=ue: e.scalar_tensor_tensor(
                        out=pooled[ph:ph + 64, ct, :], in0=slv[g][ph:ph + 64, ct, 16:144], scalar=1.0 / win, in1=ue[ph:ph + 64, ct, 16:144],
                        op0=ALU.mult, op1=ALU.subtract), r=[sR, uR[i % 2]], w=[pooledR])
                S.stage(1.8)
                py = B1[:, 256:512].rearrange("p (c t) -> p c t", t=128)
                for ct in range(2):
                    pe(lambda e, ct=ct: e.matmul(py[:, ct, :], lhsT=poolW[:, l, ct, :], rhs=pooled[:, ct, :], start=True, stop=True),
                       r=[pwR, pooledR], w=[R1])
                S.stage(1.9)
                for ct in range(2):
                    act(lambda e, ct=ct: e.activation(out=mixT[:, ct, :], in_=py[:, ct, :], func=AF.Identity, scale=pscol[:, l, ct:ct + 1]),
                        r=[R1, colR], w=[mixR])

                S.stage(2)
                for j, bank in enumerate((B2, B3)):
                    for k in range(8):
                        pe(lambda e, j=j, k=k, bank=bank: e.matmul(bank, lhsT=hT[:, k, :], rhs=w_in[:, k, 256 + j * 512:256 + (j + 1) * 512],
                                                                   start=(k == 0), stop=(k == 7)), r=[hTR, winR], w=[R23])
                hq_p, hf_p, hi_p, hg_p = B2[:, 0:256], B2[:, 256:512], B3[:, 0:256], B3[:, 256:512]
                act(lambda e: e.activation(out=H["t0"], in_=hf_p, func=AF.Exp, scale=-1.0), r=[R23], w=[HR["t0"]])
                act(lambda e: e.activation(out=H["t1"], in_=hq_p, func=AF.Exp, scale=-1.0), r=[R23], w=[HR["t1"]])
                act(lambda e: e.activation(out=H["gate"], in_=hg_p, func=AF.Exp, scale=-1.0), r=[R23], w=[HR["gate"]])
                act(lambda e: e.activation(out=v_b, in_=hi_p, func=AF.Copy), r=[R23], w=[vbR])
                dve(lambda e: e.tensor_scalar(out=H["t0"], in0=H["t0"], scalar1=1.0, scalar2=None, op0=ALU.add), r=[HR["t0"]], w=[HR["t0"]])
                dve(lambda e: e.reciprocal(out=H["t0"], in_=H["t0"]), r=[HR["t0"]], w=[HR["t0"]])
                dve(lambda e: e.tensor_tensor(out=H["f"], in0=H["t0"], in1=omlbB[:], op=ALU.mult), r=[HR["t0"], lbR], w=[HR["f"]])
                dve(lambda e: e.tensor_tensor(out=H["f"], in0=H["f"], in1=lbB[:], op=ALU.add), r=[HR["f"], lbR], w=[HR["f"]])
                dve(lambda e: e.tensor_scalar(out=H["kk"], in0=H["f"], scalar1=-1.0, scalar2=1.0, op0=ALU.mult, op1=ALU.add),
                     r=[HR["f"]], w=[HR["kk"]])
                dve(lambda e: e.tensor_scalar(out=H["f"], in0=H["f"], scalar1=1e-30, scalar2=None, op0=ALU.max), r=[HR["f"], HR["kk"]], w=[HR["f"]])
                act(lambda e: e.activation(out=H["logf"], in_=H["f"], func=AF.Ln), r=[HR["f"]], w=[HR["logf"]])
                pe(lambda e: e.matmul(B4[:, 0:256], lhsT=ltri[:], rhs=H["logf"], start=True, stop=True), r=[cR, HR["logf"]], w=[R4])
                pe(lambda e: e.matmul(B4[:, 256:512], lhsT=lrem[:], rhs=H["logf"], start=True, stop=True), r=[cR, HR["logf"]], w=[R4])
                dl = B0[0:64, 0:8].rearrange("p (h c) -> p h c", c=2)
                for h in range(4):
                    pe(lambda e, h=h: e.matmul(dl[:, h, :], lhsT=H["logf"][:, h * 64:(h + 1) * 64], rhs=chunkind[:], start=True, stop=True),
                       r=[cR, HR["logf"]], w=[R0])
                act(lambda e: e.activation(out=H["eb"], in_=B4[:, 0:256], func=AF.Exp), r=[R4], w=[HR["eb"]])
                act(lambda e: e.activation(out=H["enb"], in_=B4[:, 0:256], func=AF.Exp, scale=-1.0), r=[R4], w=[HR["enb"]])
                act(lambda e: e.activation(out=H["ebl"], in_=B4[:, 256:512], func=AF.Exp), r=[R4], w=[HR["ebl"]])
                act(lambda e: e.activation(out=dec[0:64].rearrange("p (h c) -> p h c", c=2), in_=dl, func=AF.Exp), r=[R0], w=[decR])
                dve(lambda e: e.tensor_scalar(out=H["t1"], in0=H["t1"], scalar1=1.0, scalar2=None, op0=ALU.add), r=[HR["t1"]], w=[HR["t1"]])
                dve(lambda e: e.reciprocal(out=H["t1"], in_=H["t1"]), r=[HR["t1"]], w=[HR["t1"]])
                dve(lambda e: e.tensor_tensor(out=H["q"], in0=hq_p, in1=H["t1"], op=ALU.mult), r=[R23, HR["t1"]], w=[HR["q"]])
                dve(lambda e: e.tensor_scalar(out=H["gate"], in0=H["gate"], scalar1=1.0, scalar2=None, op0=ALU.add), r=[HR["gate"]], w=[HR["gate"]])
                dve(lambda e: e.reciprocal(out=H["gate"], in_=H["gate"]), r=[HR["gate"]], w=[HR["gate"]])
                dve(lambda e: e.tensor_tensor(out=H["gate"], in0=hg_p, in1=H["gate"], op=ALU.mult), r=[R23, HR["gate"]], w=[HR["gate"]])
                dve(lambda e: e.tensor_tensor(out=qt_b, in0=H["q"], in1=H["eb"], op=ALU.mult), r=[HR["q"], HR["eb"]], w=[qtR])
                dve(lambda e: e.tensor_tensor(out=kt_b, in0=H["kk"], in1=H["enb"], op=ALU.mult), r=[HR["kk"], HR["enb"]], w=[ktR])
                dve(lambda e: e.tensor_tensor(out=khA[0:64, :], in0=H["kk"][0:64, :], in1=H["ebl"][0:64, :], op=ALU.mult),
                     r=[HR["kk"], HR["ebl"]], w=[hbR])
                dve(lambda e: e.tensor_tensor(out=khB[64:128, :], in0=H["kk"][64:128, :], in1=H["ebl"][64:128, :], op=ALU.mult),
                     r=[HR["kk"], HR["ebl"]], w=[hbR])
                tq = bf(B5)[0:64, 0:512].rearrange("p (h t) -> p h t", t=128)
                tk = bf(B5)[0:64, 512:1024].rearrange("p (h t) -> p h t", t=128)
                for h in range(4):
                    pe(lambda e, h=h: e.transpose(out=tq[:, h, :], in_=qt_b[:, h * 64:(h + 1) * 64], identity=ident_b[:]), r=[qtR, cR], w=[R5])
                for h in range(4):
                    pe(lambda e, h=h: e.transpose(out=tk[:, h, :], in_=kt_b[:, h * 64:(h + 1) * 64], identity=ident_b[:]), r=[ktR, cR], w=[R5])
                act(lambda e: e.activation(out=qTf[0:64], in_=tq, func=AF.Copy), r=[R5], w=[hTR2])
                dve(lambda e: e.tensor_copy(out=qTA[0:64, :, 0:64], in_=tq[:, :, 0:64]), r=[R5], w=[hTR2])
                dve(lambda e: e.tensor_copy(out=qTB[0:64, :, 64:128], in_=tq[:, :, 64:128]), r=[R5], w=[hTR2])
                act(lambda e: e.activation(out=kTs[0:64], in_=tk, func=AF.Copy), r=[R5], w=[hTR2])
                pA = B6.rearrange("p (h t) -> p h t", t=128)
                for h in range(4):
                    pe(lambda e, h=h: e.matmul(pA[:, h, :], lhsT=kTs[0:64, h, :], rhs=qTf[0:64, h, :], start=True, stop=True), r=[hTR2], w=[R6])
                dve(lambda e: e.tensor_tensor(out=Am, in0=pA, in1=ltri[:].unsqueeze(1).to_broadcast([128, 4, 128]), op=ALU.mult),
                    r=[R6, cR], w=[AmR])
                pKV = B7[0:64, :].rearrange("p (c h v) -> p c h v", c=2, h=4)
                for c, kh in enumerate((khA, khB)):
                    for h in range(4):
                        pe(lambda e, c=c, h=h, kh=kh: e.matmul(pKV[:, c, h, :], lhsT=kh[:, h * 64:(h + 1) * 64], rhs=v_b[:, h * 64:(h + 1) * 64],
                                                               start=True, stop=True), r=[hbR, vbR], w=[R7])
                decv = dec[0:64].rearrange("p (h c) -> p h c", c=2)
                for c in range(2):
                    dve(lambda e, c=c: e.tensor_tensor(out=Stmp[0:64], in0=S32[0:64], in1=decv[:, :, c:c + 1].to_broadcast([64, 4, 64]), op=ALU.mult),
                        r=[SR, decR], w=[SR])
                    dve(lambda e, c=c: e.tensor_tensor(out=S32[0:64], in0=pKV[:, c], in1=Stmp[0:64], op=ALU.add), r=[SR, R7], w=[SR])
                    if c == 0:
                        dve(lambda e: e.tensor_copy(out=Sbf[0][0:64], in_=S32[0:64]), r=[SR], w=[SR])
                        po = B4[:, 0:256].rearrange("p (h v) -> p h v", v=64)
                        for h in range(4):
                            pe(lambda e, h=h: e.matmul(po[:, h, :], lhsT=Am[:, h, :], rhs=v_b[:, h * 64:(h + 1) * 64], start=True, stop=False),
                               r=[AmR, vbR], w=[R4])
                            pe(lambda e, h=h: e.matmul(po[:, h, :], lhsT=qTA[0:64, h, :], rhs=Sbf[1][0:64, h, :], start=False, stop=False),
                               r=[hTR2, SR], w=[R4])
                            pe(lambda e, h=h: e.matmul(po[:, h, :], lhsT=qTB[0:64, h, :], rhs=Sbf[0][0:64, h, :], start=False, stop=True),
                               r=[hTR2, SR], w=[R4])
                    else:
                        dve(lambda e: e.tensor_copy(out=Sbf[1][0:64], in_=S32[0:64]), r=[SR], w=[SR])
                act(lambda e: e.activation(out=osb, in_=B4[:, 0:256], func=AF.Copy), r=[R4], w=[oR, HR["enb"]])
                dve(lambda e: e.tensor_tensor(out=osq, in0=osb, in1=osb, op=ALU.mult), r=[oR], w=[oR, HR["t0"]])
                dve(lambda e: e.tensor_reduce(out=hst[:, 0:4], in_=osq.rearrange("p (h v) -> p h v", v=64), axis=AX.X, op=ALU.add), r=[oR], w=[oR])
                act(lambda e: e.activation(out=hst[:, 4:8], in_=hst[:, 0:4], func=AF.Ln, scale=1.0 / 64, bias=EPS), r=[oR], w=[oR])
                act(lambda e: e.activation(out=hst[:, 8:12], in_=hst[:, 4:8], func=AF.Exp, scale=-0.5), r=[oR], w=[oR])
                dve(lambda e: e.tensor_tensor(out=og.rearrange("p (h v) -> p h v", v=64), in0=osb.rearrange("p (h v) -> p h v", v=64),
                                              in1=hst[:, 8:12].unsqueeze(2).to_broadcast([128, 4, 64]), op=ALU.mult), r=[oR], w=[oR, HR["eb"]])
                dve(lambda e: e.tensor_tensor(out=og, in0=og, in1=H["gate"], op=ALU.mult), r=[oR, HR["gate"]], w=[oR])
                pg = B5[:, 0:256].rearrange("p (c t) -> p c t", t=128)
                for c in range(2):
                    pe(lambda e, c=c: e.transpose(out=pg[:, c, :], in_=og[:, c * 128:(c + 1) * 128], identity=ident_f[:]), r=[oR, cR], w=[R5])
                for c in range(2):
                    act(lambda e, c=c: e.activation(out=mixT[:, 2 + c, :], in_=pg[:, c, :], func=AF.Identity, scale=hngcol[:, l, c:c + 1]),
                        r=[R5, colR], w=[mixR])
                if i == NT - 1:
                    tap("S32_l%d" % l, S32[0:64], [SR])

                S.stage(3)
                for k in range(8):
                    pe(lambda e, k=k: e.matmul(B23[:, 0:512], lhsT=hT[:, k, :], rhs=w_in[:, k, 1280:1792], start=(k == 0), stop=(k == 7)),
                       r=[hTR, winR], w=[R23])
                for k in range(8):
                    pe(lambda e, k=k: e.matmul(B23[:, 512:960], lhsT=hT[:, k, :], rhs=w_in[:, k, 1792:2240], start=(k == 0), stop=(k == 7)),
                       r=[hTR, winR], w=[R23])
                for k in range(8):
                    pe(lambda e, k=k: e.matmul(B4[:, 0:132], lhsT=hT[:, k, :], rhs=w_in[:, k, 2240:2372], start=(k == 0), stop=(k == 7)),
                       r=[hTR, winR], w=[R4])
                pv = B23[:, 0:960].rearrange("p (h d) -> p h d", d=64)
                x1, x2 = pv[:, :, 0:8], pv[:, :, 8:16]
                cB = cs_sb[:, i, :].unsqueeze(1).to_broadcast([128, 15, 8])
                sB = sn_sb[:, i, :].unsqueeze(1).to_broadcast([128, 15, 8])
                dve(lambda e: e.tensor_tensor(out=rt[0], in0=x1, in1=cB, op=ALU.mult), r=[R23, ropeR], w=[aR])
                dve(lambda e: e.tensor_tensor(out=rt[1], in0=x2, in1=sB, op=ALU.mult), r=[R23, ropeR], w=[aR])
                dve(lambda e: e.tensor_tensor(out=rt[2], in0=x2, in1=cB, op=ALU.mult), r=[R23, ropeR], w=[aR])
                dve(lambda e: e.tensor_tensor(out=rt[3], in0=x1, in1=sB, op=ALU.mult), r=[R23, ropeR], w=[aR])
                qdst = qk_b[:, 0:8, :].rearrange("p (j k) d -> p k j d", k=2)
                dve(lambda e: e.tensor_tensor(out=qdst[:, :, :, 0:8], in0=rt[0][:, 0:8, :].rearrange("p (k j) d -> p k j d", k=2),
                                               in1=rt[1][:, 0:8, :].rearrange("p (k j) d -> p k j d", k=2), op=ALU.subtract), r=[aR], w=[aR])
                dve(lambda e: e.tensor_tensor(out=qdst[:, :, :, 8:16], in0=rt[2][:, 0:8, :].rearrange("p (k j) d -> p k j d", k=2),
                                               in1=rt[3][:, 0:8, :].rearrange("p (k j) d -> p k j d", k=2), op=ALU.add), r=[aR], w=[aR])
                dve(lambda e: e.tensor_tensor(out=qk_b[:, 8:10, 0:8], in0=rt[0][:, 8:10, :], in1=rt[1][:, 8:10, :], op=ALU.subtract), r=[aR], w=[aR])
                dve(lambda e: e.tensor_tensor(out=qk_b[:, 8:10, 8:16], in0=rt[2][:, 8:10, :], in1=rt[3][:, 8:10, :], op=ALU.add), r=[aR], w=[aR])
                dve(lambda e: e.tensor_tensor(out=qiki[:, :, 0:8], in0=rt[0][:, 10:15, :], in1=rt[1][:, 10:15, :], op=ALU.subtract), r=[aR], w=[aR])
                dve(lambda e: e.tensor_tensor(out=qiki[:, :, 8:16], in0=rt[2][:, 10:15, :], in1=rt[3][:, 10:15, :], op=ALU.add), r=[aR], w=[aR])
                act(lambda e: e.activation(out=qdst[:, :, :, 16:64], in_=pv[:, 0:8, 16:64].rearrange("p (k j) d -> p k j d", k=2), func=AF.Copy),
                    r=[R23], w=[aR])
                act(lambda e: e.activation(out=qk_b[:, 8:10, 16:64], in_=pv[:, 8:10, 16:64], func=AF.Copy), r=[R23], w=[aR])
                act(lambda e: e.activation(out=qiki[:, :, 16:64], in_=pv[:, 10:15, 16:64], func=AF.Copy), r=[R23], w=[aR])
                act(lambda e: e.activation(out=Vs[:, i, :, 0:64], in_=B4[:, 0:128].rearrange("p (k d) -> p k d", d=64), func=AF.Copy), r=[R4], w=[VR])
                act(lambda e: e.activation(out=ws, in_=B4[:, 128:132], func=AF.Copy, scale=1.0 / 16.0), r=[R4], w=[aR])
                for dd in range(2):
                    dve(lambda e, dd=dd: e.tensor_copy(out=qidup[:, :, dd * 64:(dd + 1) * 64], in_=qiki[:, 0:4, :]), r=[aR], w=[aR])
                    dve(lambda e, dd=dd: e.tensor_copy(out=kidup[:, dd * 64:(dd + 1) * 64], in_=qiki[:, 4, :]), r=[aR], w=[aR])
                tqa = bf(B5)[:, 0:512].rearrange("p (j t) -> p j t", t=128)
                tka = bf(B5)[:, 512:640]
                for j in range(4):
                    pe(lambda e, j=j: e.transpose(out=tqa[:, j, :], in_=qk_flat[:, j * 128:(j + 1) * 128], identity=ident_b[:]), r=[aR, cR], w=[R5])
                pe(lambda e: e.transpose(out=tka, in_=qk_flat[:, 512:640], identity=ident_b[:]), r=[aR, cR], w=[R5])
                tqi = B6.rearrange("p (h t) -> p h t", t=128)
                for h in range(4):
                    pe(lambda e, h=h: e.transpose(out=tqi[:, h, :], in_=qidup[:, h, :], identity=ident_f[:]), r=[aR, cR], w=[R6])
                tki = B7[:, 0:128]
                pe(lambda e: e.transpose(out=tki, in_=kidup, identity=ident_f[:]), r=[aR, cR], w=[R7])
                act(lambda e: e.activation(out=qT_sb, in_=tqa, func=AF.Copy), r=[R5], w=[aTR])
                act(lambda e: e.activation(out=KT[:, T0:T0 + 128], in_=tka, func=AF.Copy), r=[R5], w=[KTR])
                dve(lambda e: e.tensor_copy(out=qiT_sb, in_=tqi), r=[R6], w=[aTR])
                half = 0 if T0 < T // 2 else 64
                kcol = T0 - (0 if half == 0 else T // 2)
                dve(lambda e, half=half, kcol=kcol: e.tensor_copy(out=kiT[half:half + 64, kcol:kcol + 128], in_=tki[half:half + 64, :]),
                    r=[R7], w=[kiTR])
                S.stage(4)
                n = T0 + 128
                nblk = (n + 511) // 512
                sbanks = ((B0, R0), (B1, R1))
                cntj = 0
                segs = []
                for hh in range(2):
                    a0, a1 = hh * (T // 2), min(n, (hh + 1) * (T // 2))
                    k0 = a0
                    while k0 < a1:
                        segs.append((k0, min(512, a1 - k0), hh * 64, k0 - a0))
                        k0 += 512
                for (k0, wdt, kh_, kc0) in segs:
                    for h in range(4):
                        bank, bres = sbanks[cntj % 2]
                        rh, rhr = Rh[cntj % 2], RhR[cntj % 2]
                        cntj += 1
                        pe(lambda e, bank=bank, h=h, kh_=kh_, kc0=kc0, wdt=wdt: e.matmul(
                            bank[:, 0:wdt], lhsT=qiT_sb[kh_:kh_ + 64, h, :], rhs=kiT[kh_:kh_ + 64, kc0:kc0 + wdt], start=True, stop=True),
                           r=[aTR, kiTR], w=[bres])
                        if h == 0:
                            dve(lambda e, bank=bank, k0=k0, wdt=wdt: e.tensor_scalar(out=I_sb[:, k0:k0 + wdt], in0=bank[:, 0:wdt], scalar1=0.0,
                                                                                     scalar2=ws[:, 0:1], op0=ALU.max, op1=ALU.mult),
                                r=[bres, aR], w=[IR])
                        else:
                            act(lambda e, bank=bank, rh=rh, wdt=wdt: e.activation(out=rh[:, 0:wdt], in_=bank[:, 0:wdt], func=AF.Relu), r=[bres], w=[rhr])
                            dve(lambda e, rh=rh, h=h, k0=k0, wdt=wdt: e.scalar_tensor_tensor(
                                out=I_sb[:, k0:k0 + wdt], in0=rh[:, 0:wdt], scalar=ws[:, h:h + 1], in1=I_sb[:, k0:k0 + wdt],
                                op0=ALU.mult, op1=ALU.add), r=[rhr, aR, IR], w=[IR])
                S.stage(5)
                if n > TOPK:
                    dve(lambda e, n=n: e.tensor_reduce(out=bis[:, 0:1], in_=I_sb[:, 0:n], axis=AX.X, op=ALU.max, apply_absolute_value=True),
                        r=[IR], w=[bisR])
                dve(lambda e: e.tensor_tensor(out=I_sb[:, T0:T0 + 128], in0=I_sb[:, T0:T0 + 128], in1=cmask[:], op=ALU.add), r=[IR, cR], w=[IR])
                thr = bis[:, 1:2]
                if n > TOPK:
                    dve(lambda e: e.tensor_scalar(out=stepcol, in0=pow2tab[:], scalar1=bis[:, 0:1], scalar2=None, op0=ALU.mult), r=[bisR, cR], w=[bisR])
                    dve(lambda e: e.memset(thr, 0.0), r=[bisR], w=[bisR])
                    for k in range(NBIS):
                        dve(lambda e, n=n: e.tensor_scalar(out=Mb[:, 0:n], in0=I_sb[:, 0:n], scalar1=thr, scalar2=None, op0=ALU.is_ge, op1=ALU.add,
                                                            accum_out=bis[:, 2:3]), r=[IR, bisR], w=[MbR, bisR])
                        dve(lambda e: e.tensor_scalar(out=bis[:, 3:4], in0=bis[:, 2:3], scalar1=float(TOPK), scalar2=0.5, op0=ALU.is_ge, op1=ALU.subtract),
                            r=[bisR], w=[bisR])
                        dve(lambda e, k=k: e.scalar_tensor_tensor(out=thr, in0=bis[:, 3:4], scalar=stepcol[:, k:k + 1], in1=thr, op0=ALU.mult, op1=ALU.add),
                            r=[bisR], w=[bisR])
                    dve(lambda e: e.scalar_tensor_tensor(out=thr, in0=stepcol[:, NBIS - 1:NBIS], scalar=-0.5, in1=thr, op0=ALU.mult, op1=ALU.add),
                        r=[bisR], w=[bisR])
                else:
                    dve(lambda e: e.memset(thr, -1e29), r=[bisR], w=[bisR])
                dve(lambda e, n=n: e.tensor_scalar(out=Mb[:, 0:n], in0=I_sb[:, 0:n], scalar1=thr, scalar2=1.0, op0=ALU.is_ge, op1=ALU.subtract),
                    r=[IR, bisR], w=[MbR])
                if i == NT - 1 and l == 0:
                    tap("I", I_sb, [IR]); tap("bis", bis, [bisR]); tap("Mb", Mb, [MbR]); tap("qT", qT_sb, [aTR]); tap("KT", KT, [KTR])
                    tap("V", Vs, [VR]); tap("ws", ws, [aR]); tap("qiT", qiT_sb, [aTR]); tap("kiT", kiT, [kiTR])
                S.stage(6)
                sT = ((B4, R4), (B5, R5))
                pOT = (B6, B7)
                pOR = (R6, R7)
                pOq = (B0[:, 0:260].rearrange("p (h v) -> p h v", v=65), B1[:, 0:260].rearrange("p (h v) -> p h v", v=65))
                pOqR = (R0, R1)
                OTs = Rh[0]; OTsR = RhR[0]
                iters = [(kv, c) for kv in range(2) for c in range(i + 1)]

                def emit_qk(idx):
                    kv, c = iters[idx]
                    bank, bres = sT[idx % 2]
                    pt, ptr = PT[idx % 3], PTR[idx % 3]
                    pe(lambda e: e.matmul(bank, lhsT=KT[kv * 64:(kv + 1) * 64, c * 128:(c + 1) * 128],
                                          rhs=qT_sb[kv * 64:(kv + 1) * 64, :, :], start=True, stop=False), r=[KTR, aTR], w=[bres])
                    pe(lambda e: e.matmul(bank, lhsT=Mb[:, c * 128:(c + 1) * 128], rhs=identBIG4[:], start=False, stop=True),
                       r=[MbR, cR], w=[bres])
                    act(lambda e: e.activation(out=pt, in_=bank, func=AF.Exp, scale=0.125), r=[bres], w=[ptr])

                def emit_pv(idx):
                    kv, c = iters[idx]
                    pt, ptr = PT[idx % 3], PTR[idx % 3]
                    pe(lambda e: e.matmul(pOT[kv][0:65, :], lhsT=Vs[:, c, kv, :], rhs=pt, start=(c == 0), stop=(c == i)),
                       r=[ptr, VR], w=[pOR[kv]])
                    if c == i:
                        act(lambda e: e.activation(out=OTs[0:65, :], in_=pOT[kv][0:65, :], func=AF.Copy), r=[pOR[kv]], w=[OTsR])
                        for h4 in range(4):
                            pe(lambda e, h4=h4: e.transpose(out=pOq[kv][:, h4, :], in_=OTs[0:65, h4 * 128:(h4 + 1) * 128], identity=ident_f[0:65, 0:65]),
                               r=[OTsR, cR], w=[pOqR[kv]])
                        dve(lambda e: e.reciprocal(out=rden[:, kv * 4:(kv + 1) * 4], in_=pOq[kv][:, :, 64]), r=[pOqR[kv]], w=[yaR])
                        dve(lambda e: e.tensor_tensor(out=ya[:, kv * 256:(kv + 1) * 256].rearrange("p (h v) -> p h v", v=64), in0=pOq[kv][:, :, 0:64],
                                                      in1=rden[:, kv * 4:(kv + 1) * 4].unsqueeze(2).to_broadcast([128, 4, 64]), op=ALU.mult),
                            r=[pOqR[kv], yaR], w=[yaR])

                for idx in range(len(iters)):
                    emit_qk(idx)
                    if idx >= 1:
                        emit_pv(idx - 1)
                emit_pv(len(iters) - 1)
                if i == NT - 1 and l == 0:
                    tap("ya", ya, [yaR]); tap("rden", rden, [yaR])
                ty = bf(B0)[:, 0:512].rearrange("p (c t) -> p c t", t=128)
                for c in range(4):
                    pe(lambda e, c=c: e.transpose(out=ty[:, c, :], in_=ya[:, c * 128:(c + 1) * 128], identity=ident_b[:]), r=[yaR, cR], w=[R0])
                act(lambda e: e.activation(out=mixT[:, 4:8, :], in_=ty, func=AF.Copy), r=[R0], w=[mixR])
                S.stage(1)
                if ("mixT_l%d" % l) in tap_d:
                    dma(POOL, lambda e, i=i, l=l: e.dma_start(out=tap_d["mixT_l%d" % l][:, :, i * 128:(i + 1) * 128], in_=mixT), r=[mixR], key="tap")

                S.stage(7)
                for hf in range(2):
                    for k in range(8):
                        pe(lambda e, hf=hf, k=k: e.matmul(B23[:, hf * 512:(hf + 1) * 512], lhsT=mixT[:, k, :], rhs=w_out[:, k, hf * 512:(hf + 1) * 512],
                                                          start=(k == 0), stop=(k == 7)), r=[mixR, woutR], w=[R23])
                for hf in range(2):
                    dve(lambda e, hf=hf, i=i: e.tensor_tensor(out=x_sb[:, i, hf * 512:(hf + 1) * 512], in0=B23[:, hf * 512:(hf + 1) * 512],
                                                               in1=x_sb[:, i, hf * 512:(hf + 1) * 512], op=ALU.add), r=[R23, xR[i]], w=[xR[i]])

            if ("xmid_l%d" % l) in tap_d:
                for i in range(NT):
                    dma(SP, lambda e, i=i, l=l: e.dma_start(out=tap_d["xmid_l%d" % l][i * 128:(i + 1) * 128, :], in_=x_sb[:, i, :]), r=[xR[i]], key="tap")

            S.stage(8)
            S.barrier()
            AA.reset(); AB.reset()
            h2T = AA.take([8, T], BF16); h2R = [Res("h2T%d" % i) for i in range(NT)]
            NCH = 8
            W1 = [AB.take([8, 512], BF16) for _ in range(2)]; W1R = [Res("W1_0"), Res("W1_1")]
            W2 = [AB.take([4, D], BF16) for _ in range(2)]; W2R = [Res("W2_0"), Res("W2_1")]
            aT = [AB.take([4, 512], BF16) for _ in range(2)]; aTRs = [[Res("aT%d_%d" % (q_, j_)) for j_ in range(4)] for q_ in range(2)]
            r32 = [AB.take([512], F32) for _ in range(4)]; r32R = [Res("r32_%d" % q_) for q_ in range(4)]
            st2 = AB.take([8], F32); st2R = Res("st2")
            hn2 = AB.take([D], BF16); hn2R = Res("hn2")
            dma(SP, lambda e: e.dma_start(out=gB[:], in_=n2_d[l:l + 1, :].to_broadcast([128, D])), w=[gBR], key="g")
            w1src = w1_d[l].rearrange("(c p) n -> p c n", p=128)
            w2src = w2_d[l].rearrange("(c p) n -> p c n", p=128)

            def load_chunk(c):
                s = c % 2
                for k in range(8):
                    dma(POOL, lambda e, k=k, s=s, c=c: e.dma_start(out=W1[s][:, k, :], in_=w1src[:, k, c * 512:(c + 1) * 512]), w=[W1R[s]], key="w1_%d" % s)
                for j in range(4):
                    dma(POOL, lambda e, j=j, s=s, c=c: e.dma_start(out=W2[s][:, j, :], in_=w2src[:, c * 4 + j, :]), w=[W2R[s]], key="w2_%d" % s)

            load_chunk(0)
            load_chunk(1)
            nbanks = ((B0, R0), (B1, R1))
            for i in range(NT):
                bank, bres = nbanks[i % 2]
                rmsnorm_to_hT(i, h2T[:, :, i * 128:(i + 1) * 128], h2R[i], bank, bres, (st2, hn2, st2R, hn2R))
            abanks = ((B0, R0), (B1, R1), (B23[:, 0:512], R23))
            ybanks = ((B4, R4), (B5, R5), (B6, R6), (B7, R7))
            aj = 0
            yj = 0
            NG = NT // 4 if NT >= 4 else 1
            GT = NT // NG
            ntok = GT * 128
            cnt_ = {"aj": 0, "yj": 0}

            def emit_w1(idx):
                c, tg = idx // NG, idx % NG
                s = c % 2
                a_t, a_r = aT[idx % 2], aTRs[idx % 2]
                for j in range(4):
                    bank, bres = abanks[cnt_["aj"] % 3]
                    cnt_["aj"] += 1
                    for k in range(8):
                        pe(lambda e: e.matmul(bank[:, 0:ntok], lhsT=W1[s][:, k, j * 128:(j + 1) * 128], rhs=h2T[:, k, tg * ntok:(tg + 1) * ntok],
                                              start=(k == 0), stop=(k == 7)), r=[W1R[s]] + h2R[tg * GT:(tg + 1) * GT], w=[bres])
                    rr, rrR = r32[cnt_["aj"] % 4], r32R[cnt_["aj"] % 4]
                    act(lambda e: e.activation(out=rr[:, 0:ntok], in_=bank[:, 0:ntok], func=AF.Relu), r=[bres], w=[rrR])
                    dve(lambda e: e.tensor_tensor(out=a_t[:, j, 0:ntok], in0=rr[:, 0:ntok], in1=rr[:, 0:ntok], op=ALU.mult), r=[rrR], w=[a_r[j]])

            def emit_y(idx):
                c, tg = idx // NG, idx % NG
                s = c % 2
                a_t, a_r = aT[idx % 2], aTRs[idx % 2]
                for t4 in range(GT):
                    ti = tg * GT + t4
                    for hf in range(2):
                        bank, bres = ybanks[cnt_["yj"] % 4]
                        cnt_["yj"] += 1
                        for j in range(4):
                            pe(lambda e: e.matmul(bank, lhsT=a_t[:, j, t4 * 128:(t4 + 1) * 128], rhs=W2[s][:, j, hf * 512:(hf + 1) * 512],
                                                  start=(j == 0), stop=(j == 3)), r=[a_r[j], W2R[s]], w=[bres])
                        dve(lambda e: e.tensor_tensor(out=x_sb[:, ti, hf * 512:(hf + 1) * 512], in0=bank,
                                                      in1=x_sb[:, ti, hf * 512:(hf + 1) * 512], op=ALU.add), r=[bres, xR[ti]], w=[xR[ti]])
                if tg == NG - 1 and c + 2 < NCH:
                    load_chunk(c + 2)

            nsteps = NCH * NG
            for idx in range(nsteps):
                emit_w1(idx)
                if idx >= 1:
                    emit_y(idx - 1)
            emit_y(nsteps - 1)
            if ("xout_l%d" % l) in tap_d:
                for i in range(NT):
                    dma(SP, lambda e, i=i, l=l: e.dma_start(out=tap_d["xout_l%d" % l][i * 128:(i + 1) * 128, :], in_=x_sb[:, i, :]), r=[xR[i]], key="tap")

        S.stage(0)
        S.barrier()
        AB.reset()
        if not final:
            for i in range(NT):
                dma(SP, lambda e, i=i: e.dma_start(out=out_d[i * 128:(i + 1) * 128, :], in_=x_sb[:, i, :]), r=[xR[i]], key="out")
        else:
            dma(SP, lambda e: e.dma_start(out=gB[:], in_=fg_d.rearrange("(o d) -> o d", o=1).to_broadcast([128, D])), w=[gBR], key="g")
            fo = [AB.take([D], F32) for _ in range(2)]; foR = [Res("fo0"), Res("fo1")]
            fj = AB.take([D], BF16); fjR = Res("fj")
            fst = AB.take([8], F32); fstR = Res("fst")
            for i in range(NT):
                o_, oR_ = fo[i % 2], foR[i % 2]
                act(lambda e, i=i: e.activation(out=fj, in_=x_sb[:, i, :], func=AF.Square, accum_out=fst[:, 0:1]), r=[xR[i]], w=[fstR, fjR])
                act(lambda e: e.activation(out=fst[:, 1:2], in_=fst[:, 0:1], func=AF.Ln, scale=1.0 / D, bias=EPS), r=[fstR], w=[fstR])
                act(lambda e: e.activation(out=fst[:, 2:3], in_=fst[:, 1:2], func=AF.Exp, scale=-0.5), r=[fstR], w=[fstR])
                dve(lambda e, i=i, o_=o_: e.scalar_tensor_tensor(out=o_, in0=x_sb[:, i, :], scalar=fst[:, 2:3], in1=gB[:], op0=ALU.mult, op1=ALU.mult),
                    r=[xR[i], fstR, gBR], w=[oR_])
                dma(SP, lambda e, i=i, o_=o_: e.dma_start(out=out_d[i * 128:(i + 1) * 128, :], in_=o_), r=[oR_], key="out")

        fw = ["out"] + (["tap"] if tap_d else [])
        S.emit(final_waits=fw)
    return nc


_NC_CACHE = {}
SPLITS = [(0, 4)]


def kernel(x, positions, norm1_g, w_in, pool_w, pool_scale, lb_logits, hgrn_norm_g, w_out, norm2_g,
           w_ff_in, w_ff_out, final_norm_g):
    B, T, _ = x.shape
    NT = T // 128
    L = w_in.shape[0]
    splits = SPLITS if L == 4 else [(0, L)]
    progs = []
    for (a_, b_) in splits:
        key = (NT, L, a_, b_)
        if key not in _NC_CACHE:
            _NC_CACHE[key] = build_program(NT=NT, L=L, l0=a_, l1=b_, final=(b_ == L))
        progs.append(_NC_CACHE[key])
    f = lambda a: np.ascontiguousarray(np.asarray(a, dtype=np.float32))
    shared = {"norm1_g": f(norm1_g), "w_in": f(w_in), "pool_w": f(pool_w), "pool_scale": f(pool_scale),
              "lb_logits": f(lb_logits), "hgrn_norm_g": f(hgrn_norm_g), "w_out": f(w_out), "norm2_g": f(norm2_g),
              "w_ff_in": f(w_ff_in), "w_ff_out": f(w_ff_out), "final_norm_g": f(final_norm_g)}
    xs = np.asarray(x, dtype=np.float32)
    ps = np.asarray(positions, dtype=np.int32)
    in_maps = []
    for b in range(B):
        m = dict(shared)
        m["x"] = np.ascontiguousarray(xs[b])
        m["positions"] = np.ascontiguousarray(ps[b].reshape(NT, 128))
        in_maps.append(m)
    for nc in progs:
        res = run_bass_kernel_spmd(nc, in_maps, core_ids=list(range(B)))
        outs = [np.asarray(r["out"]) for r in res.results]
        for b in range(B):
            in_maps[b]["x"] = np.ascontiguousarray(outs[b])
    return np.stack(outs, axis=0).astype(np.float32)
```

```python
import contextlib
import numpy as np
import concourse.bass as bass
import concourse.mybir as mybir
from concourse.bass_utils import run_bass_kernel_spmd

F32 = mybir.dt.float32
BF16 = mybir.dt.bfloat16
I32 = mybir.dt.int32
ALU = mybir.AluOpType
AF = mybir.ActivationFunctionType
AX = mybir.AxisListType

PE, ACT, DVE, POOL, SP = "tensor", "scalar", "vector", "gpsimd", "sync"
ENGS = (PE, ACT, DVE, POOL, SP)

D = 1024
DIN = 2372
DFF = 4096
TOPK_MAX = 256
EPS = 1e-5
BIG = 30000.0
NBIS = 12


class Res:
    __slots__ = ("name", "last_w", "readers")

    def __init__(self, name):
        self.name = name
        self.last_w = None
        self.readers = []


class Op:
    __slots__ = ("eng", "fn", "deps", "is_dma", "sem_key", "sem_val", "seq", "needed")

    def __init__(self, eng, fn, is_dma=False, sem_key=None):
        self.eng = eng
        self.fn = fn
        self.deps = []
        self.is_dma = is_dma
        self.sem_key = sem_key
        self.sem_val = 0
        self.seq = 0
        self.needed = False


class _Rec:
    def __init__(self):
        self.call = None

    def __getattr__(self, name):
        def f(*a, **k):
            self.call = (name, a, k)
            return self
        return f


class Sched:
    def __init__(self, nc):
        self.nc = nc
        self.ops = []
        self.dma_counts = {}
        self.last_on = {}
        self.barrier_deps = []
        self.after_barrier = set()
        self.max_stage = 99
        self.cur_stage = 0

    def stage(self, k):
        self.cur_stage = k

    def _dep(self, op, d):
        if d is None or d is op:
            return
        if d.eng == PE and op.eng == PE and not d.is_dma and not op.is_dma:
            return
        op.deps.append(d)
        d.needed = True

    def add(self, eng, fn, reads=(), writes=(), is_dma=False, sem_key=None, group=False):
        if self.cur_stage > self.max_stage:
            return None
        rec = _Rec()
        fn(rec)
        op = Op(eng, rec.call, is_dma, sem_key)
        if is_dma:
            c = self.dma_counts.get(sem_key, 0) + 16
            self.dma_counts[sem_key] = c
            op.sem_val = c
        if self.barrier_deps and eng not in self.after_barrier:
            self.after_barrier.add(eng)
            for d in self.barrier_deps:
                self._dep(op, d)
        for r in reads:
            self._dep(op, r.last_w)
        for w in writes:
            lw = w.last_w
            if not (group and lw is not None and lw.is_dma and lw.sem_key == sem_key and lw.eng == eng):
                self._dep(op, lw)
            for rd in w.readers:
                self._dep(op, rd)
        for r in reads:
            r.readers.append(op)
            if len(r.readers) > 24:
                seen = {}
                for o in r.readers:
                    seen[(o.eng, o.is_dma, o.sem_key)] = o
                r.readers = list(seen.values())
        for w in writes:
            w.last_w = op
            w.readers = []
        self.ops.append(op)
        self.last_on[(eng, is_dma, sem_key if is_dma else None)] = op
        return op

    def barrier(self):
        self.barrier_deps = list(self.last_on.values())
        self.after_barrier = set()

    def emit(self, final_waits=()):
        nc = self.nc
        cnt = {e: 0 for e in ENGS}
        for op in self.ops:
            if not op.is_dma and op.needed:
                cnt[op.eng] += 1
                op.seq = cnt[op.eng]
        with contextlib.ExitStack() as st:
            esem = {e: st.enter_context(nc.semaphore("s_" + e)) for e in ENGS}
            dsem = {k: st.enter_context(nc.semaphore("d_%s" % (k,))) for k in self.dma_counts}
            block = st.enter_context(nc.Block())
            per_eng = {e: [o for o in self.ops if o.eng == e] for e in ENGS}

            def run(engname, eng):
                waited = {}
                for op in per_eng[engname]:
                    need = {}
                    for d in op.deps:
                        if d.is_dma:
                            key, val = ("d", d.sem_key), d.sem_val
                        else:
                            key, val = ("e", d.eng), d.seq
                        if need.get(key, 0) < val:
                            need[key] = val
                    todo = []
                    for key, val in need.items():
                        if waited.get(key, 0) >= val:
                            continue
                        waited[key] = val
                        todo.append((dsem[key[1]] if key[0] == "d" else esem[key[1]], val))
                    name_, a_, k_ = op.fn
                    single = (not op.is_dma) and k_.get("accum_out", None) is None
                    fused = todo.pop() if (single and todo) else None
                    for sem, val in todo:
                        eng.wait_ge(sem, val)
                    ins = getattr(eng, name_)(*a_, **k_)
                    if fused is not None:
                        ins._wait_ge(fused[0], fused[1])
                    if op.is_dma:
                        ins.then_inc(dsem[op.sem_key], 16)
                    elif op.needed:
                        ins.then_inc(esem[op.eng], 1)
                if engname == SP:
                    for k in final_waits:
                        if k in dsem:
                            eng.wait_ge(dsem[k], self.dma_counts[k])

            @block.tensor
            def _(e):
                run(PE, e)

            @block.scalar
            def _(e):
                run(ACT, e)

            @block.vector
            def _(e):
                run(DVE, e)

            @block.gpsimd
            def _(e):
                run(POOL, e)

            @block.sync
            def _(e):
                run(SP, e)


class Arena:
    def __init__(self, ap, nwords):
        self.ap = ap
        self.n = nwords
        self.off = 0

    def reset(self, off=0):
        self.off = off

    def take(self, shape, dt):
        n = 1
        for s in shape:
            n *= s
        words = n if dt == F32 or dt == I32 else (n + 1) // 2
        words = (words + 1) // 2 * 2
        assert self.off + words <= self.n, ("arena overflow", self.off, words, self.n)
        v = self.ap[:, self.off:self.off + words]
        self.off += words
        if dt != F32:
            v = v.bitcast(dt)
        v = v[:, 0:n]
        if len(shape) == 2:
            v = v.rearrange("p (a b) -> p a b", b=shape[1])
        elif len(shape) == 3:
            v = v.rearrange("p (a b c) -> p a b c", b=shape[1], c=shape[2])
        elif len(shape) == 4:
            v = v.rearrange("p (a b c d) -> p a b c d", b=shape[1], c=shape[2], d=shape[3])
        return v


def build_program(NT=16, L=4, taps=(), max_stage=99, l0=0, l1=None, final=True):
    T = NT * 128
    TOPK = min(TOPK_MAX, T // 4)
    if l1 is None:
        l1 = L
    nc = bass.Bass("TRN2", target_bir_lowering=False)
    dr = {}

    def din(name, shape, dt=F32):
        dr[name] = nc.dram_tensor(name, shape, dt, kind="ExternalInput").ap()
        return dr[name]

    x_d = din("x", [T, D])
    pos_d = din("positions", [NT, 128], I32)
    n1_d = din("norm1_g", [L, D])
    win_d = din("w_in", [L, D, DIN])
    pw_d = din("pool_w", [L, 4, 64, 64])
    psc_d = din("pool_scale", [L, 256])
    lbl_d = din("lb_logits", [L, 256])
    hng_d = din("hgrn_norm_g", [L, 256])
    wout_d = din("w_out", [L, D, D])
    n2_d = din("norm2_g", [L, D])
    w1_d = din("w_ff_in", [L, D, DFF])
    w2_d = din("w_ff_out", [L, DFF, D])
    fg_d = din("final_norm_g", [D])
    out_d = nc.dram_tensor("out", [T, D], F32, kind="ExternalOutput").ap()
    tap_d = {}
    for (tname, tshape) in taps:
        tap_d[tname] = nc.dram_tensor("tap_" + tname, tshape, F32, kind="ExternalOutput").ap()

    S = Sched(nc)
    S.max_stage = max_stage
    with contextlib.ExitStack() as st:
        def sb(name, shape, dt=F32):
            return st.enter_context(nc.sbuf_tensor(name, shape, dt))

        x_sb = sb("x_sb", [128, NT, D])
        xR = [Res("x%d" % i) for i in range(NT)]
        gB = sb("gB", [128, D])
        gBR = Res("gB")
        ident_f = sb("ident_f", [128, 128])
        ident_b = sb("ident_b", [128, 128], BF16)
        identBIG4 = sb("identBIG4", [128, 4, 128], BF16)
        ltri = sb("ltri", [128, 128])
        lrem = sb("lrem", [128, 128])
        cmask = sb("cmask", [128, 128])
        chunkind = sb("chunkind", [128, 2])
        pow2tab = sb("pow2tab", [128, NBIS])
        corr = sb("corr", [128, 2, 16])
        cs_sb = sb("cs_sb", [128, NT, 8])
        sn_sb = sb("sn_sb", [128, NT, 8])
        poolW = sb("poolW", [128, L, 2, 128], BF16)
        pscol = sb("pscol", [128, L, 2])
        hngcol = sb("hngcol", [128, L, 2])
        pB = sb("pB", [128, L, 256])
        lbB = sb("lbB", [128, 256])
        omlbB = sb("omlbB", [128, 256])
        cR = Res("consts")
        pwR = Res("poolW"); colR = Res("cols"); pBR = Res("pB"); ropeR = Res("rope")
        lbR = Res("lb")

        arenaA_t = sb("arenaA", [128, 9728])
        AA = Arena(arenaA_t, 9728)

        def psb(name, shape):
            return st.enter_context(nc.psum_tensor(name, shape, F32))
        B0 = psb("B0", [128, 512]); B1 = psb("B1", [128, 512])
        B23 = psb("B23", [128, 1024])
        B4 = psb("B4", [128, 512]); B5 = psb("B5", [128, 512])
        B6 = psb("B6", [128, 512]); B7 = psb("B7", [128, 512])
        B0, B1, B23, B4, B5, B6, B7 = [t_[:, :] for t_ in (B0, B1, B23, B4, B5, B6, B7)]
        B2 = B23[:, 0:512]; B3 = B23[:, 512:1024]
        R0, R1, R23, R4, R5, R6, R7 = [Res("B%d" % i) for i in (0, 1, 23, 4, 5, 6, 7)]

        def bf(ap_f32):
            return ap_f32.bitcast(BF16)

        nB = int(nc.sbuf_bytes_remaining) // 4 - 16
        arenaB_t = sb("arenaB", [128, nB])
        AB = Arena(arenaB_t, nB)

        def dve(fn, r=(), w=()): return S.add(DVE, fn, r, w)
        def act(fn, r=(), w=()): return S.add(ACT, fn, r, w)
        def pool(fn, r=(), w=()): return S.add(POOL, fn, r, w)
        def pe(fn, r=(), w=()): return S.add(PE, fn, r, w)
        def dma(q, fn, r=(), w=(), key=None, group=True): return S.add(q, fn, r, w, is_dma=True, sem_key=key, group=group)

        def tap(name, ap, res):
            if name in tap_d:
                dma(POOL, lambda e: e.dma_start(out=tap_d[name], in_=ap), r=res, w=(), key="tap")

        pool(lambda e: e.memset(ident_f[:], 0.0), w=[cR])
        pool(lambda e: e.affine_select(out=ident_f[:], in_=ident_f[:], pattern=[[-1, 128]], compare_op=ALU.not_equal,
                                       fill=1.0, base=0, channel_multiplier=1), r=[cR], w=[cR])
        pool(lambda e: e.tensor_copy(out=ident_b[:], in_=ident_f[:]), r=[cR], w=[cR])
        for h4 in range(4):
            pool(lambda e, h4=h4: e.tensor_scalar(out=identBIG4[:, h4, :], in0=ident_f[:], scalar1=BIG, scalar2=None,
                                                   op0=ALU.mult), r=[cR], w=[cR])
        pool(lambda e: e.memset(ltri[:], 1.0), w=[cR])
        pool(lambda e: e.affine_select(out=ltri[:], in_=ltri[:], pattern=[[1, 128]], compare_op=ALU.is_ge, fill=0.0,
                                       base=0, channel_multiplier=-1), r=[cR], w=[cR])
        pool(lambda e: e.memset(ltri[0:64, 64:128], 0.0), r=[cR], w=[cR])
        pool(lambda e: e.memset(lrem[:], 1.0), w=[cR])
        pool(lambda e: e.affine_select(out=lrem[:], in_=lrem[:], pattern=[[-1, 128]], compare_op=ALU.is_gt, fill=0.0,
                                       base=0, channel_multiplier=1), r=[cR], w=[cR])
        pool(lambda e: e.memset(lrem[64:128, 0:64], 0.0), r=[cR], w=[cR])
        pool(lambda e: e.memset(cmask[:], 0.0), w=[cR])
        pool(lambda e: e.affine_select(out=cmask[:], in_=cmask[:], pattern=[[-1, 128]], compare_op=ALU.is_ge, fill=-1e30,
                                       base=0, channel_multiplier=1), r=[cR], w=[cR])
        pool(lambda e: e.memset(chunkind[:], 0.0), w=[cR])
        pool(lambda e: e.memset(chunkind[0:64, 0:1], 1.0), r=[cR], w=[cR])
        pool(lambda e: e.memset(chunkind[64:128, 1:2], 1.0), r=[cR], w=[cR])
        for k in range(NBIS):
            pool(lambda e, k=k: e.memset(pow2tab[:, k:k + 1], 2.0 ** (-k)), w=[cR])
        pool(lambda e: e.memset(corr[:], 1.0), w=[cR])
        for g, win in enumerate((2, 4, 8, 16)):
            ct, ph = g // 2, (g % 2) * 64
            for t in range(win - 1):
                pool(lambda e, ct=ct, ph=ph, t=t, win=win: e.memset(corr[ph:ph + 64, ct, t:t + 1], float(win) / (t + 1)),
                     r=[cR], w=[cR])

        pool(lambda e: e.memset(poolW[:], 0.0), w=[pwR])
        for l in range(L):
            for g in range(4):
                ct, ph = g // 2, (g % 2) * 64
                dma(POOL, lambda e, l=l, g=g, ct=ct, ph=ph: e.dma_start(out=poolW[ph:ph + 64, l, ct, ph:ph + 64], in_=pw_d[l, g]),
                    w=[pwR], key="c_pw")
        dma(SP, lambda e: e.dma_start(out=pscol[:], in_=psc_d.rearrange("l (c p) -> p l c", p=128), allow_slow_non_contiguous=True), w=[colR], key="c_col")
        dma(SP, lambda e: e.dma_start(out=hngcol[:], in_=hng_d.rearrange("l (c p) -> p l c", p=128), allow_slow_non_contiguous=True), w=[colR], key="c_col")
        dma(SP, lambda e: e.dma_start(out=pB[:], in_=lbl_d.rearrange("(o l) c -> o l c", o=1).to_broadcast([128, L, 256])), w=[pBR], key="c_pB")
        pos_i = AB.take([NT], I32)
        pos_f = AB.take([NT], F32)
        ang = AB.take([NT, 8], F32)
        angr = AB.take([NT, 8], F32)
        dma(SP, lambda e: e.dma_start(out=pos_i[:], in_=pos_d.rearrange("i p -> p i"), allow_slow_non_contiguous=True), w=[ropeR], key="c_pos")
        dve(lambda e: e.tensor_copy(out=pos_f[:], in_=pos_i[:]), r=[ropeR], w=[ropeR])
        for j in range(8):
            inv = float(np.float32(500000.0) ** np.float32(-(2.0 * j) / 16.0))
            dve(lambda e, j=j, inv=inv: e.tensor_scalar(out=ang[:, :, j], in0=pos_f[:], scalar1=inv, scalar2=None, op0=ALU.mult),
                r=[ropeR], w=[ropeR])
        TWO_PI = 2.0 * np.pi
        kf = AB.take([NT, 8], F32)
        ki_ = AB.take([NT, 8], I32)
        tt = AB.take([NT, 8], F32)
        for (dst, shift) in ((sn_sb, 0.0), (cs_sb, 0.5 * np.pi)):
            dve(lambda e, shift=shift: e.tensor_scalar(out=angr[:], in0=ang[:], scalar1=float(shift), scalar2=None, op0=ALU.add), r=[ropeR], w=[ropeR])
            dve(lambda e: e.tensor_scalar(out=kf[:], in0=angr[:], scalar1=float(1.0 / TWO_PI), scalar2=None, op0=ALU.mult), r=[ropeR], w=[ropeR])
            dve(lambda e: e.tensor_copy(out=ki_[:], in_=kf[:]), r=[ropeR], w=[ropeR])
            dve(lambda e: e.tensor_copy(out=kf[:], in_=ki_[:]), r=[ropeR], w=[ropeR])
            dve(lambda e: e.scalar_tensor_tensor(out=angr[:], in0=kf[:], scalar=float(-TWO_PI), in1=angr[:], op0=ALU.mult, op1=ALU.add), r=[ropeR], w=[ropeR])
            dve(lambda e: e.tensor_scalar(out=tt[:], in0=angr[:], scalar1=float(np.pi), scalar2=float(-TWO_PI), op0=ALU.is_gt, op1=ALU.mult), r=[ropeR], w=[ropeR])
            dve(lambda e: e.tensor_tensor(out=angr[:], in0=angr[:], in1=tt[:], op=ALU.add), r=[ropeR], w=[ropeR])
            dve(lambda e: e.tensor_scalar(out=tt[:], in0=angr[:], scalar1=float(-np.pi), scalar2=float(TWO_PI), op0=ALU.is_lt, op1=ALU.mult), r=[ropeR], w=[ropeR])
            dve(lambda e: e.tensor_tensor(out=angr[:], in0=angr[:], in1=tt[:], op=ALU.add), r=[ropeR], w=[ropeR])
            dve(lambda e: e.tensor_scalar(out=angr[:], in0=angr[:], scalar1=3.141592, scalar2=-3.141592, op0=ALU.min, op1=ALU.max), r=[ropeR], w=[ropeR])
            act(lambda e, dst=dst: e.activation(out=dst[:], in_=angr[:], func=AF.Sin), r=[ropeR], w=[ropeR])
        mx = AB.take([256], F32)
        zs = AB.take([256], F32)
        dve(lambda e: e.tensor_copy(out=mx[:], in_=pB[:, 0, :]), r=[pBR], w=[pBR])
        for l in range(1, L):
            dve(lambda e, l=l: e.tensor_tensor(out=mx[:], in0=mx[:], in1=pB[:, l, :], op=ALU.max), r=[pBR], w=[pBR])
        for l in range(L):
            dve(lambda e, l=l: e.tensor_tensor(out=pB[:, l, :], in0=pB[:, l, :], in1=mx[:], op=ALU.subtract), r=[pBR], w=[pBR])
        act(lambda e: e.activation(out=pB[:], in_=pB[:], func=AF.Exp), r=[pBR], w=[pBR])
        dve(lambda e: e.tensor_copy(out=zs[:], in_=pB[:, 0, :]), r=[pBR], w=[pBR])
        for l in range(1, L):
            dve(lambda e, l=l: e.tensor_tensor(out=zs[:], in0=zs[:], in1=pB[:, l, :], op=ALU.add), r=[pBR], w=[pBR])
        dve(lambda e: e.reciprocal(out=zs[:], in_=zs[:]), r=[pBR], w=[pBR])
        for l in range(L):
            dve(lambda e, l=l: e.tensor_tensor(out=pB[:, l, :], in0=pB[:, l, :], in1=zs[:], op=ALU.mult), r=[pBR], w=[pBR])

        for i in range(NT):
            q = SP if i % 2 == 0 else ACT
            dma(q, lambda e, i=i: e.dma_start(out=x_sb[:, i, :], in_=x_d[i * 128:(i + 1) * 128, :]), w=[xR[i]], key="x%d" % i)

        def rmsnorm_to_hT(i, hT_dst, hT_res, bank, bank_res, tmp):
            st_, hn, stR, hnR = tmp
            act(lambda e: e.activation(out=hn[:], in_=x_sb[:, i, :], func=AF.Square, accum_out=st_[:, 0:1]),
                r=[xR[i]], w=[stR, hnR])
            act(lambda e: e.activation(out=st_[:, 1:2], in_=st_[:, 0:1], func=AF.Ln, scale=1.0 / D, bias=EPS), r=[stR], w=[stR])
            act(lambda e: e.activation(out=st_[:, 2:3], in_=st_[:, 1:2], func=AF.Exp, scale=-0.5), r=[stR], w=[stR])
            dve(lambda e: e.scalar_tensor_tensor(out=hn[:], in0=x_sb[:, i, :], scalar=st_[:, 2:3], in1=gB[:],
                                                 op0=ALU.mult, op1=ALU.mult), r=[xR[i], stR, gBR], w=[hnR])
            pT = bf(bank).rearrange("p (k t) -> p k t", t=128)
            for k in range(8):
                pe(lambda e, k=k: e.transpose(out=pT[:, k, :], in_=hn[:, k * 128:(k + 1) * 128], identity=ident_b[:]),
                   r=[hnR, cR], w=[bank_res])
            act(lambda e: e.activation(out=hT_dst, in_=pT[:, 0:8, :], func=AF.Copy), r=[bank_res], w=[hT_res])

        AA.reset()
        w_in = AA.take([8, DIN], BF16)
        winR = Res("w_in")

        def load_win(l):
            wsrc = win_d[l].rearrange("(c p) n -> p c n", p=128)
            for k in range(8):
                dma(POOL, lambda e, k=k: e.dma_start(out=w_in[:, k, 0:1920], in_=wsrc[:, k, 0:1920]), w=[winR], key="win")
            dma(POOL, lambda e: e.dma_start(out=w_in[:, :, 1920:2240], in_=wsrc[:, :, 2048:2368]), w=[winR], key="win")
            dma(POOL, lambda e: e.dma_start(out=w_in[:, :, 2240:2368], in_=wsrc[:, :, 1920:2048]), w=[winR], key="win")
            dma(POOL, lambda e: e.dma_start(out=w_in[:, :, 2368:2372], in_=wsrc[:, :, 2368:2372]), w=[winR], key="win")

        load_win(l0)
        for l in range(l0):
            if l == 0:
                dve(lambda e: e.memset(lbB[:], 0.0), w=[lbR])
            else:
                dve(lambda e, l=l: e.tensor_tensor(out=lbB[:], in0=lbB[:], in1=pB[:, l, :], op=ALU.add), r=[pBR, lbR], w=[lbR])
        for l in range(l0, l1):
            S.stage(0)
            S.barrier()
            AB.reset()
            w_out = AB.take([8, D], BF16)
            woutR = Res("w_out")
            wosrc = wout_d[l].rearrange("(c p) n -> p c n", p=128)
            for k in range(8):
                dma(POOL, lambda e, k=k: e.dma_start(out=w_out[:, k, :], in_=wosrc[:, k, :]), w=[woutR], key="wout")
            dma(SP, lambda e: e.dma_start(out=gB[:], in_=n1_d[l:l + 1, :].to_broadcast([128, D])), w=[gBR], key="g")
            if l == 0:
                dve(lambda e: e.memset(lbB[:], 0.0), w=[lbR])
            else:
                dve(lambda e, l=l: e.tensor_tensor(out=lbB[:], in0=lbB[:], in1=pB[:, l, :], op=ALU.add), r=[pBR, lbR], w=[lbR])
            dve(lambda e: e.tensor_scalar(out=omlbB[:], in0=lbB[:], scalar1=-1.0, scalar2=1.0, op0=ALU.mult, op1=ALU.add),
                r=[lbR], w=[lbR])

            KT = AB.take([T], BF16); KTR = Res("KT")
            Vs = AB.take([NT, 2, 65], BF16); VR = Res("V")
            kiT = AB.take([T // 2], F32); kiTR = Res("kiT")
            I_sb = AB.take([T], F32); IR = Res("I")
            Mb = AB.take([T], BF16); MbR = Res("Mb")
            Rh = [AB.take([512], F32) for _ in range(2)]; RhR = [Res("Rh0"), Res("Rh1")]
            PT = [AB.take([512], BF16) for _ in range(3)]; PTR = [Res("PT%d" % j) for j in range(3)]
            st_ = AB.take([8], F32); stR = Res("st")
            hn = AB.take([D], BF16); hnR = Res("hn")
            hT = AB.take([8, 128], BF16); hTR = Res("hT")
            mixT = AB.take([8, 128], BF16); mixR = Res("mixT")
            uext = [AB.take([2, 144], F32) for _ in range(2)]; uR = [Res("u0"), Res("u1")]
            slv = [AB.take([2, 144], F32) for _ in range(4)]; sR = Res("slv")
            pooled = AB.take([2, 128], BF16); pooledR = Res("pooled")
            H = {n: AB.take([256], F32) for n in ("t0", "t1", "f", "kk", "eb", "enb", "ebl", "gate")}
            HR = {n: Res("h_" + n) for n in H}
            H["logf"] = H["f"]; HR["logf"] = HR["f"]
            H["q"] = H["t1"]; HR["q"] = HR["t1"]
            qt_b = AB.take([256], BF16); kt_b = AB.take([256], BF16)
            khA = AB.take([256], BF16); khB = AB.take([256], BF16); v_b = AB.take([256], BF16)
            hbR = Res("h_bf"); qtR = Res("h_qt"); ktR = Res("h_kt"); vbR = Res("h_v")
            qTf = AB.take([4, 128], BF16); qTA = AB.take([4, 128], BF16); qTB = AB.take([4, 128], BF16); kTs = AB.take([4, 128], BF16)
            hTR2 = Res("h_T")
            Am = AB.take([4, 128], BF16); AmR = Res("Am")
            dec = AB.take([8], F32); decR = Res("dec")
            S32 = AB.take([4, 64], F32); Sbf = [AB.take([4, 64], BF16) for _ in range(2)]; Stmp = AB.take([4, 64], F32)
            SR = Res("S")
            hst = AB.take([16], F32)
            osb = H["enb"]; osq = H["t0"]; og = H["eb"]
            oR = Res("o")
            qk_b = AB.take([10, 64], BF16); qiki = AB.take([5, 64], F32)
            qk_flat = qk_b.rearrange("p h d -> p (h d)")
            rt = [AB.take([15, 8], F32) for _ in range(4)]
            ws = AB.take([4], F32)
            aR = Res("attn_tm")
            qidup = AB.take([4, 128], F32); kidup = AB.take([128], F32)
            qT_sb = AB.take([4, 128], BF16); qiT_sb = AB.take([4, 128], F32)
            aTR = Res("attn_T")
            bis = AB.take([32], F32); bisR = Res("bis")
            stepcol = AB.take([NBIS], F32)
            rden = AB.take([8], F32); ya = hn[:, 0:512]; yaR = hnR
            pool(lambda e: e.memset(uext[0][:, :, 0:16], 0.0), w=[uR[0]])
            pool(lambda e: e.memset(S32[0:64], 0.0), w=[SR])
            pool(lambda e: e.memset(Sbf[1][0:64], 0.0), w=[SR])
            pool(lambda e: e.memset(qTA[0:64], 0.0), w=[hTR2])
            pool(lambda e: e.memset(qTB[0:64], 0.0), w=[hTR2])
            pool(lambda e: e.memset(khA[64:128, :], 0.0), w=[hbR])
            pool(lambda e: e.memset(khB[0:64, :], 0.0), w=[hbR])
            pool(lambda e: e.memset(Vs[:, :, :, 64:65], 1.0), w=[VR])

            for i in range(NT):
                T0 = i * 128
                S.stage(1)
                rmsnorm_to_hT(i, hT[:], hTR, B0, R0, (st_, hn, stR, hnR))

                S.stage(1.5)
                pu = B1[:, 0:256].rearrange("p (c t) -> p c t", t=128)
                for ct in range(2):
                    for k in range(8):
                        pe(lambda e, ct=ct, k=k: e.matmul(pu[:, ct, :], lhsT=w_in[:, k, ct * 128:(ct + 1) * 128], rhs=hT[:, k, :],
                                                           start=(k == 0), stop=(k == 7)), r=[winR, hTR], w=[R1])
                ue = uext[i % 2]; un = uext[(i + 1) % 2]
                act(lambda e, ue=ue: e.activation(out=ue[:, :, 16:144], in_=pu, func=AF.Copy), r=[R1], w=[uR[i % 2]])
                dve(lambda e, ue=ue, un=un: e.tensor_copy(out=un[:, :, 0:16], in_=ue[:, :, 128:144]), r=[uR[i % 2]], w=[uR[(i + 1) % 2]])
                prev = ue
                S.stage(1.6)
                for lv, sh in enumerate((1, 2, 4, 8)):
                    lo = 2 * sh - 1
                    dve(lambda e, lv=lv, sh=sh, lo=lo, prev=prev: e.tensor_tensor(out=slv[lv][:, :, lo:144], in0=prev[:, :, lo:144],
                                                                                   in1=prev[:, :, lo - sh:144 - sh], op=ALU.add),
                         r=[uR[i % 2], sR], w=[sR])
                    prev = slv[lv]
                S.stage(1.7)
                for g, win in enumerate((2, 4, 8, 16)):
                    ct, ph = g // 2, (g % 2) * 64
                    if i == 0:
                        dve(lambda e, g=g, ct=ct, ph=ph: e.tensor_tensor(out=slv[g][ph:ph + 64, ct, 16:32], in0=slv[g][ph:ph + 64, ct, 16:32],
                                                                          in1=corr[ph:ph + 64, ct, :], op=ALU.mult), r=[sR, cR], w=[sR])
                    dve(lambda e, g=g, ct=ct, ph=ph, win=win, ue=ue: e.scalar_tensor_tensor(
                        out=pooled[ph:ph + 64, ct, :], in0=slv[g][ph:ph + 64, ct, 16:144], scalar=1.0 / win, in1=ue[ph:ph + 64, ct, 16:144],
                        op0=ALU.mult, op1=ALU.subtract), r=[sR, uR[i % 2]], w=[pooledR])
                S.stage(1.8)
                py = B1[:, 256:512].rearrange("p (c t) -> p c t", t=128)
                for ct in range(2):
                    pe(lambda e, ct=ct: e.matmul(py[:, ct, :], lhsT=poolW[:, l, ct, :], rhs=pooled[:, ct, :], start=True, stop=True),
                       r=[pwR, pooledR], w=[R1])
                S.stage(1.9)
                for ct in range(2):
                    act(lambda e, ct=ct: e.activation(out=mixT[:, ct, :], in_=py[:, ct, :], func=AF.Identity, scale=pscol[:, l, ct:ct + 1]),
                        r=[R1, colR], w=[mixR])

                S.stage(2)
                for j, bank in enumerate((B2, B3)):
                    for k in range(8):
                        pe(lambda e, j=j, k=k, bank=bank: e.matmul(bank, lhsT=hT[:, k, :], rhs=w_in[:, k, 256 + j * 512:256 + (j + 1) * 512],
                                                                   start=(k == 0), stop=(k == 7)), r=[hTR, winR], w=[R23])
                hq_p, hf_p, hi_p, hg_p = B2[:, 0:256], B2[:, 256:512], B3[:, 0:256], B3[:, 256:512]
                act(lambda e: e.activation(out=H["t0"], in_=hf_p, func=AF.Exp, scale=-1.0), r=[R23], w=[HR["t0"]])
                act(lambda e: e.activation(out=H["t1"], in_=hq_p, func=AF.Exp, scale=-1.0), r=[R23], w=[HR["t1"]])
                act(lambda e: e.activation(out=H["gate"], in_=hg_p, func=AF.Exp, scale=-1.0), r=[R23], w=[HR["gate"]])
                act(lambda e: e.activation(out=v_b, in_=hi_p, func=AF.Copy), r=[R23], w=[vbR])
                dve(lambda e: e.tensor_scalar(out=H["t0"], in0=H["t0"], scalar1=1.0, scalar2=None, op0=ALU.add), r=[HR["t0"]], w=[HR["t0"]])
                dve(lambda e: e.reciprocal(out=H["t0"], in_=H["t0"]), r=[HR["t0"]], w=[HR["t0"]])
                dve(lambda e: e.tensor_tensor(out=H["f"], in0=H["t0"], in1=omlbB[:], op=ALU.mult), r=[HR["t0"], lbR], w=[HR["f"]])
                dve(lambda e: e.tensor_tensor(out=H["f"], in0=H["f"], in1=lbB[:], op=ALU.add), r=[HR["f"], lbR], w=[HR["f"]])
                dve(lambda e: e.tensor_scalar(out=H["kk"], in0=H["f"], scalar1=-1.0, scalar2=1.0, op0=ALU.mult, op1=ALU.add),
                     r=[HR["f"]], w=[HR["kk"]])
                dve(lambda e: e.tensor_scalar(out=H["f"], in0=H["f"], scalar1=1e-30, scalar2=None, op0=ALU.max), r=[HR["f"], HR["kk"]], w=[HR["f"]])
                act(lambda e: e.activation(out=H["logf"], in_=H["f"], func=AF.Ln), r=[HR["f"]], w=[HR["logf"]])
                pe(lambda e: e.matmul(B4[:, 0:256], lhsT=ltri[:], rhs=H["logf"], start=True, stop=True), r=[cR, HR["logf"]], w=[R4])
                pe(lambda e: e.matmul(B4[:, 256:512], lhsT=lrem[:], rhs=H["logf"], start=True, stop=True), r=[cR, HR["logf"]], w=[R4])
                dl = B0[0:64, 0:8].rearrange("p (h c) -> p h c", c=2)
                for h in range(4):
                    pe(lambda e, h=h: e.matmul(dl[:, h, :], lhsT=H["logf"][:, h * 64:(h + 1) * 64], rhs=chunkind[:], start=True, stop=True),
                       r=[cR, HR["logf"]], w=[R0])
                act(lambda e: e.activation(out=H["eb"], in_=B4[:, 0:256], func=AF.Exp), r=[R4], w=[HR["eb"]])
                act(lambda e: e.activation(out=H["enb"], in_=B4[:, 0:256], func=AF.Exp, scale=-1.0), r=[R4], w=[HR["enb"]])
                act(lambda e: e.activation(out=H["ebl"], in_=B4[:, 256:512], func=AF.Exp), r=[R4], w=[HR["ebl"]])
                act(lambda e: e.activation(out=dec[0:64].rearrange("p (h c) -> p h c", c=2), in_=dl, func=AF.Exp), r=[R0], w=[decR])
                dve(lambda e: e.tensor_scalar(out=H["t1"], in0=H["t1"], scalar1=1.0, scalar2=None, op0=ALU.add), r=[HR["t1"]], w=[HR["t1"]])
                dve(lambda e: e.reciprocal(out=H["t1"], in_=H["t1"]), r=[HR["t1"]], w=[HR["t1"]])
                dve(lambda e: e.tensor_tensor(out=H["q"], in0=hq_p, in1=H["t1"], op=ALU.mult), r=[R23, HR["t1"]], w=[HR["q"]])
                dve(lambda e: e.tensor_scalar(out=H["gate"], in0=H["gate"], scalar1=1.0, scalar2=None, op0=ALU.add), r=[HR["gate"]], w=[HR["gate"]])
                dve(lambda e: e.reciprocal(out=H["gate"], in_=H["gate"]), r=[HR["gate"]], w=[HR["gate"]])
                dve(lambda e: e.tensor_tensor(out=H["gate"], in0=hg_p, in1=H["gate"], op=ALU.mult), r=[R23, HR["gate"]], w=[HR["gate"]])
                dve(lambda e: e.tensor_tensor(out=qt_b, in0=H["q"], in1=H["eb"], op=ALU.mult), r=[HR["q"], HR["eb"]], w=[qtR])
                dve(lambda e: e.tensor_tensor(out=kt_b, in0=H["kk"], in1=H["enb"], op=ALU.mult), r=[HR["kk"], HR["enb"]], w=[ktR])
                dve(lambda e: e.tensor_tensor(out=khA[0:64, :], in0=H["kk"][0:64, :], in1=H["ebl"][0:64, :], op=ALU.mult),
                     r=[HR["kk"], HR["ebl"]], w=[hbR])
                dve(lambda e: e.tensor_tensor(out=khB[64:128, :], in0=H["kk"][64:128, :], in1=H["ebl"][64:128, :], op=ALU.mult),
                     r=[HR["kk"], HR["ebl"]], w=[hbR])
                tq = bf(B5)[0:64, 0:512].rearrange("p (h t) -> p h t", t=128)
                tk = bf(B5)[0:64, 512:1024].rearrange("p (h t) -> p h t", t=128)
                for h in range(4):
                    pe(lambda e, h=h: e.transpose(out=tq[:, h, :], in_=qt_b[:, h * 64:(h + 1) * 64], identity=ident_b[:]), r=[qtR, cR], w=[R5])
                for h in range(4):
                    pe(lambda e, h=h: e.transpose(out=tk[:, h, :], in_=kt_b[:, h * 64:(h + 1) * 64], identity=ident_b[:]), r=[ktR, cR], w=[R5])
                act(lambda e: e.activation(out=qTf[0:64], in_=tq, func=AF.Copy), r=[R5], w=[hTR2])
                dve(lambda e: e.tensor_copy(out=qTA[0:64, :, 0:64], in_=tq[:, :, 0:64]), r=[R5], w=[hTR2])
                dve(lambda e: e.tensor_copy(out=qTB[0:64, :, 64:128], in_=tq[:, :, 64:128]), r=[R5], w=[hTR2])
                act(lambda e: e.activation(out=kTs[0:64], in_=tk, func=AF.Copy), r=[R5], w=[hTR2])
                pA = B6.rearrange("p (h t) -> p h t", t=128)
                for h in range(4):
                    pe(lambda e, h=h: e.matmul(pA[:, h, :], lhsT=kTs[0:64, h, :], rhs=qTf[0:64, h, :], start=True, stop=True), r=[hTR2], w=[R6])
                dve(lambda e: e.tensor_tensor(out=Am, in0=pA, in1=ltri[:].unsqueeze(1).to_broadcast([128, 4, 128]), op=ALU.mult),
                    r=[R6, cR], w=[AmR])
                pKV = B7[0:64, :].rearrange("p (c h v) -> p c h v", c=2, h=4)
                for c, kh in enumerate((khA, khB)):
                    for h in range(4):
                        pe(lambda e, c=c, h=h, kh=kh: e.matmul(pKV[:, c, h, :], lhsT=kh[:, h * 64:(h + 1) * 64], rhs=v_b[:, h * 64:(h + 1) * 64],
                                                               start=True, stop=True), r=[hbR, vbR], w=[R7])
                decv = dec[0:64].rearrange("p (h c) -> p h c", c=2)
                for c in range(2):
                    dve(lambda e, c=c: e.tensor_tensor(out=Stmp[0:64], in0=S32[0:64], in1=decv[:, :, c:c + 1].to_broadcast([64, 4, 64]), op=ALU.mult),
                        r=[SR, decR], w=[SR])
                    dve(lambda e, c=c: e.tensor_tensor(out=S32[0:64], in0=pKV[:, c], in1=Stmp[0:64], op=ALU.add), r=[SR, R7], w=[SR])
                    if c == 0:
                        dve(lambda e: e.tensor_copy(out=Sbf[0][0:64], in_=S32[0:64]), r=[SR], w=[SR])
                        po = B4[:, 0:256].rearrange("p (h v) -> p h v", v=64)
                        for h in range(4):
                            pe(lambda e, h=h: e.matmul(po[:, h, :], lhsT=Am[:, h, :], rhs=v_b[:, h * 64:(h + 1) * 64], start=True, stop=False),
                               r=[AmR, vbR], w=[R4])
                            pe(lambda e, h=h: e.matmul(po[:, h, :], lhsT=qTA[0:64, h, :], rhs=Sbf[1][0:64, h, :], start=False, stop=False),
                               r=[hTR2, SR], w=[R4])
                            pe(lambda e, h=h: e.matmul(po[:, h, :], lhsT=qTB[0:64, h, :], rhs=Sbf[0][0:64, h, :], start=False, stop=True),
                               r=[hTR2, SR], w=[R4])
                    else:
                        dve(lambda e: e.tensor_copy(out=Sbf[1][0:64], in_=S32[0:64]), r=[SR], w=[SR])
                act(lambda e: e.activation(out=osb, in_=B4[:, 0:256], func=AF.Copy), r=[R4], w=[oR, HR["enb"]])
                dve(lambda e: e.tensor_tensor(out=osq, in0=osb, in1=osb, op=ALU.mult), r=[oR], w=[oR, HR["t0"]])
                dve(lambda e: e.tensor_reduce(out=hst[:, 0:4], in_=osq.rearrange("p (h v) -> p h v", v=64), axis=AX.X, op=ALU.add), r=[oR], w=[oR])
                act(lambda e: e.activation(out=hst[:, 4:8], in_=hst[:, 0:4], func=AF.Ln, scale=1.0 / 64, bias=EPS), r=[oR], w=[oR])
                act(lambda e: e.activation(out=hst[:, 8:12], in_=hst[:, 4:8], func=AF.Exp, scale=-0.5), r=[oR], w=[oR])
                dve(lambda e: e.tensor_tensor(out=og.rearrange("p (h v) -> p h v", v=64), in0=osb.rearrange("p (h v) -> p h v", v=64),
                                              in1=hst[:, 8:12].unsqueeze(2).to_broadcast([128, 4, 64]), op=ALU.mult), r=[oR], w=[oR, HR["eb"]])
                dve(lambda e: e.tensor_tensor(out=og, in0=og, in1=H["gate"], op=ALU.mult), r=[oR, HR["gate"]], w=[oR])
                pg = B5[:, 0:256].rearrange("p (c t) -> p c t", t=128)
                for c in range(2):
                    pe(lambda e, c=c: e.transpose(out=pg[:, c, :], in_=og[:, c * 128:(c + 1) * 128], identity=ident_f[:]), r=[oR, cR], w=[R5])
                for c in range(2):
                    act(lambda e, c=c: e.activation(out=mixT[:, 2 + c, :], in_=pg[:, c, :], func=AF.Identity, scale=hngcol[:, l, c:c + 1]),
                        r=[R5, colR], w=[mixR])
                if i == NT - 1:
                    tap("S32_l%d" % l, S32[0:64], [SR])

                S.stage(3)
                for k in range(8):
                    pe(lambda e, k=k: e.matmul(B23[:, 0:512], lhsT=hT[:, k, :], rhs=w_in[:, k, 1280:1792], start=(k == 0), stop=(k == 7)),
                       r=[hTR, winR], w=[R23])
                for k in range(8):
                    pe(lambda e, k=k: e.matmul(B23[:, 512:960], lhsT=hT[:, k, :], rhs=w_in[:, k, 1792:2240], start=(k == 0), stop=(k == 7)),
                       r=[hTR, winR], w=[R23])
                for k in range(8):
                    pe(lambda e, k=k: e.matmul(B4[:, 0:132], lhsT=hT[:, k, :], rhs=w_in[:, k, 2240:2372], start=(k == 0), stop=(k == 7)),
                       r=[hTR, winR], w=[R4])
                pv = B23[:, 0:960].rearrange("p (h d) -> p h d", d=64)
                x1, x2 = pv[:, :, 0:8], pv[:, :, 8:16]
                cB = cs_sb[:, i, :].unsqueeze(1).to_broadcast([128, 15, 8])
                sB = sn_sb[:, i, :].unsqueeze(1).to_broadcast([128, 15, 8])
                dve(lambda e: e.tensor_tensor(out=rt[0], in0=x1, in1=cB, op=ALU.mult), r=[R23, ropeR], w=[aR])
                dve(lambda e: e.tensor_tensor(out=rt[1], in0=x2, in1=sB, op=ALU.mult), r=[R23, ropeR], w=[aR])
                dve(lambda e: e.tensor_tensor(out=rt[2], in0=x2, in1=cB, op=ALU.mult), r=[R23, ropeR], w=[aR])
                dve(lambda e: e.tensor_tensor(out=rt[3], in0=x1, in1=sB, op=ALU.mult), r=[R23, ropeR], w=[aR])
                qdst = qk_b[:, 0:8, :].rearrange("p (j k) d -> p k j d", k=2)
                dve(lambda e: e.tensor_tensor(out=qdst[:, :, :, 0:8], in0=rt[0][:, 0:8, :].rearrange("p (k j) d -> p k j d", k=2),
                                               in1=rt[1][:, 0:8, :].rearrange("p (k j) d -> p k j d", k=2), op=ALU.subtract), r=[aR], w=[aR])
                dve(lambda e: e.tensor_tensor(out=qdst[:, :, :, 8:16], in0=rt[2][:, 0:8, :].rearrange("p (k j) d -> p k j d", k=2),
                                               in1=rt[3][:, 0:8, :].rearrange("p (k j) d -> p k j d", k=2), op=ALU.add), r=[aR], w=[aR])
                dve(lambda e: e.tensor_tensor(out=qk_b[:, 8:10, 0:8], in0=rt[0][:, 8:10, :], in1=rt[1][:, 8:10, :], op=ALU.subtract), r=[aR], w=[aR])
                dve(lambda e: e.tensor_tensor(out=qk_b[:, 8:10, 8:16], in0=rt[2][:, 8:10, :], in1=rt[3][:, 8:10, :], op=ALU.add), r=[aR], w=[aR])
                dve(lambda e: e.tensor_tensor(out=qiki[:, :, 0:8], in0=rt[0][:, 10:15, :], in1=rt[1][:, 10:15, :], op=ALU.subtract), r=[aR], w=[aR])
                dve(lambda e: e.tensor_tensor(out=qiki[:, :, 8:16], in0=rt[2][:, 10:15, :], in1=rt[3][:, 10:15, :], op=ALU.add), r=[aR], w=[aR])
                act(lambda e: e.activation(out=qdst[:, :, :, 16:64], in_=pv[:, 0:8, 16:64].rearrange("p (k j) d -> p k j d", k=2), func=AF.Copy),
                    r=[R23], w=[aR])
                act(lambda e: e.activation(out=qk_b[:, 8:10, 16:64], in_=pv[:, 8:10, 16:64], func=AF.Copy), r=[R23], w=[aR])
                act(lambda e: e.activation(out=qiki[:, :, 16:64], in_=pv[:, 10:15, 16:64], func=AF.Copy), r=[R23], w=[aR])
                act(lambda e: e.activation(out=Vs[:, i, :, 0:64], in_=B4[:, 0:128].rearrange("p (k d) -> p k d", d=64), func=AF.Copy), r=[R4], w=[VR])
                act(lambda e: e.activation(out=ws, in_=B4[:, 128:132], func=AF.Copy, scale=1.0 / 16.0), r=[R4], w=[aR])
                for dd in range(2):
                    dve(lambda e, dd=dd: e.tensor_copy(out=qidup[:, :, dd * 64:(dd + 1) * 64], in_=qiki[:, 0:4, :]), r=[aR], w=[aR])
                    dve(lambda e, dd=dd: e.tensor_copy(out=kidup[:, dd * 64:(dd + 1) * 64], in_=qiki[:, 4, :]), r=[aR], w=[aR])
                tqa = bf(B5)[:, 0:512].rearrange("p (j t) -> p j t", t=128)
                tka = bf(B5)[:, 512:640]
                for j in range(4):
                    pe(lambda e, j=j: e.transpose(out=tqa[:, j, :], in_=qk_flat[:, j * 128:(j + 1) * 128], identity=ident_b[:]), r=[aR, cR], w=[R5])
                pe(lambda e: e.transpose(out=tka, in_=qk_flat[:, 512:640], identity=ident_b[:]), r=[aR, cR], w=[R5])
                tqi = B6.rearrange("p (h t) -> p h t", t=128)
                for h in range(4):
                    pe(lambda e, h=h: e.transpose(out=tqi[:, h, :], in_=qidup[:, h, :], identity=ident_f[:]), r=[aR, cR], w=[R6])
                tki = B7[:, 0:128]
                pe(lambda e: e.transpose(out=tki, in_=kidup, identity=ident_f[:]), r=[aR, cR], w=[R7])
                act(lambda e: e.activation(out=qT_sb, in_=tqa, func=AF.Copy), r=[R5], w=[aTR])
                act(lambda e: e.activation(out=KT[:, T0:T0 + 128], in_=tka, func=AF.Copy), r=[R5], w=[KTR])
                dve(lambda e: e.tensor_copy(out=qiT_sb, in_=tqi), r=[R6], w=[aTR])
                half = 0 if T0 < T // 2 else 64
                kcol = T0 - (0 if half == 0 else T // 2)
                dve(lambda e, half=half, kcol=kcol: e.tensor_copy(out=kiT[half:half + 64, kcol:kcol + 128], in_=tki[half:half + 64, :]),
                    r=[R7], w=[kiTR])
                S.stage(4)
                n = T0 + 128
                nblk = (n + 511) // 512
                sbanks = ((B0, R0), (B1, R1))
                cntj = 0
                segs = []
                for hh in range(2):
                    a0, a1 = hh * (T // 2), min(n, (hh + 1) * (T // 2))
                    k0 = a0
                    while k0 < a1:
                        segs.append((k0, min(512, a1 - k0), hh * 64, k0 - a0))
                        k0 += 512
                for (k0, wdt, kh_, kc0) in segs:
                    for h in range(4):
                        bank, bres = sbanks[cntj % 2]
                        rh, rhr = Rh[cntj % 2], RhR[cntj % 2]
                        cntj += 1
                        pe(lambda e, bank=bank, h=h, kh_=kh_, kc0=kc0, wdt=wdt: e.matmul(
                            bank[:, 0:wdt], lhsT=qiT_sb[kh_:kh_ + 64, h, :], rhs=kiT[kh_:kh_ + 64, kc0:kc0 + wdt], start=True, stop=True),
                           r=[aTR, kiTR], w=[bres])
                        if h == 0:
                            dve(lambda e, bank=bank, k0=k0, wdt=wdt: e.tensor_scalar(out=I_sb[:, k0:k0 + wdt], in0=bank[:, 0:wdt], scalar1=0.0,
                                                                                     scalar2=ws[:, 0:1], op0=ALU.max, op1=ALU.mult),
                                r=[bres, aR], w=[IR])
                        else:
                            act(lambda e, bank=bank, rh=rh, wdt=wdt: e.activation(out=rh[:, 0:wdt], in_=bank[:, 0:wdt], func=AF.Relu), r=[bres], w=[rhr])
                            dve(lambda e, rh=rh, h=h, k0=k0, wdt=wdt: e.scalar_tensor_tensor(
                                out=I_sb[:, k0:k0 + wdt], in0=rh[:, 0:wdt], scalar=ws[:, h:h + 1], in1=I_sb[:, k0:k0 + wdt],
                                op0=ALU.mult, op1=ALU.add), r=[rhr, aR, IR], w=[IR])
                S.stage(5)
                if n > TOPK:
                    dve(lambda e, n=n: e.tensor_reduce(out=bis[:, 0:1], in_=I_sb[:, 0:n], axis=AX.X, op=ALU.max, apply_absolute_value=True),
                        r=[IR], w=[bisR])
                dve(lambda e: e.tensor_tensor(out=I_sb[:, T0:T0 + 128], in0=I_sb[:, T0:T0 + 128], in1=cmask[:], op=ALU.add), r=[IR, cR], w=[IR])
                thr = bis[:, 1:2]
                if n > TOPK:
                    dve(lambda e: e.tensor_scalar(out=stepcol, in0=pow2tab[:], scalar1=bis[:, 0:1], scalar2=None, op0=ALU.mult), r=[bisR, cR], w=[bisR])
                    dve(lambda e: e.memset(thr, 0.0), r=[bisR], w=[bisR])
                    for k in range(NBIS):
                        dve(lambda e, n=n: e.tensor_scalar(out=Mb[:, 0:n], in0=I_sb[:, 0:n], scalar1=thr, scalar2=None, op0=ALU.is_ge, op1=ALU.add,
                                                            accum_out=bis[:, 2:3]), r=[IR, bisR], w=[MbR, bisR])
                        dve(lambda e: e.tensor_scalar(out=bis[:, 3:4], in0=bis[:, 2:3], scalar1=float(TOPK), scalar2=0.5, op0=ALU.is_ge, op1=ALU.subtract),
                            r=[bisR], w=[bisR])
                        dve(lambda e, k=k: e.scalar_tensor_tensor(out=thr, in0=bis[:, 3:4], scalar=stepcol[:, k:k + 1], in1=thr, op0=ALU.mult, op1=ALU.add),
                            r=[bisR], w=[bisR])
                    dve(lambda e: e.scalar_tensor_tensor(out=thr, in0=stepcol[:, NBIS - 1:NBIS], scalar=-0.5, in1=thr, op0=ALU.mult, op1=ALU.add),
                        r=[bisR], w=[bisR])
                else:
                    dve(lambda e: e.memset(thr, -1e29), r=[bisR], w=[bisR])
                dve(lambda e, n=n: e.tensor_scalar(out=Mb[:, 0:n], in0=I_sb[:, 0:n], scalar1=thr, scalar2=1.0, op0=ALU.is_ge, op1=ALU.subtract),
                    r=[IR, bisR], w=[MbR])
                if i == NT - 1 and l == 0:
                    tap("I", I_sb, [IR]); tap("bis", bis, [bisR]); tap("Mb", Mb, [MbR]); tap("qT", qT_sb, [aTR]); tap("KT", KT, [KTR])
                    tap("V", Vs, [VR]); tap("ws", ws, [aR]); tap("qiT", qiT_sb, [aTR]); tap("kiT", kiT, [kiTR])
                S.stage(6)
                sT = ((B4, R4), (B5, R5))
                pOT = (B6, B7)
                pOR = (R6, R7)
                pOq = (B0[:, 0:260].rearrange("p (h v) -> p h v", v=65), B1[:, 0:260].rearrange("p (h v) -> p h v", v=65))
                pOqR = (R0, R1)
                OTs = Rh[0]; OTsR = RhR[0]
                iters = [(kv, c) for kv in range(2) for c in range(i + 1)]

                def emit_qk(idx):
                    kv, c = iters[idx]
                    bank, bres = sT[idx % 2]
                    pt, ptr = PT[idx % 3], PTR[idx % 3]
                    pe(lambda e: e.matmul(bank, lhsT=KT[kv * 64:(kv + 1) * 64, c * 128:(c + 1) * 128],
                                          rhs=qT_sb[kv * 64:(kv + 1) * 64, :, :], start=True, stop=False), r=[KTR, aTR], w=[bres])
                    pe(lambda e: e.matmul(bank, lhsT=Mb[:, c * 128:(c + 1) * 128], rhs=identBIG4[:], start=False, stop=True),
                       r=[MbR, cR], w=[bres])
                    act(lambda e: e.activation(out=pt, in_=bank, func=AF.Exp, scale=0.125), r=[bres], w=[ptr])

                def emit_pv(idx):
                    kv, c = iters[idx]
                    pt, ptr = PT[idx % 3], PTR[idx % 3]
                    pe(lambda e: e.matmul(pOT[kv][0:65, :], lhsT=Vs[:, c, kv, :], rhs=pt, start=(c == 0), stop=(c == i)),
                       r=[ptr, VR], w=[pOR[kv]])
                    if c == i:
                        act(lambda e: e.activation(out=OTs[0:65, :], in_=pOT[kv][0:65, :], func=AF.Copy), r=[pOR[kv]], w=[OTsR])
                        for h4 in range(4):
                            pe(lambda e, h4=h4: e.transpose(out=pOq[kv][:, h4, :], in_=OTs[0:65, h4 * 128:(h4 + 1) * 128], identity=ident_f[0:65, 0:65]),
                               r=[OTsR, cR], w=[pOqR[kv]])
                        dve(lambda e: e.reciprocal(out=rden[:, kv * 4:(kv + 1) * 4], in_=pOq[kv][:, :, 64]), r=[pOqR[kv]], w=[yaR])
                        dve(lambda e: e.tensor_tensor(out=ya[:, kv * 256:(kv + 1) * 256].rearrange("p (h v) -> p h v", v=64), in0=pOq[kv][:, :, 0:64],
                                                      in1=rden[:, kv * 4:(kv + 1) * 4].unsqueeze(2).to_broadcast([128, 4, 64]), op=ALU.mult),
                            r=[pOqR[kv], yaR], w=[yaR])

                for idx in range(len(iters)):
                    emit_qk(idx)
                    if idx >= 1:
                        emit_pv(idx - 1)
                emit_pv(len(iters) - 1)
                if i == NT - 1 and l == 0:
                    tap("ya", ya, [yaR]); tap("rden", rden, [yaR])
                ty = bf(B0)[:, 0:512].rearrange("p (c t) -> p c t", t=128)
                for c in range(4):
                    pe(lambda e, c=c: e.transpose(out=ty[:, c, :], in_=ya[:, c * 128:(c + 1) * 128], identity=ident_b[:]), r=[yaR, cR], w=[R0])
                act(lambda e: e.activation(out=mixT[:, 4:8, :], in_=ty, func=AF.Copy), r=[R0], w=[mixR])
                S.stage(1)
                if ("mixT_l%d" % l) in tap_d:
                    dma(POOL, lambda e, i=i, l=l: e.dma_start(out=tap_d["mixT_l%d" % l][:, :, i * 128:(i + 1) * 128], in_=mixT), r=[mixR], key="tap")

                S.stage(7)
                for hf in range(2):
                    for k in range(8):
                        pe(lambda e, hf=hf, k=k: e.matmul(B23[:, hf * 512:(hf + 1) * 512], lhsT=mixT[:, k, :], rhs=w_out[:, k, hf * 512:(hf + 1) * 512],
                                                          start=(k == 0), stop=(k == 7)), r=[mixR, woutR], w=[R23])
                for hf in range(2):
                    dve(lambda e, hf=hf, i=i: e.tensor_tensor(out=x_sb[:, i, hf * 512:(hf + 1) * 512], in0=B23[:, hf * 512:(hf + 1) * 512],
                                                               in1=x_sb[:, i, hf * 512:(hf + 1) * 512], op=ALU.add), r=[R23, xR[i]], w=[xR[i]])

            if ("xmid_l%d" % l) in tap_d:
                for i in range(NT):
                    dma(SP, lambda e, i=i, l=l: e.dma_start(out=tap_d["xmid_l%d" % l][i * 128:(i + 1) * 128, :], in_=x_sb[:, i, :]), r=[xR[i]], key="tap")

            S.stage(8)
            S.barrier()
            AB.reset()
            h2T = AB.take([8, T], BF16); h2R = [Res("h2T%d" % i) for i in range(NT)]
            NCH = 8
            W1 = [AB.take([8, 512], BF16) for _ in range(2)]; W1R = [Res("W1_0"), Res("W1_1")]
            W2 = [AB.take([4, D], BF16) for _ in range(2)]; W2R = [Res("W2_0"), Res("W2_1")]
            aT = [AB.take([4, 512], BF16) for _ in range(2)]; aTRs = [[Res("aT%d_%d" % (q_, j_)) for j_ in range(4)] for q_ in range(2)]
            r32 = [AB.take([512], F32) for _ in range(4)]; r32R = [Res("r32_%d" % q_) for q_ in range(4)]
            st2 = AB.take([8], F32); st2R = Res("st2")
            hn2 = AB.take([D], BF16); hn2R = Res("hn2")
            dma(SP, lambda e: e.dma_start(out=gB[:], in_=n2_d[l:l + 1, :].to_broadcast([128, D])), w=[gBR], key="g")
            w1src = w1_d[l].rearrange("(c p) n -> p c n", p=128)
            w2src = w2_d[l].rearrange("(c p) n -> p c n", p=128)

            def load_chunk(c):
                s = c % 2
                for k in range(8):
                    dma(POOL, lambda e, k=k, s=s, c=c: e.dma_start(out=W1[s][:, k, :], in_=w1src[:, k, c * 512:(c + 1) * 512]), w=[W1R[s]], key="w1_%d" % s)
                for j in range(4):
                    dma(POOL, lambda e, j=j, s=s, c=c: e.dma_start(out=W2[s][:, j, :], in_=w2src[:, c * 4 + j, :]), w=[W2R[s]], key="w2_%d" % s)

            load_chunk(0)
            load_chunk(1)
            if l + 1 < l1:
                load_win(l + 1)
            nbanks = ((B0, R0), (B1, R1))
            for i in range(NT):
                bank, bres = nbanks[i % 2]
                rmsnorm_to_hT(i, h2T[:, :, i * 128:(i + 1) * 128], h2R[i], bank, bres, (st2, hn2, st2R, hn2R))
            abanks = ((B0, R0), (B1, R1), (B23[:, 0:512], R23))
            ybanks = ((B4, R4), (B5, R5), (B6, R6), (B7, R7))
            aj = 0
            yj = 0
            NG = NT // 4 if NT >= 4 else 1
            GT = NT // NG
            ntok = GT * 128
            cnt_ = {"aj": 0, "yj": 0}

            def emit_w1(idx):
                c, tg = idx // NG, idx % NG
                s = c % 2
                a_t, a_r = aT[idx % 2], aTRs[idx % 2]
                for j in range(4):
                    bank, bres = abanks[cnt_["aj"] % 3]
                    cnt_["aj"] += 1
                    for k in range(8):
                        pe(lambda e: e.matmul(bank[:, 0:ntok], lhsT=W1[s][:, k, j * 128:(j + 1) * 128], rhs=h2T[:, k, tg * ntok:(tg + 1) * ntok],
                                              start=(k == 0), stop=(k == 7)), r=[W1R[s]] + h2R[tg * GT:(tg + 1) * GT], w=[bres])
                    rr, rrR = r32[cnt_["aj"] % 4], r32R[cnt_["aj"] % 4]
                    act(lambda e: e.activation(out=rr[:, 0:ntok], in_=bank[:, 0:ntok], func=AF.Relu), r=[bres], w=[rrR])
                    dve(lambda e: e.tensor_tensor(out=a_t[:, j, 0:ntok], in0=rr[:, 0:ntok], in1=rr[:, 0:ntok], op=ALU.mult), r=[rrR], w=[a_r[j]])

            def emit_y(idx):
                c, tg = idx // NG, idx % NG
                s = c % 2
                a_t, a_r = aT[idx % 2], aTRs[idx % 2]
                for t4 in range(GT):
                    ti = tg * GT + t4
                    for hf in range(2):
                        bank, bres = ybanks[cnt_["yj"] % 4]
                        cnt_["yj"] += 1
                        for j in range(4):
                            pe(lambda e: e.matmul(bank, lhsT=a_t[:, j, t4 * 128:(t4 + 1) * 128], rhs=W2[s][:, j, hf * 512:(hf + 1) * 512],
                                                  start=(j == 0), stop=(j == 3)), r=[a_r[j], W2R[s]], w=[bres])
                        dve(lambda e: e.tensor_tensor(out=x_sb[:, ti, hf * 512:(hf + 1) * 512], in0=bank,
                                                      in1=x_sb[:, ti, hf * 512:(hf + 1) * 512], op=ALU.add), r=[bres, xR[ti]], w=[xR[ti]])
                if tg == NG - 1 and c + 2 < NCH:
                    load_chunk(c + 2)

            nsteps = NCH * NG
            for idx in range(nsteps):
                emit_w1(idx)
                if idx >= 1:
                    emit_y(idx - 1)
            emit_y(nsteps - 1)
            if ("xout_l%d" % l) in tap_d:
                for i in range(NT):
                    dma(SP, lambda e, i=i, l=l: e.dma_start(out=tap_d["xout_l%d" % l][i * 128:(i + 1) * 128, :], in_=x_sb[:, i, :]), r=[xR[i]], key="tap")

        S.stage(0)
        S.barrier()
        AB.reset()
        if not final:
            for i in range(NT):
                dma(SP, lambda e, i=i: e.dma_start(out=out_d[i * 128:(i + 1) * 128, :], in_=x_sb[:, i, :]), r=[xR[i]], key="out")
        else:
            dma(SP, lambda e: e.dma_start(out=gB[:], in_=fg_d.rearrange("(o d) -> o d", o=1).to_broadcast([128, D])), w=[gBR], key="g")
            fo = [AB.take([D], F32) for _ in range(2)]; foR = [Res("fo0"), Res("fo1")]
            fj = AB.take([D], BF16); fjR = Res("fj")
            fst = AB.take([8], F32); fstR = Res("fst")
            for i in range(NT):
                o_, oR_ = fo[i % 2], foR[i % 2]
                act(lambda e, i=i: e.activation(out=fj, in_=x_sb[:, i, :], func=AF.Square, accum_out=fst[:, 0:1]), r=[xR[i]], w=[fstR, fjR])
                act(lambda e: e.activation(out=fst[:, 1:2], in_=fst[:, 0:1], func=AF.Ln, scale=1.0 / D, bias=EPS), r=[fstR], w=[fstR])
                act(lambda e: e.activation(out=fst[:, 2:3], in_=fst[:, 1:2], func=AF.Exp, scale=-0.5), r=[fstR], w=[fstR])
                dve(lambda e, i=i, o_=o_: e.scalar_tensor_tensor(out=o_, in0=x_sb[:, i, :], scalar=fst[:, 2:3], in1=gB[:], op0=ALU.mult, op1=ALU.mult),
                    r=[xR[i], fstR, gBR], w=[oR_])
                dma(SP, lambda e, i=i, o_=o_: e.dma_start(out=out_d[i * 128:(i + 1) * 128, :], in_=o_), r=[oR_], key="out")

        fw = ["out"] + (["tap"] if tap_d else [])
        S.emit(final_waits=fw)
    return nc


_NC_CACHE = {}
SPLITS = [(0, 4)]


def kernel(x, positions, norm1_g, w_in, pool_w, pool_scale, lb_logits, hgrn_norm_g, w_out, norm2_g,
           w_ff_in, w_ff_out, final_norm_g):
    B, T, _ = x.shape
    NT = T // 128
    L = w_in.shape[0]
    splits = SPLITS if L == 4 else [(0, L)]
    progs = []
    for (a_, b_) in splits:
        key = (NT, L, a_, b_)
        if key not in _NC_CACHE:
            _NC_CACHE[key] = build_program(NT=NT, L=L, l0=a_, l1=b_, final=(b_ == L))
        progs.append(_NC_CACHE[key])
    f = lambda a: np.ascontiguousarray(np.asarray(a, dtype=np.float32))
    shared = {"norm1_g": f(norm1_g), "w_in": f(w_in), "pool_w": f(pool_w), "pool_scale": f(pool_scale),
              "lb_logits": f(lb_logits), "hgrn_norm_g": f(hgrn_norm_g), "w_out": f(w_out), "norm2_g": f(norm2_g),
              "w_ff_in": f(w_ff_in), "w_ff_out": f(w_ff_out), "final_norm_g": f(final_norm_g)}
    xs = np.asarray(x, dtype=np.float32)
    ps = np.asarray(positions, dtype=np.int32)
    in_maps = []
    for b in range(B):
        m = dict(shared)
        m["x"] = np.ascontiguousarray(xs[b])
        m["positions"] = np.ascontiguousarray(ps[b].reshape(NT, 128))
        in_maps.append(m)
    for nc in progs:
        res = run_bass_kernel_spmd(nc, in_maps, core_ids=list(range(B)))
        outs = [np.asarray(r["out"]) for r in res.results]
        for b in range(B):
            in_maps[b]["x"] = np.ascontiguousarray(outs[b])
    return np.stack(outs, axis=0).astype(np.float32)
```
